# Optimizing a Trainium2 kernel written in Bass

```python
import math
import jax, jax.numpy as jnp
from jax import lax
import numpy as np

D_MODEL = 1024
BATCH = 4
SEQ = 8192
DEPTH = 2

N_META = 16
D_RNN = D_MODEL
LRU_HEADS = 16
LRU_HEAD_DIM = D_RNN // LRU_HEADS
CONV_A = 4
LRU_C = 8.0
D_CONV = D_MODEL
CONV_B = 3
N_GROUPS = 4
EXPERTS_PER_GROUP = 8
N_EXPERTS = N_GROUPS * EXPERTS_PER_GROUP
TOP_K = 2
D_EXPERT = D_MODEL // 2
BLK = 128
N_MIXERS = 2
N_A_LAYERS = (DEPTH + 1) // 2
N_B_LAYERS = DEPTH // 2
ALPHA = (2.0 * DEPTH) ** 0.25
BETA = (8.0 * DEPTH) ** -0.25
LN_EPS = 1e-5

kernel_name = "hybrid_rglru_shortconv_hmoe_deepnorm"


def layer_norm(x, g, b):
    xf = x.astype(jnp.float32)
    mu = jnp.mean(xf, axis=-1, keepdims=True)
    var = jnp.mean(jnp.square(xf - mu), axis=-1, keepdims=True)
    y = (xf - mu) * lax.rsqrt(var + LN_EPS) * g.astype(jnp.float32) + b.astype(jnp.float32)
    return y.astype(x.dtype)


def causal_depthwise_conv(x, w):
    K, C = w.shape
    return lax.conv_general_dilated(
        x, w[:, None, :].astype(x.dtype), window_strides=(1,), padding=[(K - 1, 0)],
        dimension_numbers=("NWC", "WIO", "NWC"), feature_group_count=C)


def _linear_recurrence_combine(c1, c2):
    a1, b1 = c1
    a2, b2 = c2
    return a1 * a2, a2 * b1 + b2


def rglru_mixer(h, w_in, conv_w, conv_b, w_a, b_a, w_i, b_i, lam, w_out):
    Bsz, S, _ = h.shape
    u = h @ w_in
    y_br = jax.nn.gelu(u[..., :D_RNN])
    xc = causal_depthwise_conv(u[..., D_RNN:], conv_w) + conv_b
    xh = xc.reshape(Bsz, S, LRU_HEADS, LRU_HEAD_DIM)
    r = jax.nn.sigmoid(jnp.einsum("bshd,hde->bshe", xh, w_a).reshape(Bsz, S, D_RNN) + b_a)
    gi = jax.nn.sigmoid(jnp.einsum("bshd,hde->bshe", xh, w_i).reshape(Bsz, S, D_RNN) + b_i)
    log_a = -LRU_C * r.astype(jnp.float32) * jax.nn.softplus(-lam.astype(jnp.float32))
    a = jnp.exp(log_a)
    mult = jnp.sqrt(-jnp.expm1(2.0 * log_a))
    b = mult * (gi * xc).astype(jnp.float32)
    _, hs = lax.associative_scan(_linear_recurrence_combine, (a, b), axis=1)
    return (hs.astype(h.dtype) * y_br) @ w_out


def shortconv_mixer(h, w_in, conv_w, w_out):
    u = h @ w_in
    bg = u[..., :D_CONV]
    cg = u[..., D_CONV:2 * D_CONV]
    v = u[..., 2 * D_CONV:]
    zc = causal_depthwise_conv(cg * v, conv_w)
    return (bg * zc) @ w_out


def hierarchical_moe(h, w_group, b_group, w_expert, b_expert, w_gate, w_up, w_down):
    Bsz, S, D = h.shape
    x2 = h.reshape(Bsz * S, D)
    T = x2.shape[0]
    xf = x2.astype(jnp.float32)
    glog = xf @ w_group.astype(jnp.float32) + b_group.astype(jnp.float32)
    gprob = jax.nn.softmax(glog, axis=-1)
    g = jnp.argmax(glog, axis=-1)
    pg = jnp.take_along_axis(gprob, g[:, None], axis=1)
    elog = (xf @ w_expert.astype(jnp.float32)).reshape(T, N_GROUPS, EXPERTS_PER_GROUP) + b_expert.astype(jnp.float32)
    elog_g = jnp.take_along_axis(elog, g[:, None, None], axis=1)[:, 0]
    vals, loc = lax.top_k(elog_g, TOP_K)
    gates = pg * jax.nn.softmax(vals, axis=-1)
    eid = g[:, None] * EXPERTS_PER_GROUP + loc
    A = T * TOP_K
    eid_f = eid.reshape(-1)
    tok_f = jnp.repeat(jnp.arange(T, dtype=jnp.int32), TOP_K)
    gate_f = gates.reshape(-1)
    order = jnp.argsort(eid_f)
    eid_s = eid_f[order]
    tok_s = tok_f[order]
    gate_s = gate_f[order]
    counts = jnp.bincount(eid_f, length=N_EXPERTS)
    start = jnp.cumsum(counts) - counts
    padded = (counts + BLK - 1) // BLK * BLK
    pend = jnp.cumsum(padded)
    pstart = pend - padded
    rank = jnp.arange(A, dtype=jnp.int32) - start[eid_s]
    dest = pstart[eid_s] + rank
    n_blocks = (A + N_EXPERTS * (BLK - 1) + BLK - 1) // BLK
    P = n_blocks * BLK
    slot_tok = jnp.full((P,), T, dtype=jnp.int32).at[dest].set(tok_s)
    slot_gate = jnp.zeros((P,), jnp.float32).at[dest].set(gate_s)
    block_expert = jnp.clip(jnp.searchsorted(pend, jnp.arange(n_blocks) * BLK, side="right"), 0, N_EXPERTS - 1)
    xpad = jnp.concatenate([x2, jnp.zeros((1, D), x2.dtype)], axis=0)
    xs = xpad[slot_tok].reshape(n_blocks, BLK, D)

    def expert_block(args):
        xb, e = args
        hb = jax.nn.silu(xb @ w_gate[e]) * (xb @ w_up[e])
        return hb @ w_down[e]

    ys = lax.map(expert_block, (xs, block_expert)).reshape(P, D)
    y = jnp.zeros((T + 1, D), x2.dtype).at[slot_tok].add(ys * slot_gate[:, None].astype(ys.dtype))
    return y[:T].reshape(Bsz, S, D)


def setup_inputs(seed: int = 0) -> dict:
    key = jax.random.key(seed)
    ks = jax.random.split(key, 24)

    def nrm(k, shape, scale):
        return jax.random.normal(k, shape, jnp.float32) * scale

    u = jax.random.uniform(ks[10], (N_A_LAYERS, D_RNN), jnp.float32, 0.9, 0.999)
    a0 = u ** (1.0 / LRU_C)
    lru_lambda = jnp.log(a0) - jnp.log1p(-a0)
    return {
        "x": nrm(ks[0], (BATCH, SEQ, D_MODEL), 1.0),
        "meta_tokens": nrm(ks[1], (N_META, D_MODEL), 1.0),
        "lru_w_in": nrm(ks[2], (N_A_LAYERS, D_MODEL, 2 * D_RNN), D_MODEL ** -0.5),
        "lru_conv_w": nrm(ks[3], (N_A_LAYERS, CONV_A, D_RNN), CONV_A ** -0.5),
        "lru_conv_b": nrm(ks[4], (N_A_LAYERS, D_RNN), 0.01),
        "lru_w_a": nrm(ks[5], (N_A_LAYERS, LRU_HEADS, LRU_HEAD_DIM, LRU_HEAD_DIM), LRU_HEAD_DIM ** -0.5),
        "lru_b_a": nrm(ks[6], (N_A_LAYERS, D_RNN), 0.01),
        "lru_w_i": nrm(ks[7], (N_A_LAYERS, LRU_HEADS, LRU_HEAD_DIM, LRU_HEAD_DIM), LRU_HEAD_DIM ** -0.5),
        "lru_b_i": nrm(ks[8], (N_A_LAYERS, D_RNN), 0.01),
        "lru_lambda": lru_lambda,
        "lru_w_out": nrm(ks[9], (N_A_LAYERS, D_RNN, D_MODEL), BETA * D_RNN ** -0.5),
        "sc_w_in": nrm(ks[11], (N_B_LAYERS, D_MODEL, 3 * D_CONV), D_MODEL ** -0.5),
        "sc_conv_w": nrm(ks[12], (N_B_LAYERS, CONV_B, D_CONV), CONV_B ** -0.5),
        "sc_w_out": nrm(ks[13], (N_B_LAYERS, D_CONV, D_MODEL), BETA * D_CONV ** -0.5),
        "moe_w_group": nrm(ks[14], (DEPTH, D_MODEL, N_GROUPS), D_MODEL ** -0.5),
        "moe_b_group": nrm(ks[15], (DEPTH, N_GROUPS), 0.01),
        "moe_w_expert": nrm(ks[16], (DEPTH, D_MODEL, N_EXPERTS), D_MODEL ** -0.5),
        "moe_b_expert": nrm(ks[17], (DEPTH, N_GROUPS, EXPERTS_PER_GROUP), 0.01),
        "moe_w_gate": nrm(ks[18], (DEPTH, N_EXPERTS, D_MODEL, D_EXPERT), D_MODEL ** -0.5),
        "moe_w_up": nrm(ks[19], (DEPTH, N_EXPERTS, D_MODEL, D_EXPERT), D_MODEL ** -0.5),
        "moe_w_down": nrm(ks[20], (DEPTH, N_EXPERTS, D_EXPERT, D_MODEL), BETA * D_EXPERT ** -0.5),
        "ln_g": 1.0 + nrm(ks[21], (DEPTH, 2, D_MODEL), 0.02),
        "ln_b": nrm(ks[22], (DEPTH, 2, D_MODEL), 0.02),
    }


def reference(x, meta_tokens, lru_w_in, lru_conv_w, lru_conv_b, lru_w_a, lru_b_a, lru_w_i, lru_b_i,
              lru_lambda, lru_w_out, sc_w_in, sc_conv_w, sc_w_out, moe_w_group, moe_b_group,
              moe_w_expert, moe_b_expert, moe_w_gate, moe_w_up, moe_w_down, ln_g, ln_b):
    Bsz = x.shape[0]
    meta = jnp.broadcast_to(meta_tokens.astype(x.dtype)[None], (Bsz, N_META, D_MODEL))
    h = jnp.concatenate([meta, x], axis=1)
    for i in range(DEPTH):
        j = i // N_MIXERS
        if i % N_MIXERS == 0:
            mixed = rglru_mixer(h, lru_w_in[j], lru_conv_w[j], lru_conv_b[j], lru_w_a[j], lru_b_a[j],
                                lru_w_i[j], lru_b_i[j], lru_lambda[j], lru_w_out[j])
        else:
            mixed = shortconv_mixer(h, sc_w_in[j], sc_conv_w[j], sc_w_out[j])
        h = layer_norm(ALPHA * h + mixed, ln_g[i, 0], ln_b[i, 0])
        ffn = hierarchical_moe(h, moe_w_group[i], moe_b_group[i], moe_w_expert[i], moe_b_expert[i],
                               moe_w_gate[i], moe_w_up[i], moe_w_down[i])
        h = layer_norm(ALPHA * h + ffn, ln_g[i, 1], ln_b[i, 1])
    return h[:, N_META:]
```

```python
import numpy as np
import concourse.bass as bass
import concourse.mybir as mybir
from concourse.bass_utils import run_bass_kernel_spmd

F32 = mybir.dt.float32
BF16 = mybir.dt.bfloat16
I32 = mybir.dt.int32
AF = mybir.ActivationFunctionType
ALU = mybir.AluOpType
AX = mybir.AxisListType

D = 1024
KC = 8
NHEAD = 16
NMAIN = 4096
NT = NHEAD + NMAIN
NPRE = 4096
NE = 32
CAP = 384
NB = CAP // 128
NSLOT = NE * CAP
DEXP = 512
ALPHA = (2.0 * 2) ** 0.25
LN_EPS = 1e-5
GC0 = 0.7978845608028654
GC1 = 0.044715
BIG = 1.0e9

TT = [(0, NHEAD)] + [(NHEAD + 128 * i, 128) for i in range(NMAIN // 128)]
NTT = len(TT)
FM = [(0, NHEAD, [0])] + [(NHEAD + 512 * k, 512, [1 + 4 * k + j for j in range(4)]) for k in range(NMAIN // 512)]
FMPRE = [(512 * k, 512, None) for k in range(NPRE // 512)]


STRICT_SAME_ENGINE = True


class Sched:
    def __init__(self, nc, n_dma_sems=20):
        self.nc = nc
        self.eng = {"pe": nc.tensor, "act": nc.scalar, "dve": nc.vector, "pool": nc.gpsimd, "sp": nc.sync}
        self.sem = {k: nc.alloc_semaphore("s_" + k) for k in ("pe", "act", "dve", "pool")}
        self.cnt = {k: 0 for k in self.sem}
        self.dsem = {q: [nc.alloc_semaphore("d_%s_%d" % (q, i)) for i in range(n_dma_sems)] for q in ("sp", "pool")}
        self.dsem["stg"] = [nc.alloc_semaphore("d_stg_%d" % i) for i in range(10)]
        self.dcnt = {q: [0] * len(self.dsem[q]) for q in self.dsem}
        self.drr = {q: 0 for q in self.dsem}
        self.waited = {k: {} for k in self.eng}
        self.bufs = {}
        self.semobj = {}
        for k, s in self.sem.items():
            self.semobj[id(s)] = s
        self.all_events = {}

    def _wait(self, e, ev):
        s, v = ev
        w = self.waited[e]
        if w.get(id(s), 0) >= v:
            return
        self.eng[e].wait_ge(s, v)
        w[id(s)] = v

    def _deps(self, e, reads, writes, dist=3):
        evs = []
        for k in reads:
            b = self.bufs.get(k)
            if b and b["w"]:
                evs.append(b["w"])
        for k in writes:
            b = self.bufs.get(k)
            if b:
                if b["w"]:
                    evs.append(b["w"])
                evs.extend(b["r"])
        for ev in evs:
            s, v = ev
            if e in self.sem and s is self.sem[e]:
                if e == "pe" or (not STRICT_SAME_ENGINE and self.cnt[e] - v >= dist):
                    continue
            self._wait(e, ev)

    def _record(self, ev, reads, writes):
        for k in reads:
            self.bufs.setdefault(k, {"w": None, "r": []})["r"].append(ev)
        for k in writes:
            self.bufs[k] = {"w": ev, "r": []}
        self.all_events[id(ev[0])] = ev

    def op(self, e, fn, reads=(), writes=(), dist=3):
        self._deps(e, reads, writes, dist)
        ins = fn(self.eng[e])
        ins.then_inc(self.sem[e], 1)
        self.cnt[e] += 1
        ev = (self.sem[e], self.cnt[e])
        self._record(ev, reads, writes)
        return ev

    def group(self, e, fns, reads=(), writes=()):
        self._deps(e, reads, writes)
        ins = None
        for fn in fns:
            ins = fn(self.eng[e])
        ins.then_inc(self.sem[e], 1)
        self.cnt[e] += 1
        ev = (self.sem[e], self.cnt[e])
        self._record(ev, reads, writes)
        return ev

    def dma(self, q, fn, reads=(), writes=(), sems=None):
        self._deps(q, reads, writes)
        sq = sems or q
        i = self.drr[sq]
        self.drr[sq] = (i + 1) % len(self.dsem[sq])
        s = self.dsem[sq][i]
        if self.dcnt[sq][i] > 0:
            self._wait(q, (s, 16 * self.dcnt[sq][i]))
        ins = fn(self.eng[q])
        ins.then_inc(s, 16)
        self.dcnt[sq][i] += 1
        ev = (s, 16 * self.dcnt[sq][i])
        self._record(ev, reads, writes)
        return ev

    def barrier(self):
        for e in self.eng:
            for ev in list(self.all_events.values()):
                s, v = ev
                if e in self.sem and s is self.sem[e]:
                    continue
                self._wait(e, ev)
        self.bufs = {}


def build_program(phases=("A", "M0", "B", "M1"), debug_out=False):
    nc = bass.Bass("TRN2", target_bir_lowering=False)
    S = Sched(nc)

    def din(name, shape, dt=F32):
        return nc.dram_tensor(name, list(shape), dt, kind="ExternalInput").ap()

    scratch_kind = "ExternalOutput" if debug_out else "Internal"

    def dsc(name, shape, dt=F32):
        return nc.dram_tensor(name, list(shape), dt, kind=scratch_kind).ap()

    xin = din("xin", [NT, D])
    xpre = din("xpre", [NPRE, D])
    flag = din("flag", [128, 1])
    a_win = din("a_win", [D, 2 * D])
    a_wout = din("a_wout", [D, D])
    a_vec = din("a_vec", [128, 8 * KC])
    a_cbrow = din("a_cbrow", [2, D])
    a_wa = din("a_wa", [128, KC, 128])
    a_wi = din("a_wi", [128, KC, 128])
    b_win = din("b_win", [D, 3 * D])
    b_wout = din("b_wout", [D, D])
    b_vec = din("b_vec", [128, 3 * KC])
    lnp = din("lnp", [4, 2, 128, D])
    wr = din("wr", [2, D, 36])
    br = din("br", [2, 128, 36])
    wgate = din("wgate", [2, NE, D, DEXP])
    wup = din("wup", [2, NE, D, DEXP])
    wdown = din("wdown", [2, NE, DEXP, D])
    cst = din("cst", [128, 128 * 4 + 64])
    csti = din("csti", [128, NTT * 2], I32)
    out = nc.dram_tensor("out", [NMAIN, D], F32, kind="ExternalOutput").ap()

    hA = dsc("hA", [NT, D])
    hAb = dsc("hAb", [NT + 1, D], BF16)
    hB = dsc("hB", [NT, D])
    stok = dsc("stok", [128 * NE * NB, 2], I32)
    ys = dsc("ys", [NSLOT + 1, D])
    wgbL = [nc.dram_tensor("wgb%d" % l, [NE, D, DEXP], BF16, kind="Internal").ap() for l in range(2)]
    wubL = [nc.dram_tensor("wub%d" % l, [NE, D, DEXP], BF16, kind="Internal").ap() for l in range(2)]
    wdbL = [nc.dram_tensor("wdb%d" % l, [NE, DEXP, D], BF16, kind="Internal").ap() for l in range(2)]

    STAGED = {0: ("g", "u", "d"), 1: ("g", "u", "d")}

    def staging_list(layer):
        lst = []
        wgb, wub, wdb = wgbL[layer], wubL[layer], wdbL[layer]
        for e in range(NE):
            if "g" in STAGED[layer]:
                lst.append(lambda e=e: S.dma("pool", lambda g: g.dma_start(out=wgb[e], in_=wgate[layer, e]), writes=[("wgb", e)], sems="stg"))
            if "u" in STAGED[layer]:
                lst.append(lambda e=e: S.dma("pool", lambda g: g.dma_start(out=wub[e], in_=wup[layer, e]), writes=[("wub", e)], sems="stg"))
            if "d" in STAGED[layer]:
                lst.append(lambda e=e: S.dma("pool", lambda g: g.dma_start(out=wdb[e], in_=wdown[layer, e]), writes=[("wdb", e)], sems="stg"))
        return lst

    import contextlib
    top = contextlib.ExitStack()

    uniq = [0]

    def sb(stack, name, shape, dt=F32):
        uniq[0] += 1
        return stack.enter_context(nc.sbuf_tensor("%s_%d" % (name, uniq[0]), list(shape), dt))

    def ps(stack, name, shape, dt=F32):
        return stack.enter_context(nc.psum_tensor(name, list(shape), dt))

    ident_b = sb(top, "ident_b", [128, 128], BF16)
    ident_f = sb(top, "ident_f", [128, 128])
    tri_b = sb(top, "tri_b", [128, 128], BF16)
    ones_b = sb(top, "ones_b", [128, 128], BF16)
    ecrow = sb(top, "ecrow", [128, NE])
    tokidx = sb(top, "tokidx", [128, NTT, 2], I32)
    dest = [sb(top, "dest%d" % k, [128, NTT], I32) for k in range(2)]
    ysrow = [sb(top, "ysrow%d" % k, [128, NTT], I32) for k in range(2)]
    gate = [sb(top, "gate%d" % k, [128, NTT]) for k in range(2)]
    c_mhalf = sb(top, "c_mhalf", [128, 1])
    zrow = sb(top, "zrow", [1, D], BF16)
    zrowf = sb(top, "zrowf", [1, D])
    bank = [ps(top, "bank%d" % i, [128, 512]) for i in range(8)]
    tpb_f = bank[0]
    tpb_all = bank[0][:].bitcast(BF16)
    tpb_7 = bank[7][:].bitcast(BF16)

    bc_reg = nc.gpsimd.alloc_register("bc_reg")
    nc.gpsimd.reg_mov(bc_reg, 128 * NE * NB - 1)
    S.dma("pool", lambda g: g.dma_start(out=ident_b[:], in_=cst[:, 0:128]), writes=["ident_b"])
    S.dma("sp", lambda g: g.dma_start(out=ident_f[:], in_=cst[:, 0:128]), writes=["ident_f"])
    S.dma("pool", lambda g: g.dma_start(out=tri_b[:], in_=cst[:, 128:256]), writes=["tri_b"])
    S.dma("pool", lambda g: g.dma_start(out=ones_b[:], in_=cst[:, 256:384]), writes=["ones_b"])
    S.dma("sp", lambda g: g.dma_start(out=ecrow[:], in_=cst[:, 512:512 + NE]), writes=["ecrow"])
    S.dma("sp", lambda g: g.dma_start(out=tokidx[:], in_=csti.rearrange("p (t o) -> p t o", o=2)), writes=["tokidx"])
    S.op("dve", lambda v: v.memset(c_mhalf[:], -0.5), writes=["c_mhalf"])
    S.op("dve", lambda v: v.memset(zrow[:], 0.0), writes=["zrow"])
    S.op("dve", lambda v: v.memset(zrowf[:], 0.0), writes=["zrowf"])
    S.dma("sp", lambda g: g.dma_start(out=hAb[NT:NT + 1, :], in_=zrow[:]), reads=["zrow"], writes=["hAb_z"])
    S.dma("sp", lambda g: g.dma_start(out=ys[NSLOT:NSLOT + 1, :], in_=zrowf[:]), reads=["zrowf"], writes=["ys_z"])

    STG_DONE = [0]
    STG_NEED = {0: 3 * NE, 1: 6 * NE}
    STG_Q = []
    for l_ in range(2):
        for fn_ in staging_list(l_):
            STG_Q.append(lambda fn_=fn_: (fn_(), STG_DONE.__setitem__(0, STG_DONE[0] + 1)))

    def mixer_phase(kind, layer, src, src_pre):
        st = contextlib.ExitStack()
        isA = (kind == "A")
        ncol = 2 if isA else 3
        win_d = a_win if isA else b_win
        wout_d = a_wout if isA else b_wout
        win = sb(st, "win", [128, KC, ncol * D], BF16)
        wout = sb(st, "wout", [128, KC, D], BF16)
        lng = sb(st, "lng", [128, D])
        lnb = sb(st, "lnb", [128, D])
        wrt = sb(st, "wrt", [128, KC, 36])
        brt = sb(st, "brt", [128, 36])
        ecrow4 = sb(st, "ecrow4", [128, 4, NE])
        for k in range(KC):
            S.dma("pool", lambda g, k=k: g.dma_start(out=win[:, k, :], in_=win_d[k * 128:(k + 1) * 128, :]),
                  writes=[("win", k)])
        S.dma("pool", lambda g: g.dma_start(out=wout[:], in_=wout_d.rearrange("(k p) m -> p k m", p=128)),
              writes=["wout"])
        S.dma("sp", lambda g: g.dma_start(out=lng[:], in_=lnp[layer * 2, 0]), writes=["lng"])
        S.dma("sp", lambda g: g.dma_start(out=lnb[:], in_=lnp[layer * 2, 1]), writes=["lnb"])
        S.dma("sp", lambda g: g.dma_start(out=wrt[:], in_=wr[layer].rearrange("(k p) n -> p k n", p=128)),
              writes=["wrt"])
        S.dma("sp", lambda g: g.dma_start(out=brt[:], in_=br[layer]), writes=["brt"])
        for j in range(4):
            S.dma("sp", lambda g, j=j: g.dma_start(out=ecrow4[:, j, :], in_=cst[:, 512:512 + NE]), writes=[("ecrow4", j)])
        EC4 = [("ecrow4", j) for j in range(4)]
        WIN_R = [("win", k) for k in range(KC)]

        if isA:
            HIST, NK = 3, 4
            vec = sb(st, "vec", [128, 8, KC])
            wa = sb(st, "wa", [128, KC, 128], BF16)
            wi = sb(st, "wi", [128, KC, 128], BF16)
            S.dma("sp", lambda g: g.dma_start(out=vec[:], in_=a_vec.rearrange("p (v c) -> p v c", v=8)), writes=["vec"])
            S.dma("pool", lambda g: g.dma_start(out=wa[:], in_=a_wa[:, :, :]), writes=["wa"])
            S.dma("pool", lambda g: g.dma_start(out=wi[:], in_=a_wi[:, :, :]), writes=["wi"])
            flg = sb(st, "flg", [128, 1])
            S.dma("sp", lambda g: g.dma_start(out=flg[:], in_=flag[:, :]), writes=["flg"])
            sc = sb(st, "sc", [128, KC]); sch = sb(st, "sch", [128, KC])
            hba = sb(st, "hba", [128, KC]); hbi = sb(st, "hbi", [128, KC])
            t1 = sb(st, "spt1", [128, KC]); t2 = sb(st, "spt2", [128, KC]); t3 = sb(st, "spt3", [128, KC])
            t4 = sb(st, "spt4", [128, KC])
            lam = vec[:, 7, :]
            S.op("dve", lambda v: v.tensor_scalar_mul(out=t1[:], in0=lam, scalar1=-1.0), reads=["vec"], writes=["t1"])
            S.op("dve", lambda v: v.tensor_max(out=t1[:], in0=t1[:], in1=lam), reads=["vec", "t1"], writes=["t1"])
            S.op("act", lambda a: a.activation(out=t2[:], in_=t1[:], func=AF.Exp, scale=-1.0), reads=["t1"], writes=["t2"])
            S.op("dve", lambda v: v.tensor_scalar_add(out=t3[:], in0=t2[:], scalar1=2.0), reads=["t2"], writes=["t3"])
            S.op("dve", lambda v: v.reciprocal(out=t3[:], in_=t3[:]), reads=["t3"], writes=["t3"])
            S.op("dve", lambda v: v.tensor_mul(out=t2[:], in0=t2[:], in1=t3[:]), reads=["t2", "t3"], writes=["t2"])
            S.op("dve", lambda v: v.tensor_mul(out=t3[:], in0=t2[:], in1=t2[:]), reads=["t2"], writes=["t3"])
            S.op("dve", lambda v: v.memset(t4[:], 1.0 / 17.0), writes=["t4"])
            for nn in (15, 13, 11, 9, 7, 5, 3, 1):
                S.op("dve", lambda v: v.tensor_mul(out=t4[:], in0=t4[:], in1=t3[:]), reads=["t4", "t3"], writes=["t4"])
                S.op("dve", lambda v, nn=nn: v.tensor_scalar_add(out=t4[:], in0=t4[:], scalar1=1.0 / nn), reads=["t4"], writes=["t4"])
            S.op("dve", lambda v: v.tensor_mul(out=t4[:], in0=t4[:], in1=t2[:]), reads=["t4", "t2"], writes=["t4"])
            S.op("dve", lambda v: v.tensor_scalar(out=t1[:], in0=lam, scalar1=-1.0, scalar2=0.0, op0=ALU.mult, op1=ALU.max),
                 reads=["vec", "t1"], writes=["t1"])
            S.op("dve", lambda v: v.scalar_tensor_tensor(out=t1[:], in0=t4[:], scalar=2.0, in1=t1[:], op0=ALU.mult, op1=ALU.add),
                 reads=["t4", "t1"], writes=["t1"])
            S.op("dve", lambda v: v.tensor_scalar_mul(out=sc[:], in0=t1[:], scalar1=-8.0), reads=["t1"], writes=["sc"])
            S.op("dve", lambda v: v.tensor_scalar_mul(out=sch[:], in0=t1[:], scalar1=-4.0), reads=["t1"], writes=["sch"])
            S.op("dve", lambda v: v.tensor_scalar_mul(out=hba[:], in0=vec[:, 5, :], scalar1=0.5), reads=["vec"], writes=["hba"])
            S.op("dve", lambda v: v.tensor_scalar_mul(out=hbi[:], in0=vec[:, 6, :], scalar1=0.5), reads=["vec"], writes=["hbi"])
            state = sb(st, "state", [128, KC])
            S.op("dve", lambda v: v.memset(state[:], 0.0), writes=[("state", c) for c in range(KC)])
            cbhl = sb(st, "cbhl", [2, D], BF16); ones2 = sb(st, "ones2", [2, 512], BF16)
            st_tmp = contextlib.ExitStack()
            cb2 = sb(st_tmp, "cb2", [2, D]); cbh = sb(st_tmp, "cbh", [2, D], BF16); cbhf = sb(st_tmp, "cbhf", [2, D])
            S.dma("sp", lambda g: g.dma_start(out=cb2[:], in_=a_cbrow[:, :]), writes=["cb2"])
            S.op("dve", lambda v: v.tensor_copy(out=cbh[:], in_=cb2[:]), reads=["cb2"], writes=["cbh"])
            S.op("dve", lambda v: v.tensor_copy(out=cbhf[:], in_=cbh[:]), reads=["cbh"], writes=["cbhf"])
            S.op("dve", lambda v: v.tensor_sub(out=cb2[:], in0=cb2[:], in1=cbhf[:]), reads=["cb2", "cbhf"], writes=["cb2"])
            S.op("dve", lambda v: v.tensor_scalar(out=cbhf[:], in0=cbhf[:], scalar1=ident_f[0:2, 0:1], scalar2=None, op0=ALU.mult),
                 reads=["cbhf", "ident_f"], writes=["cbhf"])
            S.op("dve", lambda v: v.scalar_tensor_tensor(out=cbhl[:], in0=cb2[:], scalar=ident_f[0:2, 1:2], in1=cbhf[:],
                                                         op0=ALU.mult, op1=ALU.add), reads=["cb2", "cbhf", "ident_f"], writes=["cbhl"])
            S.op("dve", lambda v: v.memset(ones2[:], 1.0), writes=["ones2"])
            S.barrier()
            st_tmp.close()
        else:
            HIST, NK = 2, 3
            vec = sb(st, "vec", [128, 3, KC])
            S.dma("sp", lambda g: g.dma_start(out=vec[:], in_=b_vec.rearrange("p (v c) -> p v c", v=3)), writes=["vec"])
        ub = sb(st, "ub", [128, KC, HIST + 512], BF16)
        S.op("dve", lambda v: v.memset(ub[:], 0.0), writes=[("ub", c) for c in range(KC)])
        dg = sb(st, "dg", [128, KC, NK, 128], BF16)
        for c in range(KC):
            for k in range(NK):
                S.op("dve", lambda v, c=c, k=k: v.tensor_scalar(out=dg[:, c, k, :], in0=ident_f[:], scalar1=vec[:, k, c:c + 1], scalar2=None,
                                                                op0=ALU.mult), reads=["vec", "ident_f"], writes=[("dg", c)])

        xtok = sb(st, "xtok", [128, 4, D], BF16)
        xT = [sb(st, "xT%d" % i, [128, KC, 512], BF16) for i in range(2)]
        zT = [sb(st, "zT%d" % i, [128, KC, 512], BF16) for i in range(2)]
        NS = 4
        names = ("thr", "a2", "thi", "g", "uys") if isA else ("g", "thr")
        T = [{nm: sb(st, "t_%s%d" % (nm, i), [128, 512]) for nm in names} for i in range(NS)]
        xcb = [sb(st, "xcb%d" % i, [128, 512], BF16) for i in range(NS)] if isA else None
        if isA:
            B_TP, B_UX, B_UY, B_XC, B_R, B_I, B_MIX = 0, 1, 2, (3, 4), 5, 6, 7
            LGB = 2
        else:
            B_TP, B_CG, B_V, B_BG, B_XC, B_MIX = 0, 1, 2, 3, (4, 5), 7
            LGB = 6
        xres2 = [sb(st, "xres%d" % i, [128, D]) for i in range(3)]
        h1 = sb(st, "h1", [128, D]); h1T = sb(st, "h1T", [128, D])
        stats = sb(st, "stats", [128, 2, 6]); mv = sb(st, "mv", [128, 2]); rstd = sb(st, "rstd", [128, 1])
        nmr = sb(st, "nmr", [128, 1])
        lg2 = [sb(st, "lg%d" % i, [128, 4, 36]) for i in range(2)]; gmx = sb(st, "gmx", [128, 4]); gsh = sb(st, "gsh", [128, 4, 4])
        gex = sb(st, "gex", [128, 4, 4]); gsum = sb(st, "gsum", [128, 4]); pg = sb(st, "pg", [128, 4])
        goh = sb(st, "goh", [128, 4, 4]); elm = sb(st, "elm", [128, 4, NE]); el2 = sb(st, "el2", [128, 4, NE])
        m1 = sb(st, "m1", [128, 4]); m2 = sb(st, "m2", [128, 4]); oh1 = sb(st, "oh1", [128, 4, NE]); oh2 = sb(st, "oh2", [128, 4, NE])
        ohb = sb(st, "ohb", [128, 4, NE], BF16); w1 = sb(st, "w1", [128, 4])
        tot = sb(st, "tot", [128, NE]); rk = sb(st, "rk", [128, 4, NE]); ov = sb(st, "ov", [128, 4, NE])
        sel = sb(st, "sel", [128, 4, NE]); rkk = sb(st, "rkk", [128, 4]); ysf = sb(st, "ysf", [128, 4])
        pf = sb(st, "pf", [128, 4]); bf = sb(st, "bf", [128, 4]); bf2 = sb(st, "bf2", [128, 4])
        eif = sb(st, "eif", [128, 4]); ovk = sb(st, "ovk", [128, 4])
        S.op("dve", lambda v: v.memset(tot[:], 0.0), writes=["tot"])
        S.op("dve", lambda v: v.memset(ohb[:], 0.0), writes=["ohb"])
        for k in range(2):
            S.op("dve", lambda v, k=k: v.memset(dest[k][:], 1 << 24), writes=[("dest", k)])
            S.op("dve", lambda v, k=k: v.memset(ysrow[k][:], NSLOT), writes=[("ysrow", k)])
            S.op("dve", lambda v, k=k: v.memset(gate[k][:], 0.0), writes=[("gate", k)])
        stinit = sb(st, "stinit", [128, NE * NB * 2], I32)
        S.op("dve", lambda v: v.memset(stinit[:], NT), writes=["stinit"])
        S.dma("sp", lambda g: g.dma_start(out=stok.rearrange("(p f) o -> p (f o)", p=128), in_=stinit[:]),
              reads=["stinit"], writes=["stok"])

        def load_dma(dsrc, tok0, n):
            nt = (n + 127) // 128
            if n >= 128:
                S.dma("pool", lambda g: g.dma_start(out=xtok[:, 0:nt, :],
                                                    in_=dsrc[tok0:tok0 + n, :].rearrange("(t p) d -> p t d", p=128)),
                      writes=["xtok"])
            else:
                S.dma("pool", lambda g: g.dma_start(out=xtok[0:n, 0, :], in_=dsrc[tok0:tok0 + n, :]), writes=["xtok"])

        def load_tr(n, slot):
            nt = (n + 127) // 128
            rows = min(n, 128)
            for t in range(nt):
                bk_ = 0 if t % 2 == 0 else 7
                tpx = tpb_all if bk_ == 0 else tpb_7
                S.group("pe", [lambda pe, t=t, j=j: pe.transpose(out=tpx[:, j * 128:j * 128 + rows],
                                                                 in_=xtok[0:rows, t, j * 128:(j + 1) * 128],
                                                                 identity=ident_b[0:rows, 0:rows]) for j in range(KC)],
                        reads=["xtok", "ident_b"], writes=[("bank", bk_)])
                if t % 2 == 0:
                    S.op("act", lambda a, t=t: a.activation(out=xT[slot][:, :, t * 128:t * 128 + rows],
                                                            in_=tpx.rearrange("p (k t) -> p k t", k=KC)[:, :, 0:rows], func=AF.Copy),
                         reads=[("bank", bk_)], writes=[("xT", slot, t)])
                else:
                    S.op("dve", lambda v, t=t: v.tensor_copy(out=xT[slot][:, :, t * 128:t * 128 + rows],
                                                             in_=tpx.rearrange("p (k t) -> p k t", k=KC)[:, :, 0:rows]),
                         reads=[("bank", bk_)], writes=[("xT", slot, t)])

        def mm_acc(bk, col0, n, slot):
            return [lambda pe, k=k: pe.matmul(out=bank[bk][:, 0:n], lhsT=win[:, k, col0:col0 + 128], rhs=xT[slot][:, k, 0:n],
                                              start=(k == 0), stop=(k == KC - 1)) for k in range(KC)]

        def conv_mm(c, n, bk):
            fns = [lambda pe, k=k: pe.matmul(out=bank[bk][:, 0:n], lhsT=dg[:, c, k, :], rhs=ub[:, c, k:k + n],
                                             start=(k == 0), stop=(k == NK - 1 and not isA)) for k in range(NK)]
            if isA:
                fns.append(lambda pe: pe.matmul(out=bank[bk][:, 0:n], lhsT=cbhl[0:2, c * 128:(c + 1) * 128], rhs=ones2[0:2, 0:n],
                                                start=False, stop=True))
            return fns

        def hist_update(c, n):
            S.op("pool", lambda g: g.tensor_copy(out=ub[:, c, 0:HIST], in_=ub[:, c, n:n + HIST]),
                 reads=[("ub", c)], writes=[("ub", c)])

        F = 1

        def A_F1(P, n, slot, full):
            XTR = [("xT", slot, t_) for t_ in range(4)]
            bux = {}
            for idx, (c, si, xb) in enumerate(P):
                bux[c] = B_UX if idx == 0 else B_UY
                S.group("pe", mm_acc(bux[c], D + c * 128, n, slot), reads=WIN_R + XTR, writes=[("bank", bux[c])])
                if full:
                    S.op("act", lambda a: a.activation(out=ub[:, c, HIST:HIST + n], in_=bank[bux[c]][:, 0:n], func=AF.Copy),
                         reads=[("bank", bux[c])], writes=[("ub", c)])
                else:
                    S.op("dve", lambda v: v.tensor_copy(out=ub[:, c, HIST:HIST + n], in_=bank[bux[c]][:, 0:n]),
                         reads=[("bank", bux[c])], writes=[("ub", c)])
            for (c, si, xb) in P:
                S.group("pe", conv_mm(c, n, xb), reads=[("ub", c), ("dg", c), "cbhl", "ones2"], writes=[("bank", xb)])
                S.op("act", lambda a: a.activation(out=xcb[si][:, 0:n], in_=bank[xb][:, 0:n], func=AF.Copy),
                     reads=[("bank", xb)], writes=[("xcb", si)])
                hist_update(c, n)
            for (c, si, xb) in P:
                t = T[si]
                S.op("pe", lambda pe: pe.matmul(out=bank[B_R][:, 0:n], lhsT=wa[:, c, :], rhs=xcb[si][:, 0:n], start=True, stop=True),
                     reads=["wa", ("xcb", si)], writes=[("bank", B_R)])
                S.op("pe", lambda pe: pe.matmul(out=bank[B_I][:, 0:n], lhsT=wi[:, c, :], rhs=xcb[si][:, 0:n], start=True, stop=True),
                     reads=["wi", ("xcb", si)], writes=[("bank", B_I)])
                S.op("act", lambda a: a.activation(out=t["thr"][:, 0:n], in_=bank[B_R][:, 0:n], func=AF.Tanh, scale=0.5,
                                                   bias=hba[:, c:c + 1]), reads=[("bank", B_R), "hba"], writes=[("thr", si)])
                S.op("act", lambda a: a.activation(out=t["thi"][:, 0:n], in_=bank[B_I][:, 0:n], func=AF.Tanh, scale=0.5,
                                                   bias=hbi[:, c:c + 1]), reads=[("bank", B_I), "hbi"], writes=[("thi", si)])
            if full:
                for (c, si, xb) in P:
                    t = T[si]
                    S.op("act", lambda a: a.activation(out=t["a2"][:, 0:n], in_=t["thr"][:, 0:n], func=AF.Exp,
                                                       scale=sc[:, c:c + 1], bias=sc[:, c:c + 1]), reads=[("thr", si), "sc"], writes=[("a2", si)])
            for (c, si, xb) in P:
                t = T[si]
                S.op("act", lambda a: a.activation(out=t["thr"][:, 0:n], in_=t["thr"][:, 0:n], func=AF.Exp,
                                                   scale=sch[:, c:c + 1], bias=sch[:, c:c + 1]), reads=[("thr", si), "sch"], writes=[("thr", si)])
            if full:
                for (c, si, xb) in P:
                    S.group("pe", mm_acc(bux[c], c * 128, n, slot), reads=WIN_R + XTR, writes=[("bank", bux[c])])
                    S.op("act", lambda a: a.activation(out=T[si]["uys"][:, 0:n], in_=bank[bux[c]][:, 0:n], func=AF.Copy),
                         reads=[("bank", bux[c])], writes=[("uys", si)])
                for (c, si, xb) in P:
                    t = T[si]
                    S.op("act", lambda a: a.activation(out=t["g"][:, 0:n], in_=t["uys"][:, 0:n], func=AF.Square),
                         reads=[("uys", si)], writes=[("g", si)])

        def A_F2(P, n, slot, full):
            if not full:
                for (c, si, xb) in P:
                    t = T[si]
                    S.op("dve", lambda v: v.tensor_tensor(out=t["a2"][:, 0:n], in0=t["thr"][:, 0:n], in1=t["thr"][:, 0:n], op=ALU.mult),
                         reads=[("thr", si)], writes=[("a2", si)])
            for (c, si, xb) in P:
                t = T[si]
                S.op("dve", lambda v: v.scalar_tensor_tensor(out=t["thi"][:, 0:n], in0=t["thi"][:, 0:n], scalar=1.0,
                                                             in1=bank[xb][:, 0:n], op0=ALU.add, op1=ALU.mult),
                     reads=[("thi", si), ("bank", xb)], writes=[("thi", si)])
            if full:
                for (c, si, xb) in P:
                    t = T[si]
                    S.op("dve", lambda v: v.tensor_scalar(out=t["g"][:, 0:n], in0=t["g"][:, 0:n], scalar1=GC0 * GC1, scalar2=GC0,
                                                          op0=ALU.mult, op1=ALU.add), reads=[("g", si)], writes=[("g", si)])
                for (c, si, xb) in P:
                    t = T[si]
                    S.op("dve", lambda v: v.tensor_tensor(out=t["g"][:, 0:n], in0=t["g"][:, 0:n], in1=t["uys"][:, 0:n], op=ALU.mult),
                         reads=[("g", si), ("uys", si)], writes=[("g", si)])

        def A_BQ(P, n):
            for (c, si, xb) in P:
                t = T[si]
                S.op("act", lambda a: a.activation(out=t["a2"][:, 0:n], in_=t["a2"][:, 0:n], func=AF.Sqrt,
                                                   scale=-1.0, bias=1.0), reads=[("a2", si)], writes=[("a2", si)], dist=F)

        def A_B1(P, n):
            for (c, si, xb) in P:
                t = T[si]
                S.op("dve", lambda v: v.tensor_tensor(out=t["thi"][:, 0:n], in0=t["a2"][:, 0:n], in1=t["thi"][:, 0:n], op=ALU.mult),
                     reads=[("a2", si), ("thi", si)], writes=[("thi", si)], dist=F)
            for (c, si, xb) in P:
                t = T[si]
                S.op("dve", lambda v: v.tensor_tensor_scan(out=t["a2"][:, 0:n], data0=t["thr"][:, 0:n], data1=t["thi"][:, 0:n],
                                                           initial=state[:, c:c + 1], op0=ALU.mult, op1=ALU.add),
                     reads=[("thr", si), ("thi", si), ("state", c)], writes=[("a2", si)], dist=F)
            for (c, si, xb) in P:
                t = T[si]
                S.op("dve", lambda v: v.tensor_copy(out=state[:, c:c + 1], in_=t["a2"][:, n - 1:n]), reads=[("a2", si)], writes=[("state", c)], dist=F)

        def A_B2(P, n, slot):
            for (c, si, xb) in P:
                t = T[si]
                S.op("act", lambda a: a.activation(out=t["g"][:, 0:n], in_=t["g"][:, 0:n], func=AF.Tanh),
                     reads=[("g", si)], writes=[("g", si)], dist=F)
            for (c, si, xb) in P:
                t = T[si]
                S.op("dve", lambda v: v.scalar_tensor_tensor(out=t["g"][:, 0:n], in0=t["g"][:, 0:n], scalar=1.0,
                                                             in1=t["uys"][:, 0:n], op0=ALU.add, op1=ALU.mult),
                     reads=[("g", si), ("uys", si)], writes=[("g", si)], dist=F)
            for (c, si, xb) in P:
                t = T[si]
                S.op("dve", lambda v: v.scalar_tensor_tensor(out=zT[slot][:, c, 0:n], in0=t["a2"][:, 0:n], scalar=0.25,
                                                             in1=t["g"][:, 0:n], op0=ALU.mult, op1=ALU.mult),
                     reads=[("a2", si), ("g", si)], writes=[("zT", slot, c)], dist=F)

        def B_F1(P, n, slot):
            for (c, si, xb) in P:
                t = T[si]
                S.group("pe", mm_acc(B_CG, D + c * 128, n, slot), reads=WIN_R + [("xT", slot, t_) for t_ in range(4)], writes=[("bank", B_CG)])
                S.op("act", lambda a: a.activation(out=t["g"][:, 0:n], in_=bank[B_CG][:, 0:n], func=AF.Copy),
                     reads=[("bank", B_CG)], writes=[("g", si)], dist=F)
                S.group("pe", mm_acc(B_V, 2 * D + c * 128, n, slot), reads=WIN_R + [("xT", slot, t_) for t_ in range(4)], writes=[("bank", B_V)])
                S.op("dve", lambda v: v.tensor_tensor(out=ub[:, c, HIST:HIST + n], in0=t["g"][:, 0:n], in1=bank[B_V][:, 0:n], op=ALU.mult),
                     reads=[("g", si), ("bank", B_V)], writes=[("ub", c)], dist=F)
            for (c, si, xb) in P:
                t = T[si]
                S.group("pe", mm_acc(B_BG, c * 128, n, slot), reads=WIN_R + [("xT", slot, t_) for t_ in range(4)], writes=[("bank", B_BG)])
                S.op("act", lambda a: a.activation(out=t["thr"][:, 0:n], in_=bank[B_BG][:, 0:n], func=AF.Copy),
                     reads=[("bank", B_BG)], writes=[("thr", si)], dist=F)

        def B_F2(P, n, slot):
            for (c, si, xb) in P:
                S.group("pe", conv_mm(c, n, xb), reads=[("ub", c), ("dg", c)], writes=[("bank", xb)])
                hist_update(c, n)
            for (c, si, xb) in P:
                t = T[si]
                S.op("dve", lambda v: v.tensor_tensor(out=zT[slot][:, c, 0:n], in0=t["thr"][:, 0:n], in1=bank[xb][:, 0:n], op=ALU.mult),
                     reads=[("thr", si), ("bank", xb)], writes=[("zT", slot, c)], dist=F)

        ZT_R = lambda slot: [("zT", slot, c) for c in range(KC)]

        def mix_mm(ti, slot, off, half):
            tok0, n = TT[ti]
            if half == 0 and ti == 0:
                S.dma("sp", lambda g: g.dma_start(out=xres2[0][0:n, :], in_=src[tok0:tok0 + n, :]), writes=[("xres", 0)])
            S.group("pe", [lambda pe, k=k: pe.matmul(out=bank[B_MIX][0:n, :], lhsT=zT[slot][:, k, off:off + n],
                                                     rhs=wout[:, k, half * 512:(half + 1) * 512],
                                                     start=(k == 0), stop=(k == KC - 1)) for k in range(KC)],
                    reads=ZT_R(slot) + ["wout"], writes=[("bank", B_MIX)])

        def mix_res(ti, half):
            tok0, n = TT[ti]
            xres = xres2[ti % 3]
            S.op("dve", lambda v: v.scalar_tensor_tensor(
                out=xres[0:n, half * 512:(half + 1) * 512], in0=xres[0:n, half * 512:(half + 1) * 512], scalar=ALPHA,
                in1=bank[B_MIX][0:n, :], op0=ALU.mult, op1=ALU.add),
                reads=[("xres", ti % 3), ("bank", B_MIX)], writes=[("xres", ti % 3)])
            S.op("dve", lambda v: v.bn_stats(out=stats[0:n, half, :], in_=xres[0:n, half * 512:(half + 1) * 512]),
                 reads=[("xres", ti % 3)], writes=[("stats", half)])
            if half == 1:
                S.op("dve", lambda v: v.bn_aggr(out=mv[0:n, :], in_=stats[0:n].rearrange("p a b -> p (a b)")),
                     reads=[("stats", 0), ("stats", 1)], writes=["mv"])
                S.op("dve", lambda v: v.tensor_scalar_add(out=rstd[0:n, :], in0=mv[0:n, 1:2], scalar1=LN_EPS), reads=["mv"], writes=["rstd"])
                S.op("dve", lambda v: v.tensor_scalar_mul(out=nmr[0:n, :], in0=mv[0:n, 0:1], scalar1=-1.0), reads=["mv"], writes=["nmr"])
                S.op("pool", lambda v: v.tensor_tensor(out=rstd[0:n, :], in0=rstd[0:n, :], in1=c_mhalf[0:n, :], op=ALU.pow),
                     reads=["rstd", "c_mhalf"], writes=["rstd"])
                S.op("pool", lambda v: v.tensor_tensor(out=nmr[0:n, :], in0=nmr[0:n, :], in1=rstd[0:n, :], op=ALU.mult),
                     reads=["rstd", "nmr"], writes=["nmr"])

        def ln_out(ti):
            tok0, n = TT[ti]
            xres = xres2[ti % 3]
            S.op("act", lambda a: a.activation(out=h1[0:n, :], in_=xres[0:n, :], func=AF.Identity, scale=rstd[0:n, :], bias=nmr[0:n, :]),
                 reads=[("xres", ti % 3), "rstd", "nmr"], writes=["h1"])
            S.op("dve", lambda g: g.tensor_tensor(out=h1[0:n, :], in0=h1[0:n, :], in1=lng[0:n, :], op=ALU.mult),
                 reads=["h1", "lng"], writes=["h1"], dist=1)
            S.op("dve", lambda g: g.tensor_tensor(out=h1[0:n, :], in0=h1[0:n, :], in1=lnb[0:n, :], op=ALU.add),
                 reads=["h1", "lnb"], writes=["h1"])
            S.dma("sp", lambda g: g.dma_start(out=hA[tok0:tok0 + n, :], in_=h1[0:n, :]), reads=["h1"], writes=["hA"])
            S.dma("pool", lambda g: g.dma_start(out=hAb[tok0:tok0 + n, :], in_=h1[0:n, :]), reads=["h1"], writes=["hAb"])

        def rt_tr(ti, half):
            tok0, n = TT[ti]
            S.group("pe", [lambda pe, jj=jj: pe.transpose(out=tpb_f[:, jj * 128:jj * 128 + n],
                                                          in_=h1[0:n, (half * 4 + jj) * 128:(half * 4 + jj + 1) * 128],
                                                          identity=ident_f[0:n, 0:n]) for jj in range(4)],
                    reads=["h1", "ident_f"], writes=[("bank", 0)])

        def rt_cp(ti, half):
            tok0, n = TT[ti]
            S.op("act", lambda a: a.activation(
                out=h1T[:, half * 512:(half + 1) * 512].rearrange("p (j t) -> p j t", j=4)[:, :, 0:n],
                in_=tpb_f.rearrange("p (j t) -> p j t", j=4)[:, :, 0:n], func=AF.Copy),
                reads=[("bank", 0)], writes=[("h1T", half)])

        def rt_logits(ti, j, gp):
            tok0, n = TT[ti]
            lg = lg2[gp]
            S.group("pe", [lambda pe, k=k: pe.matmul(out=bank[LGB][0:n, 0:36], lhsT=h1T[:, k * 128:k * 128 + n], rhs=wrt[:, k, :],
                                                     start=(k == 0), stop=(k == KC - 1)) for k in range(KC)],
                    reads=[("h1T", 0), ("h1T", 1), "wrt"], writes=[("bank", LGB)])
            S.op("dve", lambda v: v.tensor_tensor(out=lg[0:n, j, :], in0=bank[LGB][0:n, 0:36], in1=brt[0:n, :], op=ALU.add),
                 reads=[("bank", LGB), "brt"], writes=[("lg", gp, j)])

        BG = []

        class _Deferred:
            def op(self, *a, **k):
                BG.append(("op", a, k))

            def group(self, *a, **k):
                BG.append(("group", a, k))

            def dma(self, *a, **k):
                BG.append(("dma", a, k))

        SD = _Deferred()

        def drain_bg(nmax):
            for _ in range(nmax):
                if not BG:
                    return
                kind_, a, k = BG.pop(0)
                getattr(S, kind_)(*a, **k)

        def router_part(tis, part, gp):
            T_ = len(tis); ti0 = tis[0]; n = TT[ti0][1]
            lg = lg2[gp]
            LG = [("lg", gp, j) for j in range(T_)]
            V = lambda fn, r, w: SD.op("dve", fn, reads=r, writes=w)
            bc = lambda ap, shape: ap.unsqueeze(2).to_broadcast(shape)
            lgv = lg[0:n, 0:T_, :]
            if part == 1:
                V(lambda v: v.reduce_max(out=gmx[0:n, 0:T_], in_=lgv[:, :, 0:4], axis=AX.X), LG, ["gmx"])
                V(lambda v: v.tensor_tensor(out=gsh[0:n, 0:T_, :], in0=lgv[:, :, 0:4], in1=bc(gmx[0:n, 0:T_], [n, T_, 4]), op=ALU.subtract), LG + ["gmx"], ["gsh"])
                SD.op("act", lambda a: a.activation(out=gex[0:n, 0:T_, :], in_=gsh[0:n, 0:T_, :], func=AF.Exp), reads=["gsh"], writes=["gex"])
                V(lambda v: v.tensor_scalar(out=goh[0:n, 0:T_, :], in0=gsh[0:n, 0:T_, :], scalar1=0.0, scalar2=None, op0=ALU.is_ge), ["gsh"], ["goh"])
                V(lambda v: v.tensor_scalar(out=goh[0:n, 0:T_, :], in0=goh[0:n, 0:T_, :], scalar1=BIG, scalar2=-BIG, op0=ALU.mult, op1=ALU.add), ["goh"], ["goh"])
                V(lambda v: v.tensor_tensor(out=elm[0:n, 0:T_, :].rearrange("p t (g e) -> p t g e", g=4),
                                            in0=lgv[:, :, 4:36].rearrange("p t (g e) -> p t g e", g=4),
                                            in1=goh[0:n, 0:T_, :].unsqueeze(3).to_broadcast([n, T_, 4, 8]), op=ALU.add), LG + ["goh"], ["elm"])
                V(lambda v: v.reduce_sum(out=gsum[0:n, 0:T_], in_=gex[0:n, 0:T_, :], axis=AX.X), ["gex"], ["gsum"])
                V(lambda v: v.reduce_max(out=m1[0:n, 0:T_], in_=elm[0:n, 0:T_, :], axis=AX.X), ["elm"], ["m1"])
                V(lambda v: v.reciprocal(out=pg[0:n, 0:T_], in_=gsum[0:n, 0:T_]), ["gsum"], ["pg"])
                V(lambda v: v.tensor_tensor(out=oh1[0:n, 0:T_, :], in0=elm[0:n, 0:T_, :], in1=bc(m1[0:n, 0:T_], [n, T_, NE]), op=ALU.is_ge), ["elm", "m1"], ["oh1"])
                V(lambda v: v.scalar_tensor_tensor(out=el2[0:n, 0:T_, :], in0=oh1[0:n, 0:T_, :], scalar=-BIG, in1=elm[0:n, 0:T_, :],
                                                   op0=ALU.mult, op1=ALU.add), ["oh1", "elm"], ["el2"])
                V(lambda v: v.reduce_max(out=m2[0:n, 0:T_], in_=el2[0:n, 0:T_, :], axis=AX.X), ["el2"], ["m2"])
                V(lambda v: v.tensor_tensor(out=oh2[0:n, 0:T_, :], in0=el2[0:n, 0:T_, :], in1=bc(m2[0:n, 0:T_], [n, T_, NE]), op=ALU.is_ge), ["el2", "m2"], ["oh2"])
                V(lambda v: v.tensor_tensor(out=ohb[0:n, 0:T_, :], in0=oh1[0:n, 0:T_, :], in1=oh2[0:n, 0:T_, :], op=ALU.add), ["oh1", "oh2"], ["ohb"])
                V(lambda v: v.tensor_sub(out=w1[0:n, 0:T_], in0=m1[0:n, 0:T_], in1=m2[0:n, 0:T_]), ["m1", "m2"], ["w1"])
                SD.op("act", lambda a: a.activation(out=w1[0:n, 0:T_], in_=w1[0:n, 0:T_], func=AF.Tanh, scale=0.5), reads=["w1"], writes=["w1"])
            if part == 2:
                fns = []
                for j in range(T_):
                    seq = [(tri_b, j)] + [(ones_b, i) for i in range(j)]
                    for q_, (lh, i) in enumerate(seq):
                        fns.append(lambda pe, lh=lh, i=i, j=j, q_=q_, L=len(seq): pe.matmul(
                            out=bank[LGB][:, 64 + j * NE:64 + (j + 1) * NE], lhsT=lh[:], rhs=ohb[:, i, :], start=(q_ == 0), stop=(q_ == L - 1)))
                for j in range(T_):
                    fns.append(lambda pe, j=j: pe.matmul(out=bank[LGB][:, 64 + T_ * NE:64 + (T_ + 1) * NE], lhsT=ones_b[:], rhs=ohb[:, j, :],
                                                         start=(j == 0), stop=(j == T_ - 1)))
                SD.group("pe", fns, reads=["tri_b", "ones_b", "ohb"] + LG, writes=[("bank", LGB)])
                V(lambda v: v.tensor_scalar(out=w1[0:n, 0:T_], in0=w1[0:n, 0:T_], scalar1=0.5, scalar2=0.5, op0=ALU.mult, op1=ALU.add), ["w1"], ["w1"])
                V(lambda v: v.tensor_mul(out=gate[0][0:n, ti0:ti0 + T_], in0=w1[0:n, 0:T_], in1=pg[0:n, 0:T_]), ["w1", "pg", ("gate", 0)], [("gate", 0)])
                V(lambda v: v.tensor_sub(out=gate[1][0:n, ti0:ti0 + T_], in0=pg[0:n, 0:T_], in1=gate[0][0:n, ti0:ti0 + T_]), ["pg", ("gate", 0), ("gate", 1)], [("gate", 1)])
                V(lambda v: v.tensor_tensor(out=rk[:, 0:T_, :], in0=bank[LGB][:, 64:64 + T_ * NE].rearrange("p (t e) -> p t e", e=NE),
                                            in1=tot[:].unsqueeze(1).to_broadcast([128, T_, NE]), op=ALU.add), [("bank", LGB), "tot"], ["rk"])
                V(lambda v: v.tensor_tensor(out=tot[:], in0=bank[LGB][:, 64 + T_ * NE:64 + (T_ + 1) * NE], in1=tot[:], op=ALU.add), [("bank", LGB), "tot", "rk"], ["tot"])
                V(lambda v: v.tensor_scalar(out=ov[0:n, 0:T_, :], in0=rk[0:n, 0:T_, :], scalar1=float(CAP), scalar2=BIG, op0=ALU.is_ge, op1=ALU.mult), ["rk"], ["ov"])
            for k, ohk in ((0, oh1), (1, oh2)):
                if (part == 2 and k == 1) or (part == 3 and k == 0) or part == 1:
                    continue
                kk = ["sel", "rkk", "ysf", "ovk", "eif", "pf", "bf", "bf2"]
                V(lambda v, ohk=ohk: v.tensor_mul(out=sel[0:n, 0:T_, :], in0=ohk[0:n, 0:T_, :], in1=rk[0:n, 0:T_, :]), ["oh1", "oh2", "rk"] + kk, ["sel"])
                V(lambda v: v.reduce_sum(out=rkk[0:n, 0:T_], in_=sel[0:n, 0:T_, :], axis=AX.X), ["sel"], ["rkk"])
                V(lambda v, ohk=ohk: v.tensor_mul(out=sel[0:n, 0:T_, :], in0=ohk[0:n, 0:T_, :], in1=ov[0:n, 0:T_, :]), ["oh1", "oh2", "ov", "sel"], ["sel"])
                V(lambda v: v.reduce_sum(out=ovk[0:n, 0:T_], in_=sel[0:n, 0:T_, :], axis=AX.X), ["sel"], ["ovk"])
                V(lambda v, ohk=ohk: v.tensor_mul(out=sel[0:n, 0:T_, :], in0=ohk[0:n, 0:T_, :], in1=ecrow4[0:n, 0:T_, :]), ["oh1", "oh2", "sel"] + EC4, ["sel"])
                V(lambda v: v.reduce_sum(out=eif[0:n, 0:T_], in_=sel[0:n, 0:T_, :], axis=AX.X), ["sel"], ["eif"])
                V(lambda v: v.tensor_add(out=ysf[0:n, 0:T_], in0=eif[0:n, 0:T_], in1=rkk[0:n, 0:T_]), ["eif", "rkk"], ["ysf"])
                V(lambda v: v.tensor_scalar(out=bf[0:n, 0:T_], in0=rkk[0:n, 0:T_], scalar1=128.0, scalar2=None, op0=ALU.is_ge), ["rkk"], ["bf"])
                V(lambda v: v.tensor_add(out=ysf[0:n, 0:T_], in0=ysf[0:n, 0:T_], in1=ovk[0:n, 0:T_]), ["ysf", "ovk"], ["ysf"])
                for m in range(2, NB):
                    V(lambda v, m=m: v.tensor_scalar(out=bf2[0:n, 0:T_], in0=rkk[0:n, 0:T_], scalar1=128.0 * m, scalar2=None, op0=ALU.is_ge), ["rkk"], ["bf2"])
                    V(lambda v: v.tensor_add(out=bf[0:n, 0:T_], in0=bf[0:n, 0:T_], in1=bf2[0:n, 0:T_]), ["bf", "bf2"], ["bf"])
                V(lambda v: v.tensor_scalar_min(out=ysf[0:n, 0:T_], in0=ysf[0:n, 0:T_], scalar1=float(NSLOT)), ["ysf"], ["ysf"])
                V(lambda v: v.scalar_tensor_tensor(out=pf[0:n, 0:T_], in0=bf[0:n, 0:T_], scalar=-128.0, in1=rkk[0:n, 0:T_],
                                                   op0=ALU.mult, op1=ALU.add), ["bf", "rkk"], ["pf"])
                V(lambda v, k=k: v.tensor_copy(out=ysrow[k][0:n, ti0:ti0 + T_], in_=ysf[0:n, 0:T_]), ["ysf", ("ysrow", k)], [("ysrow", k)])
                V(lambda v: v.scalar_tensor_tensor(out=pf[0:n, 0:T_], in0=pf[0:n, 0:T_], scalar=float(NE * NB), in1=bf[0:n, 0:T_],
                                                   op0=ALU.mult, op1=ALU.add), ["pf", "bf"], ["pf"])
                V(lambda v: v.scalar_tensor_tensor(out=pf[0:n, 0:T_], in0=eif[0:n, 0:T_], scalar=float(NB) / float(CAP), in1=pf[0:n, 0:T_],
                                                   op0=ALU.mult, op1=ALU.add), ["eif", "pf"], ["pf"])
                V(lambda v: v.tensor_add(out=pf[0:n, 0:T_], in0=pf[0:n, 0:T_], in1=ovk[0:n, 0:T_]), ["pf", "ovk"], ["pf"])
                V(lambda v, k=k: v.tensor_copy(out=dest[k][0:n, ti0:ti0 + T_], in_=pf[0:n, 0:T_]), ["pf", ("dest", k)], [("dest", k)])
                for j, ti in enumerate(tis):
                    SD.dma("pool", lambda g, k=k, ti=ti: g.indirect_dma_start(
                        out=stok[:, :], out_offset=bass.IndirectOffsetOnAxis(ap=dest[k][:, ti:ti + 1], axis=0),
                        in_=tokidx[:, ti, :], in_offset=None, bounds_check=bc_reg, oob_is_err=False),
                        reads=[("dest", k), "tokidx", "stok"], writes=[("stok_sc", ti, k)])

        gctr = [0]
        pending = []
        active = []
        groups = []
        stg = STG_Q

        def jobs_in(ph):
            return [jb for jb in active if jb["ph"] == ph]

        NBG = 8

        def hook_pe_early():
            drain_bg(NBG)
            for jb in jobs_in(1):
                mix_mm(jb["ti"], jb["slot"], jb["off"], 1)
            for jb in jobs_in(2):
                rt_tr(jb["ti"], 0)

        def hook_dve_mid():
            drain_bg(NBG)
            for jb in jobs_in(1):
                mix_res(jb["ti"], 1)
            for jb in jobs_in(2):
                rt_cp(jb["ti"], 0)

        def hook_pe_mid():
            drain_bg(NBG)
            for jb in jobs_in(0):
                mix_mm(jb["ti"], jb["slot"], jb["off"], 0)
            for jb in jobs_in(2):
                rt_tr(jb["ti"], 1)

        def hook_start():
            for jb in jobs_in(2):
                ln_out(jb["ti"])

        def hook_out():
            drain_bg(NBG)
            for jb in jobs_in(2):
                rt_cp(jb["ti"], 1)

        def hook_end():
            for g_ in list(groups):
                if all(jb["ph"] >= 3 for jb in g_["jobs"]):
                    for part in (1, 2, 3):
                        router_part(g_["tis"], part, g_["gp"])
                    groups.remove(g_)
                    for jb in g_["jobs"]:
                        active.remove(jb)
                    break
            for jb in jobs_in(0):
                mix_res(jb["ti"], 0)
                ti = jb["ti"]
                if ti + 1 < NTT:
                    t1_, n1 = TT[ti + 1]
                    S.dma("sp", lambda g: g.dma_start(out=xres2[(ti + 1) % 3][0:n1, :], in_=src[t1_:t1_ + n1, :]), writes=[("xres", (ti + 1) % 3)])
            for jb in jobs_in(2):
                rt_logits(jb["ti"], jb["j"], jb["gp"])
            drain_bg(NBG)

        def end_slot():
            for jb in active:
                if jb["ph"] < 3:
                    jb["ph"] += 1
            if pending:
                jb = pending.pop(0)
                jb["ph"] = 0
                active.append(jb)

        slot_ctr = [0]

        def stg_rate(k, full):
            slot_ctr[0] += 1
            if isA and not full:
                return 1
            if isA:
                return 2 + (slot_ctr[0] % 2)
            return 2

        tiles = []
        if isA:
            tiles += [(src_pre, tok0, n, None, False) for (tok0, n, _) in FMPRE]
        tiles += [(src, tok0, n, tts, True) for (tok0, n, tts) in FM]
        ntl = len(tiles)
        pairs = []
        for k in range(ntl):
            for pr in range(KC // 2):
                pairs.append((k, pr))

        def mkP(q):
            k, pr = pairs[q]
            base = 2 * (q % 2)
            return [(2 * pr, base, B_XC[0]), (2 * pr + 1, base + 1, B_XC[1])]

        def front1(q):
            k, pr = pairs[q]; n = tiles[k][2]; slot = k % 2
            if isA:
                A_F1(mkP(q), n, slot, tiles[k][4])
            else:
                B_F1(mkP(q), n, slot)

        load_dma(tiles[0][0], tiles[0][1], tiles[0][2]); load_tr(tiles[0][2], 0)
        if ntl > 1:
            load_dma(tiles[1][0], tiles[1][1], tiles[1][2])
        npairs = len(pairs)
        front1(0)
        if isA:
            A_F2(mkP(0), tiles[0][2], 0, tiles[0][4])
        for q in range(npairs):
            k, pr = pairs[q]; n = tiles[k][2]; slot = k % 2; full = tiles[k][4]
            P = mkP(q)
            if isA:
                A_BQ(P, n)
            hook_start()
            if q + 1 < npairs:
                front1(q + 1)
            hook_pe_early()
            if isA:
                A_B1(P, n)
            hook_dve_mid()
            if isA:
                if q + 1 < npairs:
                    k1, _ = pairs[q + 1]
                    A_F2(mkP(q + 1), tiles[k1][2], k1 % 2, tiles[k1][4])
            hook_pe_mid()
            if not isA:
                B_F2(P, n, slot)
            hook_out()
            if isA and full:
                A_B2(P, n, slot)
            if isA and (not full) and q + 1 < npairs and tiles[pairs[q + 1][0]][4]:
                S.op("dve", lambda v: v.tensor_scalar(out=state[:], in0=state[:], scalar1=flg[:, 0:1], scalar2=None, op0=ALU.mult),
                     reads=[("state", c) for c in range(KC)] + ["flg"], writes=[("state", c) for c in range(KC)])
            hook_end()
            if pr == 1 and k + 1 < ntl:
                load_tr(tiles[k + 1][2], (k + 1) % 2)
                if k + 2 < ntl:
                    load_dma(tiles[k + 2][0], tiles[k + 2][1], tiles[k + 2][2])
            for _ in range(stg_rate(k, full)):
                if stg:
                    stg.pop(0)()
            if pr == KC // 2 - 1 and tiles[k][3] is not None:
                tis = list(tiles[k][3])
                gctr[0] += 1
                g_ = dict(tis=tis, jobs=[], part=0, gp=gctr[0] % 2)
                for j, ti in enumerate(tis):
                    jb = dict(ti=ti, slot=slot, off=j * 128, j=j, ph=-1, gp=gctr[0] % 2)
                    g_["jobs"].append(jb); pending.append(jb)
                groups.append(g_)
            end_slot()
        while pending or active or BG:
            hook_start(); hook_pe_early(); hook_dve_mid(); hook_pe_mid(); hook_out(); hook_end()
            end_slot()
        while stg and STG_DONE[0] < STG_NEED[layer]:
            stg.pop(0)()
        S.barrier()
        st.close()

    def moe_phase(layer, final):
        st = contextlib.ExitStack()
        lng = sb(st, "m_lng", [128, D]); lnb = sb(st, "m_lnb", [128, D])
        S.dma("sp", lambda g: g.dma_start(out=lng[:], in_=lnp[layer * 2 + 1, 0]), writes=["lng"])
        S.dma("sp", lambda g: g.dma_start(out=lnb[:], in_=lnp[layer * 2 + 1, 1]), writes=["lnb"])
        idx = sb(st, "idx", [128, NE * NB, 2], I32)
        S.dma("sp", lambda g: g.dma_start(out=idx[:], in_=stok.rearrange("(p f) o -> p f o", p=128)), writes=["idx"])
        NWB = 3
        wg = [sb(st, "wg%d" % i, [128, KC, DEXP], BF16) for i in range(NWB)]
        wu = [sb(st, "wu%d" % i, [128, KC, DEXP], BF16) for i in range(NWB)]
        wd = [sb(st, "wd%d" % i, [128, 4, D], BF16) for i in range(NWB)]
        xs = [sb(st, "xs%d" % i, [128, NB, D], BF16) for i in range(NWB)]
        xsT = [sb(st, "xsT%d" % i, [128, KC, CAP], BF16) for i in range(2)]
        sg = [sb(st, "sg%d" % i, [128, CAP]) for i in range(2)]
        hbT = [sb(st, "hbT%d" % i, [128, 4, CAP], BF16) for i in range(2)]
        ysb = [sb(st, "ysb%d" % i, [128, D]) for i in range(2)]
        tpb = tpb_all

        def load_w(e):
            s = e % NWB
            if "g" in STAGED[layer]:
                S.dma("sp", lambda g: g.dma_start(out=wg[s][:], in_=wgbL[layer][e].rearrange("(k p) f -> p k f", p=128)), writes=[("wg", s)])
            else:
                S.dma("pool", lambda g: g.dma_start(out=wg[s][:], in_=wgate[layer, e].rearrange("(k p) f -> p k f", p=128)), writes=[("wg", s)])
            if "u" in STAGED[layer]:
                S.dma("sp", lambda g: g.dma_start(out=wu[s][:], in_=wubL[layer][e].rearrange("(k p) f -> p k f", p=128)), writes=[("wu", s)])
            else:
                S.dma("pool", lambda g: g.dma_start(out=wu[s][:], in_=wup[layer, e].rearrange("(k p) f -> p k f", p=128)), writes=[("wu", s)])
            if "d" in STAGED[layer]:
                S.dma("sp", lambda g: g.dma_start(out=wd[s][:], in_=wdbL[layer][e].rearrange("(j p) m -> p j m", p=128)), writes=[("wd", s)])
            else:
                S.dma("pool", lambda g: g.dma_start(out=wd[s][:], in_=wdown[layer, e].rearrange("(j p) m -> p j m", p=128)), writes=[("wd", s)])
            for b in range(NB):
                S.dma("pool", lambda g, b=b: g.indirect_dma_start(
                    out=xs[s][:, b, :], out_offset=None, in_=hAb[:, :],
                    in_offset=bass.IndirectOffsetOnAxis(ap=idx[:, e * NB + b, 0:1], axis=0)),
                    reads=["idx"], writes=[("xs", s, b)])

        def xs_transpose(e, b):
            s = e % 2
            sw = e % NWB
            tb_i = 0 if (e * NB + b) % 2 == 0 else 7
            tpx = tpb_all if tb_i == 0 else tpb_7
            S.group("pe", [lambda pe, j=j: pe.transpose(out=tpx[:, j * 128:(j + 1) * 128], in_=xs[sw][:, b, j * 128:(j + 1) * 128],
                                                        identity=ident_b[:]) for j in range(KC)],
                    reads=[("xs", sw, b), "ident_b"], writes=[("bank", tb_i)])
            if b % 2 == 0:
                S.op("act", lambda a: a.activation(out=xsT[s][:, :, b * 128:(b + 1) * 128],
                                                   in_=tpx.rearrange("p (k t) -> p k t", k=KC), func=AF.Copy),
                     reads=[("bank", tb_i)], writes=[("xsT", s, b)])
            else:
                S.op("dve", lambda v: v.tensor_copy(out=xsT[s][:, :, b * 128:(b + 1) * 128],
                                                    in_=tpx.rearrange("p (k t) -> p k t", k=KC)),
                     reads=[("bank", tb_i)], writes=[("xsT", s, b)])

        load_w(0)
        load_w(1)
        for b in range(NB):
            xs_transpose(0, b)
        ysi = 0
        for e in range(NE):
            s = e % 2
            sw = e % NWB
            if e + 2 < NE:
                load_w(e + 2)
            XR = [("xsT", s, b) for b in range(NB)]
            for j in range(4):
                gb, ubk = (1, 2) if j % 2 == 0 else (3, 4)
                S.group("pe", [lambda pe, k=k, j=j: pe.matmul(out=bank[gb][:, 0:CAP], lhsT=wg[sw][:, k, j * 128:(j + 1) * 128],
                                                              rhs=xsT[s][:, k, :], start=(k == 0), stop=(k == KC - 1)) for k in range(KC)],
                        reads=XR + [("wg", sw)], writes=[("bank", gb)])
                S.group("pe", [lambda pe, k=k, j=j: pe.matmul(out=bank[ubk][:, 0:CAP], lhsT=wu[sw][:, k, j * 128:(j + 1) * 128],
                                                              rhs=xsT[s][:, k, :], start=(k == 0), stop=(k == KC - 1)) for k in range(KC)],
                        reads=XR + [("wu", sw)], writes=[("bank", ubk)])
                S.op("act", lambda a, j=j: a.activation(out=sg[j % 2][:], in_=bank[gb][:, 0:CAP], func=AF.Silu),
                     reads=[("bank", gb)], writes=[("sg", j % 2)])
                S.op("dve", lambda v, j=j: v.tensor_tensor(out=hbT[s][:, j, :], in0=sg[j % 2][:], in1=bank[ubk][:, 0:CAP], op=ALU.mult),
                     reads=[("sg", j % 2), ("bank", ubk)], writes=[("hbT", s, j)])
                if j < NB and e + 1 < NE:
                    xs_transpose(e + 1, j)
            HR = [("hbT", s, j) for j in range(4)]
            for b in range(NB):
                yb = ysb[ysi % 2]
                for half in range(2):
                    S.group("pe", [lambda pe, j=j, b=b, half=half: pe.matmul(out=bank[5 + half][:, :], lhsT=hbT[s][:, j, b * 128:(b + 1) * 128],
                                                                             rhs=wd[sw][:, j, half * 512:(half + 1) * 512],
                                                                             start=(j == 0), stop=(j == 3)) for j in range(4)],
                            reads=HR + [("wd", sw)], writes=[("bank", 5 + half)])
                S.op("act", lambda a: a.activation(out=yb[:, 0:512], in_=bank[5][:, :], func=AF.Copy),
                     reads=[("bank", 5)], writes=[("ysb", ysi % 2, 0)])
                S.op("dve", lambda v: v.tensor_copy(out=yb[:, 512:1024], in_=bank[6][:, :]),
                     reads=[("bank", 6)], writes=[("ysb", ysi % 2, 1)])
                r0 = (e * NB + b) * 128
                S.dma("sp", lambda g, r0=r0, yb=yb: g.dma_start(out=ys[r0:r0 + 128, :], in_=yb[:]),
                      reads=[("ysb", ysi % 2, 0), ("ysb", ysi % 2, 1)], writes=[("ys", e, b)])
                ysi += 1
        S.barrier()
        ya = [sb(st, "ya%d" % i, [128, D]) for i in range(2)]
        ybb = [sb(st, "yb%d" % i, [128, D]) for i in range(2)]
        hr = [sb(st, "hr%d" % i, [128, D]) for i in range(2)]
        vvs = [sb(st, "m_vv%d" % i, [128, D]) for i in range(3)]
        ho = [sb(st, "m_ho%d" % i, [128, D]) for i in range(2)]
        stats2 = [sb(st, "m_stats%d" % i, [128, 2, 6]) for i in range(3)]
        mv2 = [sb(st, "m_mv%d" % i, [128, 2]) for i in range(3)]
        rstd2 = [sb(st, "m_rstd%d" % i, [128, 1]) for i in range(3)]
        nmr2 = [sb(st, "m_nmr%d" % i, [128, 1]) for i in range(3)]

        eps_t = sb(st, "eps_t", [128, 1])
        S.op("dve", lambda v: v.memset(eps_t[:], LN_EPS), writes=["eps_t"])

        def comb_load(ti):
            tok0, n = TT[ti]
            s = ti % 2
            S.dma("pool", lambda g: g.indirect_dma_start(out=ya[s][:], out_offset=None, in_=ys[:, :],
                                                         in_offset=bass.IndirectOffsetOnAxis(ap=ysrow[0][:, ti:ti + 1], axis=0)),
                  reads=[("ysrow", 0)], writes=[("ya", s)])
            S.dma("pool", lambda g: g.indirect_dma_start(out=ybb[s][:], out_offset=None, in_=ys[:, :],
                                                         in_offset=bass.IndirectOffsetOnAxis(ap=ysrow[1][:, ti:ti + 1], axis=0)),
                  reads=[("ysrow", 1)], writes=[("yb", s)])
            S.dma("sp", lambda g: g.dma_start(out=hr[s][0:n, :], in_=hA[tok0:tok0 + n, :]), writes=[("hr", s)])

        def comb_front(ti):
            tok0, n = TT[ti]
            s = ti % 2
            r = ti % 3
            v_ = vvs[r]; stats = stats2[r]; mv = mv2[r]; rstd = rstd2[r]; nmr = nmr2[r]
            S.op("act", lambda a: a.activation(out=v_[0:n, :], in_=ya[s][0:n, :], func=AF.Copy, scale=gate[0][0:n, ti:ti + 1]),
                 reads=[("ya", s), ("gate", 0)], writes=[("vv", r)])
            S.op("dve", lambda v: v.scalar_tensor_tensor(out=v_[0:n, :], in0=ybb[s][0:n, :], scalar=gate[1][0:n, ti:ti + 1], in1=v_[0:n, :],
                                                         op0=ALU.mult, op1=ALU.add), reads=[("yb", s), ("gate", 1), ("vv", r)], writes=[("vv", r)])
            S.op("dve", lambda v: v.scalar_tensor_tensor(out=v_[0:n, :], in0=hr[s][0:n, :], scalar=ALPHA, in1=v_[0:n, :],
                                                         op0=ALU.mult, op1=ALU.add), reads=[("hr", s), ("vv", r)], writes=[("vv", r)])
            for half in range(2):
                S.op("dve", lambda v, half=half: v.bn_stats(out=stats[0:n, half, :], in_=v_[0:n, half * 512:(half + 1) * 512]),
                     reads=[("vv", r)], writes=[("stats", r, half)])
            S.op("dve", lambda v: v.bn_aggr(out=mv[0:n, :], in_=stats[0:n].rearrange("p a b -> p (a b)")),
                 reads=[("stats", r, 0), ("stats", r, 1)], writes=[("mv", r)])
            S.op("act", lambda a: a.activation(out=rstd[0:n, :], in_=mv[0:n, 1:2], func=AF.Sqrt, bias=eps_t[0:n, :], scale=1.0),
                 reads=[("mv", r), "eps_t"], writes=[("rstd", r)])
            S.op("dve", lambda v: v.reciprocal(out=rstd[0:n, :], in_=rstd[0:n, :]), reads=[("rstd", r)], writes=[("rstd", r)])
            S.op("dve", lambda v: v.scalar_tensor_tensor(out=nmr[0:n, :], in0=mv[0:n, 0:1], scalar=-1.0, in1=rstd[0:n, :],
                                                         op0=ALU.mult, op1=ALU.mult), reads=[("mv", r), ("rstd", r)], writes=[("nmr", r)])

        def comb_tail(ti):
            tok0, n = TT[ti]
            s = ti % 2
            r = ti % 3
            v_ = vvs[r]; rstd = rstd2[r]; nmr = nmr2[r]
            S.op("act", lambda a: a.activation(out=v_[0:n, :], in_=v_[0:n, :], func=AF.Identity, scale=rstd[0:n, :], bias=nmr[0:n, :]),
                 reads=[("vv", r), ("rstd", r), ("nmr", r)], writes=[("vv", r)])
            S.op("dve", lambda g: g.tensor_tensor(out=v_[0:n, :], in0=v_[0:n, :], in1=lng[0:n, :], op=ALU.mult),
                 reads=[("vv", r), "lng"], writes=[("vv", r)])
            S.op("dve", lambda g: g.tensor_tensor(out=ho[s][0:n, :], in0=v_[0:n, :], in1=lnb[0:n, :], op=ALU.add),
                 reads=[("vv", r), "lnb"], writes=[("ho", s)])
            if final:
                if ti >= 1:
                    S.dma("sp", lambda g: g.dma_start(out=out[tok0 - NHEAD:tok0 - NHEAD + n, :], in_=ho[s][0:n, :]),
                          reads=[("ho", s)], writes=[("out", ti)])
            else:
                S.dma("sp", lambda g: g.dma_start(out=hB[tok0:tok0 + n, :], in_=ho[s][0:n, :]), reads=[("ho", s)], writes=[("hB", ti)])

        comb_load(0)
        comb_load(1)
        comb_front(0)
        for ti in range(NTT):
            if ti + 2 < NTT:
                comb_load(ti + 2)
            if ti + 1 < NTT:
                comb_front(ti + 1)
            comb_tail(ti)
        S.barrier()
        st.close()

    S.barrier()
    for ph in phases:
        if ph == "A":
            mixer_phase("A", 0, xin, xpre)
        elif ph == "B":
            mixer_phase("B", 1, hB, None)
        elif ph == "M0":
            moe_phase(0, False)
        elif ph == "M1":
            moe_phase(1, True)
    S.barrier()
    top.close()
    return nc


def _host_inputs(inp):
    f = lambda a: np.ascontiguousarray(np.asarray(a, dtype=np.float32))
    x = f(inp["x"]); meta = f(inp["meta_tokens"])

    def pvec(v):
        return np.ascontiguousarray(f(v).reshape(KC, 128).T)

    cw = f(inp["lru_conv_w"])[0]
    a_vec = np.stack([pvec(cw[0]), pvec(cw[1]), pvec(cw[2]), pvec(cw[3]), pvec(inp["lru_conv_b"][0]),
                      pvec(inp["lru_b_a"][0]), pvec(inp["lru_b_i"][0]), pvec(inp["lru_lambda"][0])], axis=1).reshape(128, 8 * KC)
    cwb = f(inp["sc_conv_w"])[0]
    b_vec = np.stack([pvec(cwb[0]), pvec(cwb[1]), pvec(cwb[2])], axis=1).reshape(128, 3 * KC)

    def blockdiag(w):
        w = f(w)
        o = np.zeros((128, KC, 128), np.float32)
        for c in range(KC):
            o[0:64, c, 0:64] = w[2 * c]
            o[64:128, c, 64:128] = w[2 * c + 1]
        return o

    lnp = np.zeros((4, 2, 128, D), np.float32)
    g = f(inp["ln_g"]); b = f(inp["ln_b"])
    for l in range(2):
        for j in range(2):
            lnp[l * 2 + j, 0] = g[l, j][None, :]
            lnp[l * 2 + j, 1] = b[l, j][None, :]
    wr = np.concatenate([f(inp["moe_w_group"]), f(inp["moe_w_expert"])], axis=2)
    brv = np.concatenate([f(inp["moe_b_group"]), f(inp["moe_b_expert"]).reshape(2, 32)], axis=1)
    br = np.ascontiguousarray(np.broadcast_to(brv[:, None, :], (2, 128, 36)))
    cst = np.zeros((128, 128 * 4 + 64), np.float32)
    cst[:, 0:128] = np.eye(128, dtype=np.float32)
    cst[:, 128:256] = np.triu(np.ones((128, 128), np.float32), k=1)
    cst[:, 256:384] = 1.0
    cst[:, 512:512 + NE] = (np.arange(NE, dtype=np.float32) * CAP)[None, :]
    csti = np.zeros((128, NTT, 2), np.int32)
    for ti, (t0, n) in enumerate(TT):
        csti[:, ti, 0] = t0 + np.arange(128)
        csti[:, ti, 1] = t0 + np.arange(128)
    csti = csti.reshape(128, NTT * 2)
    cbrow = np.ascontiguousarray(np.broadcast_to(f(inp["lru_conv_b"])[0][None, :], (2, D)))
    shared = dict(a_cbrow=cbrow, a_win=f(inp["lru_w_in"])[0], a_wout=f(inp["lru_w_out"])[0], a_vec=a_vec,
                  a_wa=blockdiag(inp["lru_w_a"][0]), a_wi=blockdiag(inp["lru_w_i"][0]),
                  b_win=f(inp["sc_w_in"])[0], b_wout=f(inp["sc_w_out"])[0], b_vec=b_vec, lnp=lnp, wr=wr, br=br,
                  wgate=f(inp["moe_w_gate"]), wup=f(inp["moe_w_up"]), wdown=f(inp["moe_w_down"]), cst=cst, csti=csti)
    maps = []
    for c in range(8):
        bb, half = c // 2, c % 2
        if half == 0:
            xin = np.concatenate([meta, x[bb, 0:NMAIN]], axis=0)
            xpre = np.zeros((NPRE, D), np.float32)
            flag = np.zeros((128, 1), np.float32)
        else:
            xin = x[bb, NMAIN - NHEAD:2 * NMAIN]
            xpre = np.concatenate([meta, x[bb, 0:NMAIN - NHEAD]], axis=0)
            flag = np.ones((128, 1), np.float32)
        m = dict(shared)
        m.update(xin=np.ascontiguousarray(xin), xpre=np.ascontiguousarray(xpre), flag=flag)
        maps.append(m)
    return maps


_NC_CACHE = {}


def kernel(**inputs):
    maps = _host_inputs(inputs)
    if "nc" not in _NC_CACHE:
        _NC_CACHE["nc"] = build_program()
    nc = _NC_CACHE["nc"]
    res = run_bass_kernel_spmd(nc, maps, core_ids=list(range(8)))
    outs = [np.asarray(r["out"], dtype=np.float32) for r in res.results]
    full = np.zeros((4, 2 * NMAIN, D), np.float32)
    for c in range(8):
        bb, half = c // 2, c % 2
        full[bb, half * NMAIN:(half + 1) * NMAIN] = outs[c]
    return full
```

```python
import numpy as np
import concourse.bass as bass
import concourse.mybir as mybir
from concourse.bass_utils import run_bass_kernel_spmd

F32 = mybir.dt.float32
BF16 = mybir.dt.bfloat16
I32 = mybir.dt.int32
AF = mybir.ActivationFunctionType
ALU = mybir.AluOpType
AX = mybir.AxisListType

D = 1024
KC = 8
NHEAD = 16
NMAIN = 4096
NT = NHEAD + NMAIN
NPRE = 4096
NE = 32
CAP = 384
NB = CAP // 128
NSLOT = NE * CAP
DEXP = 512
ALPHA = (2.0 * 2) ** 0.25
LN_EPS = 1e-5
GC0 = 0.7978845608028654
GC1 = 0.044715
BIG = 1.0e9

TT = [(0, NHEAD)] + [(NHEAD + 128 * i, 128) for i in range(NMAIN // 128)]
NTT = len(TT)
FM = [(0, NHEAD, [0])] + [(NHEAD + 512 * k, 512, [1 + 4 * k + j for j in range(4)]) for k in range(NMAIN // 512)]
FMPRE = [(512 * k, 512, None) for k in range(NPRE // 512)]


STRICT_SAME_ENGINE = True


class Sched:
    def __init__(self, nc, n_dma_sems=20):
        self.nc = nc
        self.eng = {"pe": nc.tensor, "act": nc.scalar, "dve": nc.vector, "pool": nc.gpsimd, "sp": nc.sync}
        self.sem = {k: nc.alloc_semaphore("s_" + k) for k in ("pe", "act", "dve", "pool")}
        self.cnt = {k: 0 for k in self.sem}
        self.dsem = {q: [nc.alloc_semaphore("d_%s_%d" % (q, i)) for i in range(n_dma_sems)] for q in ("sp", "pool")}
        self.dsem["stg"] = [nc.alloc_semaphore("d_stg_%d" % i) for i in range(10)]
        self.dcnt = {q: [0] * len(self.dsem[q]) for q in self.dsem}
        self.drr = {q: 0 for q in self.dsem}
        self.waited = {k: {} for k in self.eng}
        self.bufs = {}
        self.semobj = {}
        for k, s in self.sem.items():
            self.semobj[id(s)] = s
        self.all_events = {}

    def _wait(self, e, ev):
        s, v = ev
        w = self.waited[e]
        if w.get(id(s), 0) >= v:
            return
        self.eng[e].wait_ge(s, v)
        w[id(s)] = v

    def _deps(self, e, reads, writes, dist=3):
        evs = []
        for k in reads:
            b = self.bufs.get(k)
            if b and b["w"]:
                evs.append(b["w"])
        for k in writes:
            b = self.bufs.get(k)
            if b:
                if b["w"]:
                    evs.append(b["w"])
                evs.extend(b["r"])
        for ev in evs:
            s, v = ev
            if e in self.sem and s is self.sem[e]:
                if e == "pe" or (not STRICT_SAME_ENGINE and self.cnt[e] - v >= dist):
                    continue
            self._wait(e, ev)

    def _record(self, ev, reads, writes):
        for k in reads:
            self.bufs.setdefault(k, {"w": None, "r": []})["r"].append(ev)
        for k in writes:
            self.bufs[k] = {"w": ev, "r": []}
        self.all_events[id(ev[0])] = ev

    def op(self, e, fn, reads=(), writes=(), dist=3):
        self._deps(e, reads, writes, dist)
        ins = fn(self.eng[e])
        ins.then_inc(self.sem[e], 1)
        self.cnt[e] += 1
        ev = (self.sem[e], self.cnt[e])
        self._record(ev, reads, writes)
        return ev

    def group(self, e, fns, reads=(), writes=()):
        self._deps(e, reads, writes)
        ins = None
        for fn in fns:
            ins = fn(self.eng[e])
        ins.then_inc(self.sem[e], 1)
        self.cnt[e] += 1
        ev = (self.sem[e], self.cnt[e])
        self._record(ev, reads, writes)
        return ev

    def dma(self, q, fn, reads=(), writes=(), sems=None):
        self._deps(q, reads, writes)
        sq = sems or q
        i = self.drr[sq]
        self.drr[sq] = (i + 1) % len(self.dsem[sq])
        s = self.dsem[sq][i]
        if self.dcnt[sq][i] > 0:
            self._wait(q, (s, 16 * self.dcnt[sq][i]))
        ins = fn(self.eng[q])
        ins.then_inc(s, 16)
        self.dcnt[sq][i] += 1
        ev = (s, 16 * self.dcnt[sq][i])
        self._record(ev, reads, writes)
        return ev

    def barrier(self):
        for e in self.eng:
            for ev in list(self.all_events.values()):
                self._wait(e, ev)
        self.bufs = {}


def build_program(phases=("A", "M0", "B", "M1"), debug_out=False):
    nc = bass.Bass("TRN2", target_bir_lowering=False)
    S = Sched(nc)

    def din(name, shape, dt=F32):
        return nc.dram_tensor(name, list(shape), dt, kind="ExternalInput").ap()

    scratch_kind = "ExternalOutput" if debug_out else "Internal"

    def dsc(name, shape, dt=F32):
        return nc.dram_tensor(name, list(shape), dt, kind=scratch_kind).ap()

    xin = din("xin", [NT, D])
    xpre = din("xpre", [NPRE, D])
    flag = din("flag", [128, 1])
    a_win = din("a_win", [D, 2 * D])
    a_wout = din("a_wout", [D, D])
    a_vec = din("a_vec", [128, 8 * KC])
    a_cbrow = din("a_cbrow", [2, D])
    a_wa = din("a_wa", [128, KC, 128])
    a_wi = din("a_wi", [128, KC, 128])
    b_win = din("b_win", [D, 3 * D])
    b_wout = din("b_wout", [D, D])
    b_vec = din("b_vec", [128, 3 * KC])
    lnp = din("lnp", [4, 2, 128, D])
    wr = din("wr", [2, D, 36])
    br = din("br", [2, 128, 36])
    wgate = din("wgate", [2, NE, D, DEXP])
    wup = din("wup", [2, NE, D, DEXP])
    wdown = din("wdown", [2, NE, DEXP, D])
    cst = din("cst", [128, 128 * 4 + 64])
    csti = din("csti", [128, NTT * 2], I32)
    out = nc.dram_tensor("out", [NMAIN, D], F32, kind="ExternalOutput").ap()

    hA = dsc("hA", [NT, D])
    hAb = dsc("hAb", [NT + 1, D], BF16)
    hB = dsc("hB", [NT, D])
    stok = dsc("stok", [128 * NE * NB, 2], I32)
    ys = dsc("ys", [NSLOT + 1, D])
    wgbL = [nc.dram_tensor("wgb%d" % l, [NE, D, DEXP], BF16, kind="Internal").ap() for l in range(2)]
    wubL = [nc.dram_tensor("wub%d" % l, [NE, D, DEXP], BF16, kind="Internal").ap() for l in range(2)]
    wdbL = [nc.dram_tensor("wdb%d" % l, [NE, DEXP, D], BF16, kind="Internal").ap() for l in range(2)]

    STAGED = {0: ("g", "u", "d"), 1: ("g", "u", "d")}

    def staging_list(layer):
        lst = []
        wgb, wub, wdb = wgbL[layer], wubL[layer], wdbL[layer]
        for e in range(NE):
            if "g" in STAGED[layer]:
                lst.append(lambda e=e: S.dma("pool", lambda g: g.dma_start(out=wgb[e], in_=wgate[layer, e]), writes=[("wgb", e)], sems="stg"))
            if "u" in STAGED[layer]:
                lst.append(lambda e=e: S.dma("pool", lambda g: g.dma_start(out=wub[e], in_=wup[layer, e]), writes=[("wub", e)], sems="stg"))
            if "d" in STAGED[layer]:
                lst.append(lambda e=e: S.dma("pool", lambda g: g.dma_start(out=wdb[e], in_=wdown[layer, e]), writes=[("wdb", e)], sems="stg"))
        return lst

    import contextlib
    top = contextlib.ExitStack()

    uniq = [0]

    def sb(stack, name, shape, dt=F32):
        uniq[0] += 1
        return stack.enter_context(nc.sbuf_tensor("%s_%d" % (name, uniq[0]), list(shape), dt))

    def ps(stack, name, shape, dt=F32):
        return stack.enter_context(nc.psum_tensor(name, list(shape), dt))

    ident_b = sb(top, "ident_b", [128, 128], BF16)
    ident_f = sb(top, "ident_f", [128, 128])
    tri_b = sb(top, "tri_b", [128, 128], BF16)
    ones_b = sb(top, "ones_b", [128, 128], BF16)
    ecrow = sb(top, "ecrow", [128, NE])
    tokidx = sb(top, "tokidx", [128, NTT, 2], I32)
    dest = [sb(top, "dest%d" % k, [128, NTT], I32) for k in range(2)]
    ysrow = [sb(top, "ysrow%d" % k, [128, NTT], I32) for k in range(2)]
    gate = [sb(top, "gate%d" % k, [128, NTT]) for k in range(2)]
    c_mhalf = sb(top, "c_mhalf", [128, 1])
    zrow = sb(top, "zrow", [1, D], BF16)
    zrowf = sb(top, "zrowf", [1, D])
    bank = [ps(top, "bank%d" % i, [128, 512]) for i in range(8)]
    tpb_f = bank[0]
    tpb_all = bank[0][:].bitcast(BF16)
    tpb_7 = bank[7][:].bitcast(BF16)

    bc_reg = nc.gpsimd.alloc_register("bc_reg")
    nc.gpsimd.reg_mov(bc_reg, 128 * NE * NB - 1)
    S.dma("pool", lambda g: g.dma_start(out=ident_b[:], in_=cst[:, 0:128]), writes=["ident_b"])
    S.dma("sp", lambda g: g.dma_start(out=ident_f[:], in_=cst[:, 0:128]), writes=["ident_f"])
    S.dma("pool", lambda g: g.dma_start(out=tri_b[:], in_=cst[:, 128:256]), writes=["tri_b"])
    S.dma("pool", lambda g: g.dma_start(out=ones_b[:], in_=cst[:, 256:384]), writes=["ones_b"])
    S.dma("sp", lambda g: g.dma_start(out=ecrow[:], in_=cst[:, 512:512 + NE]), writes=["ecrow"])
    S.dma("sp", lambda g: g.dma_start(out=tokidx[:], in_=csti.rearrange("p (t o) -> p t o", o=2)), writes=["tokidx"])
    S.op("dve", lambda v: v.memset(c_mhalf[:], -0.5), writes=["c_mhalf"])
    S.op("dve", lambda v: v.memset(zrow[:], 0.0), writes=["zrow"])
    S.op("dve", lambda v: v.memset(zrowf[:], 0.0), writes=["zrowf"])
    S.dma("sp", lambda g: g.dma_start(out=hAb[NT:NT + 1, :], in_=zrow[:]), reads=["zrow"], writes=["hAb_z"])
    S.dma("sp", lambda g: g.dma_start(out=ys[NSLOT:NSLOT + 1, :], in_=zrowf[:]), reads=["zrowf"], writes=["ys_z"])

    STG_DONE = [0]
    STG_NEED = {0: 3 * NE, 1: 6 * NE}
    STG_Q = []
    for l_ in range(2):
        for fn_ in staging_list(l_):
            STG_Q.append(lambda fn_=fn_: (fn_(), STG_DONE.__setitem__(0, STG_DONE[0] + 1)))

    def mixer_phase(kind, layer, src, src_pre):
        st = contextlib.ExitStack()
        isA = (kind == "A")
        ncol = 2 if isA else 3
        win_d = a_win if isA else b_win
        wout_d = a_wout if isA else b_wout
        win = sb(st, "win", [128, KC, ncol * D], BF16)
        wout = sb(st, "wout", [128, KC, D], BF16)
        lng = sb(st, "lng", [128, D])
        lnb = sb(st, "lnb", [128, D])
        wrt = sb(st, "wrt", [128, KC, 36])
        brt = sb(st, "brt", [128, 36])
        ecrow4 = sb(st, "ecrow4", [128, 4, NE])
        for k in range(KC):
            S.dma("pool", lambda g, k=k: g.dma_start(out=win[:, k, :], in_=win_d[k * 128:(k + 1) * 128, :]),
                  writes=[("win", k)])
        S.dma("pool", lambda g: g.dma_start(out=wout[:], in_=wout_d.rearrange("(k p) m -> p k m", p=128)),
              writes=["wout"])
        S.dma("sp", lambda g: g.dma_start(out=lng[:], in_=lnp[layer * 2, 0]), writes=["lng"])
        S.dma("sp", lambda g: g.dma_start(out=lnb[:], in_=lnp[layer * 2, 1]), writes=["lnb"])
        S.dma("sp", lambda g: g.dma_start(out=wrt[:], in_=wr[layer].rearrange("(k p) n -> p k n", p=128)),
              writes=["wrt"])
        S.dma("sp", lambda g: g.dma_start(out=brt[:], in_=br[layer]), writes=["brt"])
        for j in range(4):
            S.dma("sp", lambda g, j=j: g.dma_start(out=ecrow4[:, j, :], in_=cst[:, 512:512 + NE]), writes=[("ecrow4", j)])
        EC4 = [("ecrow4", j) for j in range(4)]
        WIN_R = [("win", k) for k in range(KC)]

        if isA:
            HIST, NK = 3, 4
            vec = sb(st, "vec", [128, 8, KC])
            wa = sb(st, "wa", [128, KC, 128], BF16)
            wi = sb(st, "wi", [128, KC, 128], BF16)
            S.dma("sp", lambda g: g.dma_start(out=vec[:], in_=a_vec.rearrange("p (v c) -> p v c", v=8)), writes=["vec"])
            S.dma("pool", lambda g: g.dma_start(out=wa[:], in_=a_wa[:, :, :]), writes=["wa"])
            S.dma("pool", lambda g: g.dma_start(out=wi[:], in_=a_wi[:, :, :]), writes=["wi"])
            flg = sb(st, "flg", [128, 1])
            S.dma("sp", lambda g: g.dma_start(out=flg[:], in_=flag[:, :]), writes=["flg"])
            sc = sb(st, "sc", [128, KC]); sch = sb(st, "sch", [128, KC])
            hba = sb(st, "hba", [128, KC]); hbi = sb(st, "hbi", [128, KC])
            t1 = sb(st, "spt1", [128, KC]); t2 = sb(st, "spt2", [128, KC]); t3 = sb(st, "spt3", [128, KC])
            t4 = sb(st, "spt4", [128, KC])
            lam = vec[:, 7, :]
            S.op("dve", lambda v: v.tensor_scalar_mul(out=t1[:], in0=lam, scalar1=-1.0), reads=["vec"], writes=["t1"])
            S.op("dve", lambda v: v.tensor_max(out=t1[:], in0=t1[:], in1=lam), reads=["vec", "t1"], writes=["t1"])
            S.op("act", lambda a: a.activation(out=t2[:], in_=t1[:], func=AF.Exp, scale=-1.0), reads=["t1"], writes=["t2"])
            S.op("dve", lambda v: v.tensor_scalar_add(out=t3[:], in0=t2[:], scalar1=2.0), reads=["t2"], writes=["t3"])
            S.op("dve", lambda v: v.reciprocal(out=t3[:], in_=t3[:]), reads=["t3"], writes=["t3"])
            S.op("dve", lambda v: v.tensor_mul(out=t2[:], in0=t2[:], in1=t3[:]), reads=["t2", "t3"], writes=["t2"])
            S.op("dve", lambda v: v.tensor_mul(out=t3[:], in0=t2[:], in1=t2[:]), reads=["t2"], writes=["t3"])
            S.op("dve", lambda v: v.memset(t4[:], 1.0 / 17.0), writes=["t4"])
            for nn in (15, 13, 11, 9, 7, 5, 3, 1):
                S.op("dve", lambda v: v.tensor_mul(out=t4[:], in0=t4[:], in1=t3[:]), reads=["t4", "t3"], writes=["t4"])
                S.op("dve", lambda v, nn=nn: v.tensor_scalar_add(out=t4[:], in0=t4[:], scalar1=1.0 / nn), reads=["t4"], writes=["t4"])
            S.op("dve", lambda v: v.tensor_mul(out=t4[:], in0=t4[:], in1=t2[:]), reads=["t4", "t2"], writes=["t4"])
            S.op("dve", lambda v: v.tensor_scalar(out=t1[:], in0=lam, scalar1=-1.0, scalar2=0.0, op0=ALU.mult, op1=ALU.max),
                 reads=["vec", "t1"], writes=["t1"])
            S.op("dve", lambda v: v.scalar_tensor_tensor(out=t1[:], in0=t4[:], scalar=2.0, in1=t1[:], op0=ALU.mult, op1=ALU.add),
                 reads=["t4", "t1"], writes=["t1"])
            S.op("dve", lambda v: v.tensor_scalar_mul(out=sc[:], in0=t1[:], scalar1=-8.0), reads=["t1"], writes=["sc"])
            S.op("dve", lambda v: v.tensor_scalar_mul(out=sch[:], in0=t1[:], scalar1=-4.0), reads=["t1"], writes=["sch"])
            S.op("dve", lambda v: v.tensor_scalar_mul(out=hba[:], in0=vec[:, 5, :], scalar1=0.5), reads=["vec"], writes=["hba"])
            S.op("dve", lambda v: v.tensor_scalar_mul(out=hbi[:], in0=vec[:, 6, :], scalar1=0.5), reads=["vec"], writes=["hbi"])
            state = sb(st, "state", [128, KC])
            S.op("dve", lambda v: v.memset(state[:], 0.0), writes=[("state", c) for c in range(KC)])
            cbhl = sb(st, "cbhl", [2, D], BF16); ones2 = sb(st, "ones2", [2, 512], BF16)
            st_tmp = contextlib.ExitStack()
            cb2 = sb(st_tmp, "cb2", [2, D]); cbh = sb(st_tmp, "cbh", [2, D], BF16); cbhf = sb(st_tmp, "cbhf", [2, D])
            S.dma("sp", lambda g: g.dma_start(out=cb2[:], in_=a_cbrow[:, :]), writes=["cb2"])
            S.op("dve", lambda v: v.tensor_copy(out=cbh[:], in_=cb2[:]), reads=["cb2"], writes=["cbh"])
            S.op("dve", lambda v: v.tensor_copy(out=cbhf[:], in_=cbh[:]), reads=["cbh"], writes=["cbhf"])
            S.op("dve", lambda v: v.tensor_sub(out=cb2[:], in0=cb2[:], in1=cbhf[:]), reads=["cb2", "cbhf"], writes=["cb2"])
            S.op("dve", lambda v: v.tensor_scalar(out=cbhf[:], in0=cbhf[:], scalar1=ident_f[0:2, 0:1], scalar2=None, op0=ALU.mult),
                 reads=["cbhf", "ident_f"], writes=["cbhf"])
            S.op("dve", lambda v: v.scalar_tensor_tensor(out=cbhl[:], in0=cb2[:], scalar=ident_f[0:2, 1:2], in1=cbhf[:],
                                                         op0=ALU.mult, op1=ALU.add), reads=["cb2", "cbhf", "ident_f"], writes=["cbhl"])
            S.op("dve", lambda v: v.memset(ones2[:], 1.0), writes=["ones2"])
            S.barrier()
            st_tmp.close()
        else:
            HIST, NK = 2, 3
            vec = sb(st, "vec", [128, 3, KC])
            S.dma("sp", lambda g: g.dma_start(out=vec[:], in_=b_vec.rearrange("p (v c) -> p v c", v=3)), writes=["vec"])
        ub = sb(st, "ub", [128, KC, HIST + 512], BF16)
        S.op("dve", lambda v: v.memset(ub[:], 0.0), writes=[("ub", c) for c in range(KC)])
        dg = sb(st, "dg", [128, KC, NK, 128], BF16)
        for c in range(KC):
            for k in range(NK):
                S.op("dve", lambda v, c=c, k=k: v.tensor_scalar(out=dg[:, c, k, :], in0=ident_f[:], scalar1=vec[:, k, c:c + 1], scalar2=None,
                                                                op0=ALU.mult), reads=["vec", "ident_f"], writes=[("dg", c)])

        xtok = sb(st, "xtok", [128, 4, D], BF16)
        xT = [sb(st, "xT%d" % i, [128, KC, 512], BF16) for i in range(2)]
        zT = [sb(st, "zT%d" % i, [128, KC, 512], BF16) for i in range(2)]
        NS = 4
        names = ("thr", "a2", "thi", "g", "uys") if isA else ("g", "thr")
        T = [{nm: sb(st, "t_%s%d" % (nm, i), [128, 512]) for nm in names} for i in range(NS)]
        xcb = [sb(st, "xcb%d" % i, [128, 512], BF16) for i in range(NS)] if isA else None
        if isA:
            B_TP, B_UX, B_UY, B_XC, B_R, B_I, B_MIX = 0, 1, 2, (3, 4), 5, 6, 7
            LGB = 2
        else:
            B_TP, B_CG, B_V, B_BG, B_XC, B_MIX = 0, 1, 2, 3, (4, 5), 7
            LGB = 6
        xres2 = [sb(st, "xres%d" % i, [128, D]) for i in range(3)]
        h1 = sb(st, "h1", [128, D]); h1T = sb(st, "h1T", [128, D])
        stats = sb(st, "stats", [128, 2, 6]); mv = sb(st, "mv", [128, 2]); rstd = sb(st, "rstd", [128, 1])
        nmr = sb(st, "nmr", [128, 1])
        lg2 = [sb(st, "lg%d" % i, [128, 4, 36]) for i in range(2)]; gmx = sb(st, "gmx", [128, 4]); gsh = sb(st, "gsh", [128, 4, 4])
        gex = sb(st, "gex", [128, 4, 4]); gsum = sb(st, "gsum", [128, 4]); pg = sb(st, "pg", [128, 4])
        goh = sb(st, "goh", [128, 4, 4]); elm = sb(st, "elm", [128, 4, NE]); el2 = sb(st, "el2", [128, 4, NE])
        m1 = sb(st, "m1", [128, 4]); m2 = sb(st, "m2", [128, 4]); oh1 = sb(st, "oh1", [128, 4, NE]); oh2 = sb(st, "oh2", [128, 4, NE])
        ohb = sb(st, "ohb", [128, 4, NE], BF16); w1 = sb(st, "w1", [128, 4])
        tot = sb(st, "tot", [128, NE]); rk = sb(st, "rk", [128, 4, NE]); ov = sb(st, "ov", [128, 4, NE])
        sel = sb(st, "sel", [128, 4, NE]); rkk = sb(st, "rkk", [128, 4]); ysf = sb(st, "ysf", [128, 4])
        pf = sb(st, "pf", [128, 4]); bf = sb(st, "bf", [128, 4]); bf2 = sb(st, "bf2", [128, 4])
        eif = sb(st, "eif", [128, 4]); ovk = sb(st, "ovk", [128, 4])
        S.op("dve", lambda v: v.memset(tot[:], 0.0), writes=["tot"])
        S.op("dve", lambda v: v.memset(ohb[:], 0.0), writes=["ohb"])
        for k in range(2):
            S.op("dve", lambda v, k=k: v.memset(dest[k][:], 1 << 24), writes=[("dest", k)])
            S.op("dve", lambda v, k=k: v.memset(ysrow[k][:], NSLOT), writes=[("ysrow", k)])
            S.op("dve", lambda v, k=k: v.memset(gate[k][:], 0.0), writes=[("gate", k)])
        stinit = sb(st, "stinit", [128, NE * NB * 2], I32)
        S.op("dve", lambda v: v.memset(stinit[:], NT), writes=["stinit"])
        S.dma("sp", lambda g: g.dma_start(out=stok.rearrange("(p f) o -> p (f o)", p=128), in_=stinit[:]),
              reads=["stinit"], writes=["stok"])

        def load_dma(dsrc, tok0, n):
            nt = (n + 127) // 128
            if n >= 128:
                S.dma("pool", lambda g: g.dma_start(out=xtok[:, 0:nt, :],
                                                    in_=dsrc[tok0:tok0 + n, :].rearrange("(t p) d -> p t d", p=128)),
                      writes=["xtok"])
            else:
                S.dma("pool", lambda g: g.dma_start(out=xtok[0:n, 0, :], in_=dsrc[tok0:tok0 + n, :]), writes=["xtok"])

        def load_tr(n, slot):
            nt = (n + 127) // 128
            rows = min(n, 128)
            for t in range(nt):
                bk_ = 0 if t % 2 == 0 else 7
                tpx = tpb_all if bk_ == 0 else tpb_7
                S.group("pe", [lambda pe, t=t, j=j: pe.transpose(out=tpx[:, j * 128:j * 128 + rows],
                                                                 in_=xtok[0:rows, t, j * 128:(j + 1) * 128],
                                                                 identity=ident_b[0:rows, 0:rows]) for j in range(KC)],
                        reads=["xtok", "ident_b"], writes=[("bank", bk_)])
                if t % 2 == 0:
                    S.op("act", lambda a, t=t: a.activation(out=xT[slot][:, :, t * 128:t * 128 + rows],
                                                            in_=tpx.rearrange("p (k t) -> p k t", k=KC)[:, :, 0:rows], func=AF.Copy),
                         reads=[("bank", bk_)], writes=[("xT", slot, t)])
                else:
                    S.op("dve", lambda v, t=t: v.tensor_copy(out=xT[slot][:, :, t * 128:t * 128 + rows],
                                                             in_=tpx.rearrange("p (k t) -> p k t", k=KC)[:, :, 0:rows]),
                         reads=[("bank", bk_)], writes=[("xT", slot, t)])

        def mm_acc(bk, col0, n, slot):
            return [lambda pe, k=k: pe.matmul(out=bank[bk][:, 0:n], lhsT=win[:, k, col0:col0 + 128], rhs=xT[slot][:, k, 0:n],
                                              start=(k == 0), stop=(k == KC - 1)) for k in range(KC)]

        def conv_mm(c, n, bk):
            fns = [lambda pe, k=k: pe.matmul(out=bank[bk][:, 0:n], lhsT=dg[:, c, k, :], rhs=ub[:, c, k:k + n],
                                             start=(k == 0), stop=(k == NK - 1 and not isA)) for k in range(NK)]
            if isA:
                fns.append(lambda pe: pe.matmul(out=bank[bk][:, 0:n], lhsT=cbhl[0:2, c * 128:(c + 1) * 128], rhs=ones2[0:2, 0:n],
                                                start=False, stop=True))
            return fns

        def hist_update(c, n):
            S.op("pool", lambda g: g.tensor_copy(out=ub[:, c, 0:HIST], in_=ub[:, c, n:n + HIST]),
                 reads=[("ub", c)], writes=[("ub", c)])

        F = 1

        def A_F1(P, n, slot, full):
            XTR = [("xT", slot, t_) for t_ in range(4)]
            bux = {}
            for idx, (c, si, xb) in enumerate(P):
                bux[c] = B_UX if idx == 0 else B_UY
                S.group("pe", mm_acc(bux[c], D + c * 128, n, slot), reads=WIN_R + XTR, writes=[("bank", bux[c])])
                if full:
                    S.op("act", lambda a: a.activation(out=ub[:, c, HIST:HIST + n], in_=bank[bux[c]][:, 0:n], func=AF.Copy),
                         reads=[("bank", bux[c])], writes=[("ub", c)])
                else:
                    S.op("dve", lambda v: v.tensor_copy(out=ub[:, c, HIST:HIST + n], in_=bank[bux[c]][:, 0:n]),
                         reads=[("bank", bux[c])], writes=[("ub", c)])
            for (c, si, xb) in P:
                S.group("pe", conv_mm(c, n, xb), reads=[("ub", c), ("dg", c), "cbhl", "ones2"], writes=[("bank", xb)])
                S.op("act", lambda a: a.activation(out=xcb[si][:, 0:n], in_=bank[xb][:, 0:n], func=AF.Copy),
                     reads=[("bank", xb)], writes=[("xcb", si)])
                hist_update(c, n)
            for (c, si, xb) in P:
                t = T[si]
                S.op("pe", lambda pe: pe.matmul(out=bank[B_R][:, 0:n], lhsT=wa[:, c, :], rhs=xcb[si][:, 0:n], start=True, stop=True),
                     reads=["wa", ("xcb", si)], writes=[("bank", B_R)])
                S.op("pe", lambda pe: pe.matmul(out=bank[B_I][:, 0:n], lhsT=wi[:, c, :], rhs=xcb[si][:, 0:n], start=True, stop=True),
                     reads=["wi", ("xcb", si)], writes=[("bank", B_I)])
                S.op("act", lambda a: a.activation(out=t["thr"][:, 0:n], in_=bank[B_R][:, 0:n], func=AF.Tanh, scale=0.5,
                                                   bias=hba[:, c:c + 1]), reads=[("bank", B_R), "hba"], writes=[("thr", si)])
                S.op("act", lambda a: a.activation(out=t["thi"][:, 0:n], in_=bank[B_I][:, 0:n], func=AF.Tanh, scale=0.5,
                                                   bias=hbi[:, c:c + 1]), reads=[("bank", B_I), "hbi"], writes=[("thi", si)])
            if full:
                for (c, si, xb) in P:
                    t = T[si]
                    S.op("act", lambda a: a.activation(out=t["a2"][:, 0:n], in_=t["thr"][:, 0:n], func=AF.Exp,
                                                       scale=sc[:, c:c + 1], bias=sc[:, c:c + 1]), reads=[("thr", si), "sc"], writes=[("a2", si)])
            for (c, si, xb) in P:
                t = T[si]
                S.op("act", lambda a: a.activation(out=t["thr"][:, 0:n], in_=t["thr"][:, 0:n], func=AF.Exp,
                                                   scale=sch[:, c:c + 1], bias=sch[:, c:c + 1]), reads=[("thr", si), "sch"], writes=[("thr", si)])
            if full:
                for (c, si, xb) in P:
                    S.group("pe", mm_acc(bux[c], c * 128, n, slot), reads=WIN_R + XTR, writes=[("bank", bux[c])])
                    S.op("act", lambda a: a.activation(out=T[si]["uys"][:, 0:n], in_=bank[bux[c]][:, 0:n], func=AF.Copy),
                         reads=[("bank", bux[c])], writes=[("uys", si)])
                for (c, si, xb) in P:
                    t = T[si]
                    S.op("act", lambda a: a.activation(out=t["g"][:, 0:n], in_=t["uys"][:, 0:n], func=AF.Square),
                         reads=[("uys", si)], writes=[("g", si)])

        def A_F2(P, n, slot, full):
            if not full:
                for (c, si, xb) in P:
                    t = T[si]
                    S.op("dve", lambda v: v.tensor_tensor(out=t["a2"][:, 0:n], in0=t["thr"][:, 0:n], in1=t["thr"][:, 0:n], op=ALU.mult),
                         reads=[("thr", si)], writes=[("a2", si)])
            for (c, si, xb) in P:
                t = T[si]
                S.op("dve", lambda v: v.scalar_tensor_tensor(out=t["thi"][:, 0:n], in0=t["thi"][:, 0:n], scalar=1.0,
                                                             in1=bank[xb][:, 0:n], op0=ALU.add, op1=ALU.mult),
                     reads=[("thi", si), ("bank", xb)], writes=[("thi", si)])
            if full:
                for (c, si, xb) in P:
                    t = T[si]
                    S.op("dve", lambda v: v.tensor_scalar(out=t["g"][:, 0:n], in0=t["g"][:, 0:n], scalar1=GC0 * GC1, scalar2=GC0,
                                                          op0=ALU.mult, op1=ALU.add), reads=[("g", si)], writes=[("g", si)])
                for (c, si, xb) in P:
                    t = T[si]
                    S.op("dve", lambda v: v.tensor_tensor(out=t["g"][:, 0:n], in0=t["g"][:, 0:n], in1=t["uys"][:, 0:n], op=ALU.mult),
                         reads=[("g", si), ("uys", si)], writes=[("g", si)])

        def A_BQ(P, n):
            for (c, si, xb) in P:
                t = T[si]
                S.op("act", lambda a: a.activation(out=t["a2"][:, 0:n], in_=t["a2"][:, 0:n], func=AF.Sqrt,
                                                   scale=-1.0, bias=1.0), reads=[("a2", si)], writes=[("a2", si)], dist=F)

        def A_B1(P, n):
            for (c, si, xb) in P:
                t = T[si]
                S.op("dve", lambda v: v.tensor_tensor(out=t["thi"][:, 0:n], in0=t["a2"][:, 0:n], in1=t["thi"][:, 0:n], op=ALU.mult),
                     reads=[("a2", si), ("thi", si)], writes=[("thi", si)], dist=F)
            for (c, si, xb) in P:
                t = T[si]
                S.op("dve", lambda v: v.tensor_tensor_scan(out=t["a2"][:, 0:n], data0=t["thr"][:, 0:n], data1=t["thi"][:, 0:n],
                                                           initial=state[:, c:c + 1], op0=ALU.mult, op1=ALU.add),
                     reads=[("thr", si), ("thi", si), ("state", c)], writes=[("a2", si)], dist=F)
            for (c, si, xb) in P:
                t = T[si]
                S.op("dve", lambda v: v.tensor_copy(out=state[:, c:c + 1], in_=t["a2"][:, n - 1:n]), reads=[("a2", si)], writes=[("state", c)], dist=F)

        def A_B2(P, n, slot):
            for (c, si, xb) in P:
                t = T[si]
                S.op("act", lambda a: a.activation(out=t["g"][:, 0:n], in_=t["g"][:, 0:n], func=AF.Tanh),
                     reads=[("g", si)], writes=[("g", si)], dist=F)
            for (c, si, xb) in P:
                t = T[si]
                S.op("dve", lambda v: v.scalar_tensor_tensor(out=t["g"][:, 0:n], in0=t["g"][:, 0:n], scalar=1.0,
                                                             in1=t["uys"][:, 0:n], op0=ALU.add, op1=ALU.mult),
                     reads=[("g", si), ("uys", si)], writes=[("g", si)], dist=F)
            for (c, si, xb) in P:
                t = T[si]
                S.op("dve", lambda v: v.scalar_tensor_tensor(out=zT[slot][:, c, 0:n], in0=t["a2"][:, 0:n], scalar=0.25,
                                                             in1=t["g"][:, 0:n], op0=ALU.mult, op1=ALU.mult),
                     reads=[("a2", si), ("g", si)], writes=[("zT", slot, c)], dist=F)

        def B_F1(P, n, slot):
            for (c, si, xb) in P:
                t = T[si]
                S.group("pe", mm_acc(B_CG, D + c * 128, n, slot), reads=WIN_R + [("xT", slot, t_) for t_ in range(4)], writes=[("bank", B_CG)])
                S.op("act", lambda a: a.activation(out=t["g"][:, 0:n], in_=bank[B_CG][:, 0:n], func=AF.Copy),
                     reads=[("bank", B_CG)], writes=[("g", si)], dist=F)
                S.group("pe", mm_acc(B_V, 2 * D + c * 128, n, slot), reads=WIN_R + [("xT", slot, t_) for t_ in range(4)], writes=[("bank", B_V)])
                S.op("dve", lambda v: v.tensor_tensor(out=ub[:, c, HIST:HIST + n], in0=t["g"][:, 0:n], in1=bank[B_V][:, 0:n], op=ALU.mult),
                     reads=[("g", si), ("bank", B_V)], writes=[("ub", c)], dist=F)
            for (c, si, xb) in P:
                t = T[si]
                S.group("pe", mm_acc(B_BG, c * 128, n, slot), reads=WIN_R + [("xT", slot, t_) for t_ in range(4)], writes=[("bank", B_BG)])
                S.op("act", lambda a: a.activation(out=t["thr"][:, 0:n], in_=bank[B_BG][:, 0:n], func=AF.Copy),
                     reads=[("bank", B_BG)], writes=[("thr", si)], dist=F)

        def B_F2(P, n, slot):
            for (c, si, xb) in P:
                S.group("pe", conv_mm(c, n, xb), reads=[("ub", c), ("dg", c)], writes=[("bank", xb)])
                hist_update(c, n)
            for (c, si, xb) in P:
                t = T[si]
                S.op("dve", lambda v: v.tensor_tensor(out=zT[slot][:, c, 0:n], in0=t["thr"][:, 0:n], in1=bank[xb][:, 0:n], op=ALU.mult),
                     reads=[("thr", si), ("bank", xb)], writes=[("zT", slot, c)], dist=F)

        ZT_R = lambda slot: [("zT", slot, c) for c in range(KC)]

        def mix_mm(ti, slot, off, half):
            tok0, n = TT[ti]
            if half == 0 and ti == 0:
                S.dma("sp", lambda g: g.dma_start(out=xres2[0][0:n, :], in_=src[tok0:tok0 + n, :]), writes=[("xres", 0)])
            S.group("pe", [lambda pe, k=k: pe.matmul(out=bank[B_MIX][0:n, :], lhsT=zT[slot][:, k, off:off + n],
                                                     rhs=wout[:, k, half * 512:(half + 1) * 512],
                                                     start=(k == 0), stop=(k == KC - 1)) for k in range(KC)],
                    reads=ZT_R(slot) + ["wout"], writes=[("bank", B_MIX)])

        def mix_res(ti, half):
            tok0, n = TT[ti]
            xres = xres2[ti % 3]
            S.op("dve", lambda v: v.scalar_tensor_tensor(
                out=xres[0:n, half * 512:(half + 1) * 512], in0=xres[0:n, half * 512:(half + 1) * 512], scalar=ALPHA,
                in1=bank[B_MIX][0:n, :], op0=ALU.mult, op1=ALU.add),
                reads=[("xres", ti % 3), ("bank", B_MIX)], writes=[("xres", ti % 3)])
            S.op("dve", lambda v: v.bn_stats(out=stats[0:n, half, :], in_=xres[0:n, half * 512:(half + 1) * 512]),
                 reads=[("xres", ti % 3)], writes=[("stats", half)])
            if half == 1:
                S.op("dve", lambda v: v.bn_aggr(out=mv[0:n, :], in_=stats[0:n].rearrange("p a b -> p (a b)")),
                     reads=[("stats", 0), ("stats", 1)], writes=["mv"])
                S.op("dve", lambda v: v.tensor_scalar_add(out=rstd[0:n, :], in0=mv[0:n, 1:2], scalar1=LN_EPS), reads=["mv"], writes=["rstd"])
                S.op("dve", lambda v: v.tensor_scalar_mul(out=nmr[0:n, :], in0=mv[0:n, 0:1], scalar1=-1.0), reads=["mv"], writes=["nmr"])
                S.op("pool", lambda v: v.tensor_tensor(out=rstd[0:n, :], in0=rstd[0:n, :], in1=c_mhalf[0:n, :], op=ALU.pow),
                     reads=["rstd", "c_mhalf"], writes=["rstd"])
                S.op("pool", lambda v: v.tensor_tensor(out=nmr[0:n, :], in0=nmr[0:n, :], in1=rstd[0:n, :], op=ALU.mult),
                     reads=["rstd", "nmr"], writes=["nmr"])

        def ln_out(ti):
            tok0, n = TT[ti]
            xres = xres2[ti % 3]
            S.op("act", lambda a: a.activation(out=h1[0:n, :], in_=xres[0:n, :], func=AF.Identity, scale=rstd[0:n, :], bias=nmr[0:n, :]),
                 reads=[("xres", ti % 3), "rstd", "nmr"], writes=["h1"])
            S.op("dve", lambda g: g.tensor_tensor(out=h1[0:n, :], in0=h1[0:n, :], in1=lng[0:n, :], op=ALU.mult),
                 reads=["h1", "lng"], writes=["h1"], dist=1)
            S.op("dve", lambda g: g.tensor_tensor(out=h1[0:n, :], in0=h1[0:n, :], in1=lnb[0:n, :], op=ALU.add),
                 reads=["h1", "lnb"], writes=["h1"])
            S.dma("sp", lambda g: g.dma_start(out=hA[tok0:tok0 + n, :], in_=h1[0:n, :]), reads=["h1"], writes=["hA"])
            S.dma("pool", lambda g: g.dma_start(out=hAb[tok0:tok0 + n, :], in_=h1[0:n, :]), reads=["h1"], writes=["hAb"])

        def rt_tr(ti, half):
            tok0, n = TT[ti]
            S.group("pe", [lambda pe, jj=jj: pe.transpose(out=tpb_f[:, jj * 128:jj * 128 + n],
                                                          in_=h1[0:n, (half * 4 + jj) * 128:(half * 4 + jj + 1) * 128],
                                                          identity=ident_f[0:n, 0:n]) for jj in range(4)],
                    reads=["h1", "ident_f"], writes=[("bank", 0)])

        def rt_cp(ti, half):
            tok0, n = TT[ti]
            S.op("act", lambda a: a.activation(
                out=h1T[:, half * 512:(half + 1) * 512].rearrange("p (j t) -> p j t", j=4)[:, :, 0:n],
                in_=tpb_f.rearrange("p (j t) -> p j t", j=4)[:, :, 0:n], func=AF.Copy),
                reads=[("bank", 0)], writes=[("h1T", half)])

        def rt_logits(ti, j, gp):
            tok0, n = TT[ti]
            lg = lg2[gp]
            S.group("pe", [lambda pe, k=k: pe.matmul(out=bank[LGB][0:n, 0:36], lhsT=h1T[:, k * 128:k * 128 + n], rhs=wrt[:, k, :],
                                                     start=(k == 0), stop=(k == KC - 1)) for k in range(KC)],
                    reads=[("h1T", 0), ("h1T", 1), "wrt"], writes=[("bank", LGB)])
            S.op("dve", lambda v: v.tensor_tensor(out=lg[0:n, j, :], in0=bank[LGB][0:n, 0:36], in1=brt[0:n, :], op=ALU.add),
                 reads=[("bank", LGB), "brt"], writes=[("lg", gp, j)])

        BG = []

        class _Deferred:
            def op(self, *a, **k):
                BG.append(("op", a, k))

            def group(self, *a, **k):
                BG.append(("group", a, k))

            def dma(self, *a, **k):
                BG.append(("dma", a, k))

        SD = _Deferred()

        def drain_bg(nmax):
            for _ in range(nmax):
                if not BG:
                    return
                kind_, a, k = BG.pop(0)
                getattr(S, kind_)(*a, **k)

        def router_part(tis, part, gp):
            T_ = len(tis); ti0 = tis[0]; n = TT[ti0][1]
            lg = lg2[gp]
            LG = [("lg", gp, j) for j in range(T_)]
            V = lambda fn, r, w: SD.op("dve", fn, reads=r, writes=w)
            bc = lambda ap, shape: ap.unsqueeze(2).to_broadcast(shape)
            lgv = lg[0:n, 0:T_, :]
            if part == 1:
                V(lambda v: v.reduce_max(out=gmx[0:n, 0:T_], in_=lgv[:, :, 0:4], axis=AX.X), LG, ["gmx"])
                V(lambda v: v.tensor_tensor(out=gsh[0:n, 0:T_, :], in0=lgv[:, :, 0:4], in1=bc(gmx[0:n, 0:T_], [n, T_, 4]), op=ALU.subtract), LG + ["gmx"], ["gsh"])
                SD.op("act", lambda a: a.activation(out=gex[0:n, 0:T_, :], in_=gsh[0:n, 0:T_, :], func=AF.Exp), reads=["gsh"], writes=["gex"])
                V(lambda v: v.tensor_scalar(out=goh[0:n, 0:T_, :], in0=gsh[0:n, 0:T_, :], scalar1=0.0, scalar2=None, op0=ALU.is_ge), ["gsh"], ["goh"])
                V(lambda v: v.tensor_scalar(out=goh[0:n, 0:T_, :], in0=goh[0:n, 0:T_, :], scalar1=BIG, scalar2=-BIG, op0=ALU.mult, op1=ALU.add), ["goh"], ["goh"])
                V(lambda v: v.tensor_tensor(out=elm[0:n, 0:T_, :].rearrange("p t (g e) -> p t g e", g=4),
                                            in0=lgv[:, :, 4:36].rearrange("p t (g e) -> p t g e", g=4),
                                            in1=goh[0:n, 0:T_, :].unsqueeze(3).to_broadcast([n, T_, 4, 8]), op=ALU.add), LG + ["goh"], ["elm"])
                V(lambda v: v.reduce_sum(out=gsum[0:n, 0:T_], in_=gex[0:n, 0:T_, :], axis=AX.X), ["gex"], ["gsum"])
                V(lambda v: v.reduce_max(out=m1[0:n, 0:T_], in_=elm[0:n, 0:T_, :], axis=AX.X), ["elm"], ["m1"])
                V(lambda v: v.reciprocal(out=pg[0:n, 0:T_], in_=gsum[0:n, 0:T_]), ["gsum"], ["pg"])
                V(lambda v: v.tensor_tensor(out=oh1[0:n, 0:T_, :], in0=elm[0:n, 0:T_, :], in1=bc(m1[0:n, 0:T_], [n, T_, NE]), op=ALU.is_ge), ["elm", "m1"], ["oh1"])
                V(lambda v: v.scalar_tensor_tensor(out=el2[0:n, 0:T_, :], in0=oh1[0:n, 0:T_, :], scalar=-BIG, in1=elm[0:n, 0:T_, :],
                                                   op0=ALU.mult, op1=ALU.add), ["oh1", "elm"], ["el2"])
                V(lambda v: v.reduce_max(out=m2[0:n, 0:T_], in_=el2[0:n, 0:T_, :], axis=AX.X), ["el2"], ["m2"])
                V(lambda v: v.tensor_tensor(out=oh2[0:n, 0:T_, :], in0=el2[0:n, 0:T_, :], in1=bc(m2[0:n, 0:T_], [n, T_, NE]), op=ALU.is_ge), ["el2", "m2"], ["oh2"])
                V(lambda v: v.tensor_tensor(out=ohb[0:n, 0:T_, :], in0=oh1[0:n, 0:T_, :], in1=oh2[0:n, 0:T_, :], op=ALU.add), ["oh1", "oh2"], ["ohb"])
                V(lambda v: v.tensor_sub(out=w1[0:n, 0:T_], in0=m1[0:n, 0:T_], in1=m2[0:n, 0:T_]), ["m1", "m2"], ["w1"])
                SD.op("act", lambda a: a.activation(out=w1[0:n, 0:T_], in_=w1[0:n, 0:T_], func=AF.Tanh, scale=0.5), reads=["w1"], writes=["w1"])
            if part == 2:
                fns = []
                for j in range(T_):
                    seq = [(tri_b, j)] + [(ones_b, i) for i in range(j)]
                    for q_, (lh, i) in enumerate(seq):
                        fns.append(lambda pe, lh=lh, i=i, j=j, q_=q_, L=len(seq): pe.matmul(
                            out=bank[LGB][:, 64 + j * NE:64 + (j + 1) * NE], lhsT=lh[:], rhs=ohb[:, i, :], start=(q_ == 0), stop=(q_ == L - 1)))
                for j in range(T_):
                    fns.append(lambda pe, j=j: pe.matmul(out=bank[LGB][:, 64 + T_ * NE:64 + (T_ + 1) * NE], lhsT=ones_b[:], rhs=ohb[:, j, :],
                                                         start=(j == 0), stop=(j == T_ - 1)))
                SD.group("pe", fns, reads=["tri_b", "ones_b", "ohb"] + LG, writes=[("bank", LGB)])
                V(lambda v: v.tensor_scalar(out=w1[0:n, 0:T_], in0=w1[0:n, 0:T_], scalar1=0.5, scalar2=0.5, op0=ALU.mult, op1=ALU.add), ["w1"], ["w1"])
                V(lambda v: v.tensor_mul(out=gate[0][0:n, ti0:ti0 + T_], in0=w1[0:n, 0:T_], in1=pg[0:n, 0:T_]), ["w1", "pg", ("gate", 0)], [("gate", 0)])
                V(lambda v: v.tensor_sub(out=gate[1][0:n, ti0:ti0 + T_], in0=pg[0:n, 0:T_], in1=gate[0][0:n, ti0:ti0 + T_]), ["pg", ("gate", 0), ("gate", 1)], [("gate", 1)])
                V(lambda v: v.tensor_tensor(out=rk[:, 0:T_, :], in0=bank[LGB][:, 64:64 + T_ * NE].rearrange("p (t e) -> p t e", e=NE),
                                            in1=tot[:].unsqueeze(1).to_broadcast([128, T_, NE]), op=ALU.add), [("bank", LGB), "tot"], ["rk"])
                V(lambda v: v.tensor_tensor(out=tot[:], in0=bank[LGB][:, 64 + T_ * NE:64 + (T_ + 1) * NE], in1=tot[:], op=ALU.add), [("bank", LGB), "tot", "rk"], ["tot"])
                V(lambda v: v.tensor_scalar(out=ov[0:n, 0:T_, :], in0=rk[0:n, 0:T_, :], scalar1=float(CAP), scalar2=BIG, op0=ALU.is_ge, op1=ALU.mult), ["rk"], ["ov"])
            for k, ohk in ((0, oh1), (1, oh2)):
                if (part == 2 and k == 1) or (part == 3 and k == 0) or part == 1:
                    continue
                kk = ["sel", "rkk", "ysf", "ovk", "eif", "pf", "bf", "bf2"]
                V(lambda v, ohk=ohk: v.tensor_mul(out=sel[0:n, 0:T_, :], in0=ohk[0:n, 0:T_, :], in1=rk[0:n, 0:T_, :]), ["oh1", "oh2", "rk"] + kk, ["sel"])
                V(lambda v: v.reduce_sum(out=rkk[0:n, 0:T_], in_=sel[0:n, 0:T_, :], axis=AX.X), ["sel"], ["rkk"])
                V(lambda v, ohk=ohk: v.tensor_mul(out=sel[0:n, 0:T_, :], in0=ohk[0:n, 0:T_, :], in1=ov[0:n, 0:T_, :]), ["oh1", "oh2", "ov", "sel"], ["sel"])
                V(lambda v: v.reduce_sum(out=ovk[0:n, 0:T_], in_=sel[0:n, 0:T_, :], axis=AX.X), ["sel"], ["ovk"])
                V(lambda v, ohk=ohk: v.tensor_mul(out=sel[0:n, 0:T_, :], in0=ohk[0:n, 0:T_, :], in1=ecrow4[0:n, 0:T_, :]), ["oh1", "oh2", "sel"] + EC4, ["sel"])
                V(lambda v: v.reduce_sum(out=eif[0:n, 0:T_], in_=sel[0:n, 0:T_, :], axis=AX.X), ["sel"], ["eif"])
                V(lambda v: v.tensor_add(out=ysf[0:n, 0:T_], in0=eif[0:n, 0:T_], in1=rkk[0:n, 0:T_]), ["eif", "rkk"], ["ysf"])
                V(lambda v: v.tensor_scalar(out=bf[0:n, 0:T_], in0=rkk[0:n, 0:T_], scalar1=128.0, scalar2=None, op0=ALU.is_ge), ["rkk"], ["bf"])
                V(lambda v: v.tensor_add(out=ysf[0:n, 0:T_], in0=ysf[0:n, 0:T_], in1=ovk[0:n, 0:T_]), ["ysf", "ovk"], ["ysf"])
                for m in range(2, NB):
                    V(lambda v, m=m: v.tensor_scalar(out=bf2[0:n, 0:T_], in0=rkk[0:n, 0:T_], scalar1=128.0 * m, scalar2=None, op0=ALU.is_ge), ["rkk"], ["bf2"])
                    V(lambda v: v.tensor_add(out=bf[0:n, 0:T_], in0=bf[0:n, 0:T_], in1=bf2[0:n, 0:T_]), ["bf", "bf2"], ["bf"])
                V(lambda v: v.tensor_scalar_min(out=ysf[0:n, 0:T_], in0=ysf[0:n, 0:T_], scalar1=float(NSLOT)), ["ysf"], ["ysf"])
                V(lambda v: v.scalar_tensor_tensor(out=pf[0:n, 0:T_], in0=bf[0:n, 0:T_], scalar=-128.0, in1=rkk[0:n, 0:T_],
                                                   op0=ALU.mult, op1=ALU.add), ["bf", "rkk"], ["pf"])
                V(lambda v, k=k: v.tensor_copy(out=ysrow[k][0:n, ti0:ti0 + T_], in_=ysf[0:n, 0:T_]), ["ysf", ("ysrow", k)], [("ysrow", k)])
                V(lambda v: v.scalar_tensor_tensor(out=pf[0:n, 0:T_], in0=pf[0:n, 0:T_], scalar=float(NE * NB), in1=bf[0:n, 0:T_],
                                                   op0=ALU.mult, op1=ALU.add), ["pf", "bf"], ["pf"])
                V(lambda v: v.scalar_tensor_tensor(out=pf[0:n, 0:T_], in0=eif[0:n, 0:T_], scalar=float(NB) / float(CAP), in1=pf[0:n, 0:T_],
                                                   op0=ALU.mult, op1=ALU.add), ["eif", "pf"], ["pf"])
                V(lambda v: v.tensor_add(out=pf[0:n, 0:T_], in0=pf[0:n, 0:T_], in1=ovk[0:n, 0:T_]), ["pf", "ovk"], ["pf"])
                V(lambda v, k=k: v.tensor_copy(out=dest[k][0:n, ti0:ti0 + T_], in_=pf[0:n, 0:T_]), ["pf", ("dest", k)], [("dest", k)])
                for j, ti in enumerate(tis):
                    SD.dma("pool", lambda g, k=k, ti=ti: g.indirect_dma_start(
                        out=stok[:, :], out_offset=bass.IndirectOffsetOnAxis(ap=dest[k][:, ti:ti + 1], axis=0),
                        in_=tokidx[:, ti, :], in_offset=None, bounds_check=bc_reg, oob_is_err=False),
                        reads=[("dest", k), "tokidx", "stok"], writes=[("stok_sc", ti, k)])

        gctr = [0]
        pending = []
        active = []
        groups = []
        stg = STG_Q

        def jobs_in(ph):
            return [jb for jb in active if jb["ph"] == ph]

        NBG = 8

        def hook_pe_early():
            drain_bg(NBG)
            for jb in jobs_in(1):
                mix_mm(jb["ti"], jb["slot"], jb["off"], 1)
            for jb in jobs_in(2):
                rt_tr(jb["ti"], 0)

        def hook_dve_mid():
            drain_bg(NBG)
            for jb in jobs_in(1):
                mix_res(jb["ti"], 1)
            for jb in jobs_in(2):
                rt_cp(jb["ti"], 0)

        def hook_pe_mid():
            drain_bg(NBG)
            for jb in jobs_in(0):
                mix_mm(jb["ti"], jb["slot"], jb["off"], 0)
            for jb in jobs_in(2):
                rt_tr(jb["ti"], 1)

        def hook_start():
            for jb in jobs_in(2):
                ln_out(jb["ti"])

        def hook_out():
            drain_bg(NBG)
            for jb in jobs_in(2):
                rt_cp(jb["ti"], 1)

        def hook_end():
            for g_ in list(groups):
                if all(jb["ph"] >= 3 for jb in g_["jobs"]):
                    for part in (1, 2, 3):
                        router_part(g_["tis"], part, g_["gp"])
                    groups.remove(g_)
                    for jb in g_["jobs"]:
                        active.remove(jb)
                    break
            for jb in jobs_in(0):
                mix_res(jb["ti"], 0)
                ti = jb["ti"]
                if ti + 1 < NTT:
                    t1_, n1 = TT[ti + 1]
                    S.dma("sp", lambda g: g.dma_start(out=xres2[(ti + 1) % 3][0:n1, :], in_=src[t1_:t1_ + n1, :]), writes=[("xres", (ti + 1) % 3)])
            for jb in jobs_in(2):
                rt_logits(jb["ti"], jb["j"], jb["gp"])
            drain_bg(NBG)

        def end_slot():
            for jb in active:
                if jb["ph"] < 3:
                    jb["ph"] += 1
            if pending:
                jb = pending.pop(0)
                jb["ph"] = 0
                active.append(jb)

        slot_ctr = [0]

        def stg_rate(k, full):
            slot_ctr[0] += 1
            if isA and not full:
                return 1
            if isA:
                return 2 + (slot_ctr[0] % 2)
            return 2

        tiles = []
        if isA:
            tiles += [(src_pre, tok0, n, None, False) for (tok0, n, _) in FMPRE]
        tiles += [(src, tok0, n, tts, True) for (tok0, n, tts) in FM]
        ntl = len(tiles)
        pairs = []
        for k in range(ntl):
            for pr in range(KC // 2):
                pairs.append((k, pr))

        def mkP(q):
            k, pr = pairs[q]
            base = 2 * (q % 2)
            return [(2 * pr, base, B_XC[0]), (2 * pr + 1, base + 1, B_XC[1])]

        def front1(q):
            k, pr = pairs[q]; n = tiles[k][2]; slot = k % 2
            if isA:
                A_F1(mkP(q), n, slot, tiles[k][4])
            else:
                B_F1(mkP(q), n, slot)

        load_dma(tiles[0][0], tiles[0][1], tiles[0][2]); load_tr(tiles[0][2], 0)
        if ntl > 1:
            load_dma(tiles[1][0], tiles[1][1], tiles[1][2])
        npairs = len(pairs)
        front1(0)
        if isA:
            A_F2(mkP(0), tiles[0][2], 0, tiles[0][4])
        for q in range(npairs):
            k, pr = pairs[q]; n = tiles[k][2]; slot = k % 2; full = tiles[k][4]
            P = mkP(q)
            if isA:
                A_BQ(P, n)
            hook_start()
            if q + 1 < npairs:
                front1(q + 1)
            hook_pe_early()
            if isA:
                A_B1(P, n)
            hook_dve_mid()
            if isA:
                if q + 1 < npairs:
                    k1, _ = pairs[q + 1]
                    A_F2(mkP(q + 1), tiles[k1][2], k1 % 2, tiles[k1][4])
            hook_pe_mid()
            if not isA:
                B_F2(P, n, slot)
            hook_out()
            if isA and full:
                A_B2(P, n, slot)
            if isA and (not full) and q + 1 < npairs and tiles[pairs[q + 1][0]][4]:
                S.op("dve", lambda v: v.tensor_scalar(out=state[:], in0=state[:], scalar1=flg[:, 0:1], scalar2=None, op0=ALU.mult),
                     reads=[("state", c) for c in range(KC)] + ["flg"], writes=[("state", c) for c in range(KC)])
            hook_end()
            if pr == 1 and k + 1 < ntl:
                load_tr(tiles[k + 1][2], (k + 1) % 2)
                if k + 2 < ntl:
                    load_dma(tiles[k + 2][0], tiles[k + 2][1], tiles[k + 2][2])
            for _ in range(stg_rate(k, full)):
                if stg:
                    stg.pop(0)()
            if pr == KC // 2 - 1 and tiles[k][3] is not None:
                tis = list(tiles[k][3])
                gctr[0] += 1
                g_ = dict(tis=tis, jobs=[], part=0, gp=gctr[0] % 2)
                for j, ti in enumerate(tis):
                    jb = dict(ti=ti, slot=slot, off=j * 128, j=j, ph=-1, gp=gctr[0] % 2)
                    g_["jobs"].append(jb); pending.append(jb)
                groups.append(g_)
            end_slot()
        while pending or active or BG:
            hook_start(); hook_pe_early(); hook_dve_mid(); hook_pe_mid(); hook_out(); hook_end()
            end_slot()
        while stg and STG_DONE[0] < STG_NEED[layer]:
            stg.pop(0)()
        S.barrier()
        st.close()

    def moe_phase(layer, final):
        st = contextlib.ExitStack()
        lng = sb(st, "m_lng", [128, D]); lnb = sb(st, "m_lnb", [128, D])
        S.dma("sp", lambda g: g.dma_start(out=lng[:], in_=lnp[layer * 2 + 1, 0]), writes=["lng"])
        S.dma("sp", lambda g: g.dma_start(out=lnb[:], in_=lnp[layer * 2 + 1, 1]), writes=["lnb"])
        idx = sb(st, "idx", [128, NE * NB, 2], I32)
        S.dma("sp", lambda g: g.dma_start(out=idx[:], in_=stok.rearrange("(p f) o -> p f o", p=128)), writes=["idx"])
        NWB = 3
        wg = [sb(st, "wg%d" % i, [128, KC, DEXP], BF16) for i in range(NWB)]
        wu = [sb(st, "wu%d" % i, [128, KC, DEXP], BF16) for i in range(NWB)]
        wd = [sb(st, "wd%d" % i, [128, 4, D], BF16) for i in range(NWB)]
        xs = [sb(st, "xs%d" % i, [128, NB, D], BF16) for i in range(NWB)]
        xsT = [sb(st, "xsT%d" % i, [128, KC, CAP], BF16) for i in range(2)]
        sg = [sb(st, "sg%d" % i, [128, CAP]) for i in range(2)]
        hbT = [sb(st, "hbT%d" % i, [128, 4, CAP], BF16) for i in range(2)]
        ysb = [sb(st, "ysb%d" % i, [128, D]) for i in range(2)]
        tpb = tpb_all

        def load_w(e):
            s = e % NWB
            if "g" in STAGED[layer]:
                S.dma("sp", lambda g: g.dma_start(out=wg[s][:], in_=wgbL[layer][e].rearrange("(k p) f -> p k f", p=128)), writes=[("wg", s)])
            else:
                S.dma("pool", lambda g: g.dma_start(out=wg[s][:], in_=wgate[layer, e].rearrange("(k p) f -> p k f", p=128)), writes=[("wg", s)])
            if "u" in STAGED[layer]:
                S.dma("sp", lambda g: g.dma_start(out=wu[s][:], in_=wubL[layer][e].rearrange("(k p) f -> p k f", p=128)), writes=[("wu", s)])
            else:
                S.dma("pool", lambda g: g.dma_start(out=wu[s][:], in_=wup[layer, e].rearrange("(k p) f -> p k f", p=128)), writes=[("wu", s)])
            if "d" in STAGED[layer]:
                S.dma("sp", lambda g: g.dma_start(out=wd[s][:], in_=wdbL[layer][e].rearrange("(j p) m -> p j m", p=128)), writes=[("wd", s)])
            else:
                S.dma("pool", lambda g: g.dma_start(out=wd[s][:], in_=wdown[layer, e].rearrange("(j p) m -> p j m", p=128)), writes=[("wd", s)])
            for b in range(NB):
                S.dma("pool", lambda g, b=b: g.indirect_dma_start(
                    out=xs[s][:, b, :], out_offset=None, in_=hAb[:, :],
                    in_offset=bass.IndirectOffsetOnAxis(ap=idx[:, e * NB + b, 0:1], axis=0)),
                    reads=["idx"], writes=[("xs", s, b)])

        def xs_transpose(e, b):
            s = e % 2
            sw = e % NWB
            tb_i = 0 if (e * NB + b) % 2 == 0 else 7
            tpx = tpb_all if tb_i == 0 else tpb_7
            S.group("pe", [lambda pe, j=j: pe.transpose(out=tpx[:, j * 128:(j + 1) * 128], in_=xs[sw][:, b, j * 128:(j + 1) * 128],
                                                        identity=ident_b[:]) for j in range(KC)],
                    reads=[("xs", sw, b), "ident_b"], writes=[("bank", tb_i)])
            if b % 2 == 0:
                S.op("act", lambda a: a.activation(out=xsT[s][:, :, b * 128:(b + 1) * 128],
                                                   in_=tpx.rearrange("p (k t) -> p k t", k=KC), func=AF.Copy),
                     reads=[("bank", tb_i)], writes=[("xsT", s, b)])
            else:
                S.op("dve", lambda v: v.tensor_copy(out=xsT[s][:, :, b * 128:(b + 1) * 128],
                                                    in_=tpx.rearrange("p (k t) -> p k t", k=KC)),
                     reads=[("bank", tb_i)], writes=[("xsT", s, b)])

        load_w(0)
        load_w(1)
        for b in range(NB):
            xs_transpose(0, b)
        ysi = 0
        for e in range(NE):
            s = e % 2
            sw = e % NWB
            if e + 2 < NE:
                load_w(e + 2)
            XR = [("xsT", s, b) for b in range(NB)]
            for j in range(4):
                gb, ubk = (1, 2) if j % 2 == 0 else (3, 4)
                S.group("pe", [lambda pe, k=k, j=j: pe.matmul(out=bank[gb][:, 0:CAP], lhsT=wg[sw][:, k, j * 128:(j + 1) * 128],
                                                              rhs=xsT[s][:, k, :], start=(k == 0), stop=(k == KC - 1)) for k in range(KC)],
                        reads=XR + [("wg", sw)], writes=[("bank", gb)])
                S.group("pe", [lambda pe, k=k, j=j: pe.matmul(out=bank[ubk][:, 0:CAP], lhsT=wu[sw][:, k, j * 128:(j + 1) * 128],
                                                              rhs=xsT[s][:, k, :], start=(k == 0), stop=(k == KC - 1)) for k in range(KC)],
                        reads=XR + [("wu", sw)], writes=[("bank", ubk)])
                S.op("act", lambda a, j=j: a.activation(out=sg[j % 2][:], in_=bank[gb][:, 0:CAP], func=AF.Silu),
                     reads=[("bank", gb)], writes=[("sg", j % 2)])
                S.op("dve", lambda v, j=j: v.tensor_tensor(out=hbT[s][:, j, :], in0=sg[j % 2][:], in1=bank[ubk][:, 0:CAP], op=ALU.mult),
                     reads=[("sg", j % 2), ("bank", ubk)], writes=[("hbT", s, j)])
                if j < NB and e + 1 < NE:
                    xs_transpose(e + 1, j)
            HR = [("hbT", s, j) for j in range(4)]
            for b in range(NB):
                yb = ysb[ysi % 2]
                for half in range(2):
                    S.group("pe", [lambda pe, j=j, b=b, half=half: pe.matmul(out=bank[5 + half][:, :], lhsT=hbT[s][:, j, b * 128:(b + 1) * 128],
                                                                             rhs=wd[sw][:, j, half * 512:(half + 1) * 512],
                                                                             start=(j == 0), stop=(j == 3)) for j in range(4)],
                            reads=HR + [("wd", sw)], writes=[("bank", 5 + half)])
                S.op("act", lambda a: a.activation(out=yb[:, 0:512], in_=bank[5][:, :], func=AF.Copy),
                     reads=[("bank", 5)], writes=[("ysb", ysi % 2, 0)])
                S.op("dve", lambda v: v.tensor_copy(out=yb[:, 512:1024], in_=bank[6][:, :]),
                     reads=[("bank", 6)], writes=[("ysb", ysi % 2, 1)])
                r0 = (e * NB + b) * 128
                S.dma("sp", lambda g, r0=r0, yb=yb: g.dma_start(out=ys[r0:r0 + 128, :], in_=yb[:]),
                      reads=[("ysb", ysi % 2, 0), ("ysb", ysi % 2, 1)], writes=[("ys", e, b)])
                ysi += 1
        S.barrier()
        ya = [sb(st, "ya%d" % i, [128, D]) for i in range(2)]
        ybb = [sb(st, "yb%d" % i, [128, D]) for i in range(2)]
        hr = [sb(st, "hr%d" % i, [128, D]) for i in range(2)]
        vvs = [sb(st, "m_vv%d" % i, [128, D]) for i in range(3)]
        ho = [sb(st, "m_ho%d" % i, [128, D]) for i in range(2)]
        stats2 = [sb(st, "m_stats%d" % i, [128, 2, 6]) for i in range(3)]
        mv2 = [sb(st, "m_mv%d" % i, [128, 2]) for i in range(3)]
        rstd2 = [sb(st, "m_rstd%d" % i, [128, 1]) for i in range(3)]
        nmr2 = [sb(st, "m_nmr%d" % i, [128, 1]) for i in range(3)]

        eps_t = sb(st, "eps_t", [128, 1])
        S.op("dve", lambda v: v.memset(eps_t[:], LN_EPS), writes=["eps_t"])

        def comb_load(ti):
            tok0, n = TT[ti]
            s = ti % 2
            S.dma("pool", lambda g: g.indirect_dma_start(out=ya[s][:], out_offset=None, in_=ys[:, :],
                                                         in_offset=bass.IndirectOffsetOnAxis(ap=ysrow[0][:, ti:ti + 1], axis=0)),
                  reads=[("ysrow", 0)], writes=[("ya", s)])
            S.dma("pool", lambda g: g.indirect_dma_start(out=ybb[s][:], out_offset=None, in_=ys[:, :],
                                                         in_offset=bass.IndirectOffsetOnAxis(ap=ysrow[1][:, ti:ti + 1], axis=0)),
                  reads=[("ysrow", 1)], writes=[("yb", s)])
            S.dma("sp", lambda g: g.dma_start(out=hr[s][0:n, :], in_=hA[tok0:tok0 + n, :]), writes=[("hr", s)])

        def comb_front(ti):
            tok0, n = TT[ti]
            s = ti % 2
            r = ti % 3
            v_ = vvs[r]; stats = stats2[r]; mv = mv2[r]; rstd = rstd2[r]; nmr = nmr2[r]
            S.op("act", lambda a: a.activation(out=v_[0:n, :], in_=ya[s][0:n, :], func=AF.Copy, scale=gate[0][0:n, ti:ti + 1]),
                 reads=[("ya", s), ("gate", 0)], writes=[("vv", r)])
            S.op("dve", lambda v: v.scalar_tensor_tensor(out=v_[0:n, :], in0=ybb[s][0:n, :], scalar=gate[1][0:n, ti:ti + 1], in1=v_[0:n, :],
                                                         op0=ALU.mult, op1=ALU.add), reads=[("yb", s), ("gate", 1), ("vv", r)], writes=[("vv", r)])
            S.op("dve", lambda v: v.scalar_tensor_tensor(out=v_[0:n, :], in0=hr[s][0:n, :], scalar=ALPHA, in1=v_[0:n, :],
                                                         op0=ALU.mult, op1=ALU.add), reads=[("hr", s), ("vv", r)], writes=[("vv", r)])
            for half in range(2):
                S.op("dve", lambda v, half=half: v.bn_stats(out=stats[0:n, half, :], in_=v_[0:n, half * 512:(half + 1) * 512]),
                     reads=[("vv", r)], writes=[("stats", r, half)])
            S.op("dve", lambda v: v.bn_aggr(out=mv[0:n, :], in_=stats[0:n].rearrange("p a b -> p (a b)")),
                 reads=[("stats", r, 0), ("stats", r, 1)], writes=[("mv", r)])
            S.op("act", lambda a: a.activation(out=rstd[0:n, :], in_=mv[0:n, 1:2], func=AF.Sqrt, bias=eps_t[0:n, :], scale=1.0),
                 reads=[("mv", r), "eps_t"], writes=[("rstd", r)])
            S.op("dve", lambda v: v.reciprocal(out=rstd[0:n, :], in_=rstd[0:n, :]), reads=[("rstd", r)], writes=[("rstd", r)])
            S.op("dve", lambda v: v.scalar_tensor_tensor(out=nmr[0:n, :], in0=mv[0:n, 0:1], scalar=-1.0, in1=rstd[0:n, :],
                                                         op0=ALU.mult, op1=ALU.mult), reads=[("mv", r), ("rstd", r)], writes=[("nmr", r)])

        def comb_tail(ti):
            tok0, n = TT[ti]
            s = ti % 2
            r = ti % 3
            v_ = vvs[r]; rstd = rstd2[r]; nmr = nmr2[r]
            S.op("act", lambda a: a.activation(out=v_[0:n, :], in_=v_[0:n, :], func=AF.Identity, scale=rstd[0:n, :], bias=nmr[0:n, :]),
                 reads=[("vv", r), ("rstd", r), ("nmr", r)], writes=[("vv", r)])
            S.op("dve", lambda g: g.tensor_tensor(out=v_[0:n, :], in0=v_[0:n, :], in1=lng[0:n, :], op=ALU.mult),
                 reads=[("vv", r), "lng"], writes=[("vv", r)])
            S.op("dve", lambda g: g.tensor_tensor(out=ho[s][0:n, :], in0=v_[0:n, :], in1=lnb[0:n, :], op=ALU.add),
                 reads=[("vv", r), "lnb"], writes=[("ho", s)])
            if final:
                if ti >= 1:
                    S.dma("sp", lambda g: g.dma_start(out=out[tok0 - NHEAD:tok0 - NHEAD + n, :], in_=ho[s][0:n, :]),
                          reads=[("ho", s)], writes=[("out", ti)])
            else:
                S.dma("sp", lambda g: g.dma_start(out=hB[tok0:tok0 + n, :], in_=ho[s][0:n, :]), reads=[("ho", s)], writes=[("hB", ti)])

        comb_load(0)
        comb_load(1)
        comb_front(0)
        for ti in range(NTT):
            if ti + 2 < NTT:
                comb_load(ti + 2)
            if ti + 1 < NTT:
                comb_front(ti + 1)
            comb_tail(ti)
        S.barrier()
        st.close()

    S.barrier()
    for ph in phases:
        if ph == "A":
            mixer_phase("A", 0, xin, xpre)
        elif ph == "B":
            mixer_phase("B", 1, hB, None)
        elif ph == "M0":
            moe_phase(0, False)
        elif ph == "M1":
            moe_phase(1, True)
    S.barrier()
    top.close()
    return nc


def _host_inputs(inp):
    f = lambda a: np.ascontiguousarray(np.asarray(a, dtype=np.float32))
    x = f(inp["x"]); meta = f(inp["meta_tokens"])

    def pvec(v):
        return np.ascontiguousarray(f(v).reshape(KC, 128).T)

    cw = f(inp["lru_conv_w"])[0]
    a_vec = np.stack([pvec(cw[0]), pvec(cw[1]), pvec(cw[2]), pvec(cw[3]), pvec(inp["lru_conv_b"][0]),
                      pvec(inp["lru_b_a"][0]), pvec(inp["lru_b_i"][0]), pvec(inp["lru_lambda"][0])], axis=1).reshape(128, 8 * KC)
    cwb = f(inp["sc_conv_w"])[0]
    b_vec = np.stack([pvec(cwb[0]), pvec(cwb[1]), pvec(cwb[2])], axis=1).reshape(128, 3 * KC)

    def blockdiag(w):
        w = f(w)
        o = np.zeros((128, KC, 128), np.float32)
        for c in range(KC):
            o[0:64, c, 0:64] = w[2 * c]
            o[64:128, c, 64:128] = w[2 * c + 1]
        return o

    lnp = np.zeros((4, 2, 128, D), np.float32)
    g = f(inp["ln_g"]); b = f(inp["ln_b"])
    for l in range(2):
        for j in range(2):
            lnp[l * 2 + j, 0] = g[l, j][None, :]
            lnp[l * 2 + j, 1] = b[l, j][None, :]
    wr = np.concatenate([f(inp["moe_w_group"]), f(inp["moe_w_expert"])], axis=2)
    brv = np.concatenate([f(inp["moe_b_group"]), f(inp["moe_b_expert"]).reshape(2, 32)], axis=1)
    br = np.ascontiguousarray(np.broadcast_to(brv[:, None, :], (2, 128, 36)))
    cst = np.zeros((128, 128 * 4 + 64), np.float32)
    cst[:, 0:128] = np.eye(128, dtype=np.float32)
    cst[:, 128:256] = np.triu(np.ones((128, 128), np.float32), k=1)
    cst[:, 256:384] = 1.0
    cst[:, 512:512 + NE] = (np.arange(NE, dtype=np.float32) * CAP)[None, :]
    csti = np.zeros((128, NTT, 2), np.int32)
    for ti, (t0, n) in enumerate(TT):
        csti[:, ti, 0] = t0 + np.arange(128)
        csti[:, ti, 1] = t0 + np.arange(128)
    csti = csti.reshape(128, NTT * 2)
    cbrow = np.ascontiguousarray(np.broadcast_to(f(inp["lru_conv_b"])[0][None, :], (2, D)))
    shared = dict(a_cbrow=cbrow, a_win=f(inp["lru_w_in"])[0], a_wout=f(inp["lru_w_out"])[0], a_vec=a_vec,
                  a_wa=blockdiag(inp["lru_w_a"][0]), a_wi=blockdiag(inp["lru_w_i"][0]),
                  b_win=f(inp["sc_w_in"])[0], b_wout=f(inp["sc_w_out"])[0], b_vec=b_vec, lnp=lnp, wr=wr, br=br,
                  wgate=f(inp["moe_w_gate"]), wup=f(inp["moe_w_up"]), wdown=f(inp["moe_w_down"]), cst=cst, csti=csti)
    maps = []
    for c in range(8):
        bb, half = c // 2, c % 2
        if half == 0:
            xin = np.concatenate([meta, x[bb, 0:NMAIN]], axis=0)
            xpre = np.zeros((NPRE, D), np.float32)
            flag = np.zeros((128, 1), np.float32)
        else:
            xin = x[bb, NMAIN - NHEAD:2 * NMAIN]
            xpre = np.concatenate([meta, x[bb, 0:NMAIN - NHEAD]], axis=0)
            flag = np.ones((128, 1), np.float32)
        m = dict(shared)
        m.update(xin=np.ascontiguousarray(xin), xpre=np.ascontiguousarray(xpre), flag=flag)
        maps.append(m)
    return maps


_NC_CACHE = {}


def kernel(**inputs):
    maps = _host_inputs(inputs)
    if "nc" not in _NC_CACHE:
        _NC_CACHE["nc"] = build_program()
    nc = _NC_CACHE["nc"]
    res = run_bass_kernel_spmd(nc, maps, core_ids=list(range(8)))
    outs = [np.asarray(r["out"], dtype=np.float32) for r in res.results]
    full = np.zeros((4, 2 * NMAIN, D), np.float32)
    for c in range(8):
        bb, half = c // 2, c % 2
        full[bb, half * NMAIN:(half + 1) * NMAIN] = outs[c]
    return full
```

```python
import numpy as np
import concourse.bass as bass
import concourse.mybir as mybir
from concourse.bass_utils import run_bass_kernel_spmd

F32 = mybir.dt.float32
BF16 = mybir.dt.bfloat16
I32 = mybir.dt.int32
AF = mybir.ActivationFunctionType
ALU = mybir.AluOpType
AX = mybir.AxisListType

D = 1024
KC = 8
NHEAD = 16
NMAIN = 4096
NT = NHEAD + NMAIN
NPRE = 4096
NE = 32
CAP = 384
NB = CAP // 128
NSLOT = NE * CAP
DEXP = 512
ALPHA = (2.0 * 2) ** 0.25
LN_EPS = 1e-5
GC0 = 0.7978845608028654
GC1 = 0.044715
BIG = 1.0e9

TT = [(0, NHEAD)] + [(NHEAD + 128 * i, 128) for i in range(NMAIN // 128)]
NTT = len(TT)
FM = [(0, NHEAD, [0])] + [(NHEAD + 512 * k, 512, [1 + 4 * k + j for j in range(4)]) for k in range(NMAIN // 512)]
FMPRE = [(512 * k, 512, None) for k in range(NPRE // 512)]


STRICT_SAME_ENGINE = True


class Sched:
    def __init__(self, nc, n_dma_sems=20):
        self.nc = nc
        self.eng = {"pe": nc.tensor, "act": nc.scalar, "dve": nc.vector, "pool": nc.gpsimd, "sp": nc.sync}
        self.sem = {k: nc.alloc_semaphore("s_" + k) for k in ("pe", "act", "dve", "pool")}
        self.cnt = {k: 0 for k in self.sem}
        self.dsem = {q: [nc.alloc_semaphore("d_%s_%d" % (q, i)) for i in range(n_dma_sems)] for q in ("sp", "pool")}
        self.dsem["stg"] = [nc.alloc_semaphore("d_stg_%d" % i) for i in range(10)]
        self.dcnt = {q: [0] * len(self.dsem[q]) for q in self.dsem}
        self.drr = {q: 0 for q in self.dsem}
        self.waited = {k: {} for k in self.eng}
        self.bufs = {}
        self.semobj = {}
        for k, s in self.sem.items():
            self.semobj[id(s)] = s
        self.all_events = {}

    def _wait(self, e, ev):
        s, v = ev
        w = self.waited[e]
        if w.get(id(s), 0) >= v:
            return
        self.eng[e].wait_ge(s, v)
        w[id(s)] = v

    def _deps(self, e, reads, writes, dist=3):
        evs = []
        for k in reads:
            b = self.bufs.get(k)
            if b and b["w"]:
                evs.append(b["w"])
        for k in writes:
            b = self.bufs.get(k)
            if b:
                if b["w"]:
                    evs.append(b["w"])
                evs.extend(b["r"])
        for ev in evs:
            s, v = ev
            if e in self.sem and s is self.sem[e]:
                if e == "pe" or (not STRICT_SAME_ENGINE and self.cnt[e] - v >= dist):
                    continue
            self._wait(e, ev)

    def _record(self, ev, reads, writes):
        for k in reads:
            self.bufs.setdefault(k, {"w": None, "r": []})["r"].append(ev)
        for k in writes:
            self.bufs[k] = {"w": ev, "r": []}
        self.all_events[id(ev[0])] = ev

    def op(self, e, fn, reads=(), writes=(), dist=3):
        self._deps(e, reads, writes, dist)
        ins = fn(self.eng[e])
        ins.then_inc(self.sem[e], 1)
        self.cnt[e] += 1
        ev = (self.sem[e], self.cnt[e])
        self._record(ev, reads, writes)
        return ev

    def group(self, e, fns, reads=(), writes=()):
        self._deps(e, reads, writes)
        ins = None
        for fn in fns:
            ins = fn(self.eng[e])
        ins.then_inc(self.sem[e], 1)
        self.cnt[e] += 1
        ev = (self.sem[e], self.cnt[e])
        self._record(ev, reads, writes)
        return ev

    def dma(self, q, fn, reads=(), writes=(), sems=None):
        self._deps(q, reads, writes)
        sq = sems or q
        i = self.drr[sq]
        self.drr[sq] = (i + 1) % len(self.dsem[sq])
        s = self.dsem[sq][i]
        if self.dcnt[sq][i] > 0:
            self._wait(q, (s, 16 * self.dcnt[sq][i]))
        ins = fn(self.eng[q])
        ins.then_inc(s, 16)
        self.dcnt[sq][i] += 1
        ev = (s, 16 * self.dcnt[sq][i])
        self._record(ev, reads, writes)
        return ev

    def barrier(self):
        for e in self.eng:
            for ev in list(self.all_events.values()):
                self._wait(e, ev)
        self.bufs = {}


def build_program(phases=("A", "M0", "B", "M1"), debug_out=False):
    nc = bass.Bass("TRN2", target_bir_lowering=False)
    S = Sched(nc)

    def din(name, shape, dt=F32):
        return nc.dram_tensor(name, list(shape), dt, kind="ExternalInput").ap()

    scratch_kind = "ExternalOutput" if debug_out else "Internal"

    def dsc(name, shape, dt=F32):
        return nc.dram_tensor(name, list(shape), dt, kind=scratch_kind).ap()

    xin = din("xin", [NT, D])
    xpre = din("xpre", [NPRE, D])
    flag = din("flag", [128, 1])
    a_win = din("a_win", [D, 2 * D])
    a_wout = din("a_wout", [D, D])
    a_vec = din("a_vec", [128, 8 * KC])
    a_cbrow = din("a_cbrow", [2, D])
    a_wa = din("a_wa", [128, KC, 128])
    a_wi = din("a_wi", [128, KC, 128])
    b_win = din("b_win", [D, 3 * D])
    b_wout = din("b_wout", [D, D])
    b_vec = din("b_vec", [128, 3 * KC])
    lnp = din("lnp", [4, 2, 128, D])
    wr = din("wr", [2, D, 36])
    br = din("br", [2, 128, 36])
    wgate = din("wgate", [2, NE, D, DEXP])
    wup = din("wup", [2, NE, D, DEXP])
    wdown = din("wdown", [2, NE, DEXP, D])
    cst = din("cst", [128, 128 * 4 + 64])
    csti = din("csti", [128, NTT * 2], I32)
    out = nc.dram_tensor("out", [NMAIN, D], F32, kind="ExternalOutput").ap()

    hA = dsc("hA", [NT, D])
    hAb = dsc("hAb", [NT + 1, D], BF16)
    hB = dsc("hB", [NT, D])
    stok = dsc("stok", [128 * NE * NB, 2], I32)
    ys = dsc("ys", [NSLOT + 1, D])
    wgbL = [nc.dram_tensor("wgb%d" % l, [NE, D, DEXP], BF16, kind="Internal").ap() for l in range(2)]
    wubL = [nc.dram_tensor("wub%d" % l, [NE, D, DEXP], BF16, kind="Internal").ap() for l in range(2)]
    wdbL = [nc.dram_tensor("wdb%d" % l, [NE, DEXP, D], BF16, kind="Internal").ap() for l in range(2)]

    STAGED = {0: ("g", "u", "d"), 1: ("g", "u", "d")}

    def staging_list(layer):
        lst = []
        wgb, wub, wdb = wgbL[layer], wubL[layer], wdbL[layer]
        for e in range(NE):
            if "g" in STAGED[layer]:
                lst.append(lambda e=e: S.dma("pool", lambda g: g.dma_start(out=wgb[e], in_=wgate[layer, e]), writes=[("wgb", e)], sems="stg"))
            if "u" in STAGED[layer]:
                lst.append(lambda e=e: S.dma("pool", lambda g: g.dma_start(out=wub[e], in_=wup[layer, e]), writes=[("wub", e)], sems="stg"))
            if "d" in STAGED[layer]:
                lst.append(lambda e=e: S.dma("pool", lambda g: g.dma_start(out=wdb[e], in_=wdown[layer, e]), writes=[("wdb", e)], sems="stg"))
        return lst

    import contextlib
    top = contextlib.ExitStack()

    uniq = [0]

    def sb(stack, name, shape, dt=F32):
        uniq[0] += 1
        return stack.enter_context(nc.sbuf_tensor("%s_%d" % (name, uniq[0]), list(shape), dt))

    def ps(stack, name, shape, dt=F32):
        return stack.enter_context(nc.psum_tensor(name, list(shape), dt))

    ident_b = sb(top, "ident_b", [128, 128], BF16)
    ident_f = sb(top, "ident_f", [128, 128])
    tri_b = sb(top, "tri_b", [128, 128], BF16)
    ones_b = sb(top, "ones_b", [128, 128], BF16)
    ecrow = sb(top, "ecrow", [128, NE])
    tokidx = sb(top, "tokidx", [128, NTT, 2], I32)
    dest = [sb(top, "dest%d" % k, [128, NTT], I32) for k in range(2)]
    ysrow = [sb(top, "ysrow%d" % k, [128, NTT], I32) for k in range(2)]
    gate = [sb(top, "gate%d" % k, [128, NTT]) for k in range(2)]
    c_mhalf = sb(top, "c_mhalf", [128, 1])
    zrow = sb(top, "zrow", [1, D], BF16)
    zrowf = sb(top, "zrowf", [1, D])
    bank = [ps(top, "bank%d" % i, [128, 512]) for i in range(8)]
    tpb_f = bank[0]
    tpb_all = bank[0][:].bitcast(BF16)
    tpb_7 = bank[7][:].bitcast(BF16)

    bc_reg = nc.gpsimd.alloc_register("bc_reg")
    nc.gpsimd.reg_mov(bc_reg, 128 * NE * NB - 1)
    S.dma("pool", lambda g: g.dma_start(out=ident_b[:], in_=cst[:, 0:128]), writes=["ident_b"])
    S.dma("sp", lambda g: g.dma_start(out=ident_f[:], in_=cst[:, 0:128]), writes=["ident_f"])
    S.dma("pool", lambda g: g.dma_start(out=tri_b[:], in_=cst[:, 128:256]), writes=["tri_b"])
    S.dma("pool", lambda g: g.dma_start(out=ones_b[:], in_=cst[:, 256:384]), writes=["ones_b"])
    S.dma("sp", lambda g: g.dma_start(out=ecrow[:], in_=cst[:, 512:512 + NE]), writes=["ecrow"])
    S.dma("sp", lambda g: g.dma_start(out=tokidx[:], in_=csti.rearrange("p (t o) -> p t o", o=2)), writes=["tokidx"])
    S.op("dve", lambda v: v.memset(c_mhalf[:], -0.5), writes=["c_mhalf"])
    S.op("dve", lambda v: v.memset(zrow[:], 0.0), writes=["zrow"])
    S.op("dve", lambda v: v.memset(zrowf[:], 0.0), writes=["zrowf"])
    S.dma("sp", lambda g: g.dma_start(out=hAb[NT:NT + 1, :], in_=zrow[:]), reads=["zrow"], writes=["hAb_z"])
    S.dma("sp", lambda g: g.dma_start(out=ys[NSLOT:NSLOT + 1, :], in_=zrowf[:]), reads=["zrowf"], writes=["ys_z"])

    STG_DONE = [0]
    STG_NEED = {0: 3 * NE, 1: 6 * NE}
    STG_Q = []
    for l_ in range(2):
        for fn_ in staging_list(l_):
            STG_Q.append(lambda fn_=fn_: (fn_(), STG_DONE.__setitem__(0, STG_DONE[0] + 1)))

    def mixer_phase(kind, layer, src, src_pre):
        st = contextlib.ExitStack()
        isA = (kind == "A")
        ncol = 2 if isA else 3
        win_d = a_win if isA else b_win
        wout_d = a_wout if isA else b_wout
        win = sb(st, "win", [128, KC, ncol * D], BF16)
        wout = sb(st, "wout", [128, KC, D], BF16)
        lng = sb(st, "lng", [128, D])
        lnb = sb(st, "lnb", [128, D])
        wrt = sb(st, "wrt", [128, KC, 36])
        brt = sb(st, "brt", [128, 36])
        ecrow4 = sb(st, "ecrow4", [128, 4, NE])
        for k in range(KC):
            S.dma("pool", lambda g, k=k: g.dma_start(out=win[:, k, :], in_=win_d[k * 128:(k + 1) * 128, :]),
                  writes=[("win", k)])
        S.dma("pool", lambda g: g.dma_start(out=wout[:], in_=wout_d.rearrange("(k p) m -> p k m", p=128)),
              writes=["wout"])
        S.dma("sp", lambda g: g.dma_start(out=lng[:], in_=lnp[layer * 2, 0]), writes=["lng"])
        S.dma("sp", lambda g: g.dma_start(out=lnb[:], in_=lnp[layer * 2, 1]), writes=["lnb"])
        S.dma("sp", lambda g: g.dma_start(out=wrt[:], in_=wr[layer].rearrange("(k p) n -> p k n", p=128)),
              writes=["wrt"])
        S.dma("sp", lambda g: g.dma_start(out=brt[:], in_=br[layer]), writes=["brt"])
        for j in range(4):
            S.dma("sp", lambda g, j=j: g.dma_start(out=ecrow4[:, j, :], in_=cst[:, 512:512 + NE]), writes=[("ecrow4", j)])
        EC4 = [("ecrow4", j) for j in range(4)]
        WIN_R = [("win", k) for k in range(KC)]

        if isA:
            HIST, NK = 3, 4
            vec = sb(st, "vec", [128, 8, KC])
            wa = sb(st, "wa", [128, KC, 128], BF16)
            wi = sb(st, "wi", [128, KC, 128], BF16)
            S.dma("sp", lambda g: g.dma_start(out=vec[:], in_=a_vec.rearrange("p (v c) -> p v c", v=8)), writes=["vec"])
            S.dma("pool", lambda g: g.dma_start(out=wa[:], in_=a_wa[:, :, :]), writes=["wa"])
            S.dma("pool", lambda g: g.dma_start(out=wi[:], in_=a_wi[:, :, :]), writes=["wi"])
            flg = sb(st, "flg", [128, 1])
            S.dma("sp", lambda g: g.dma_start(out=flg[:], in_=flag[:, :]), writes=["flg"])
            sc = sb(st, "sc", [128, KC]); sch = sb(st, "sch", [128, KC])
            hba = sb(st, "hba", [128, KC]); hbi = sb(st, "hbi", [128, KC])
            t1 = sb(st, "spt1", [128, KC]); t2 = sb(st, "spt2", [128, KC]); t3 = sb(st, "spt3", [128, KC])
            t4 = sb(st, "spt4", [128, KC])
            lam = vec[:, 7, :]
            S.op("dve", lambda v: v.tensor_scalar_mul(out=t1[:], in0=lam, scalar1=-1.0), reads=["vec"], writes=["t1"])
            S.op("dve", lambda v: v.tensor_max(out=t1[:], in0=t1[:], in1=lam), reads=["vec", "t1"], writes=["t1"])
            S.op("act", lambda a: a.activation(out=t2[:], in_=t1[:], func=AF.Exp, scale=-1.0), reads=["t1"], writes=["t2"])
            S.op("dve", lambda v: v.tensor_scalar_add(out=t3[:], in0=t2[:], scalar1=2.0), reads=["t2"], writes=["t3"])
            S.op("dve", lambda v: v.reciprocal(out=t3[:], in_=t3[:]), reads=["t3"], writes=["t3"])
            S.op("dve", lambda v: v.tensor_mul(out=t2[:], in0=t2[:], in1=t3[:]), reads=["t2", "t3"], writes=["t2"])
            S.op("dve", lambda v: v.tensor_mul(out=t3[:], in0=t2[:], in1=t2[:]), reads=["t2"], writes=["t3"])
            S.op("dve", lambda v: v.memset(t4[:], 1.0 / 17.0), writes=["t4"])
            for nn in (15, 13, 11, 9, 7, 5, 3, 1):
                S.op("dve", lambda v: v.tensor_mul(out=t4[:], in0=t4[:], in1=t3[:]), reads=["t4", "t3"], writes=["t4"])
                S.op("dve", lambda v, nn=nn: v.tensor_scalar_add(out=t4[:], in0=t4[:], scalar1=1.0 / nn), reads=["t4"], writes=["t4"])
            S.op("dve", lambda v: v.tensor_mul(out=t4[:], in0=t4[:], in1=t2[:]), reads=["t4", "t2"], writes=["t4"])
            S.op("dve", lambda v: v.tensor_scalar(out=t1[:], in0=lam, scalar1=-1.0, scalar2=0.0, op0=ALU.mult, op1=ALU.max),
                 reads=["vec", "t1"], writes=["t1"])
            S.op("dve", lambda v: v.scalar_tensor_tensor(out=t1[:], in0=t4[:], scalar=2.0, in1=t1[:], op0=ALU.mult, op1=ALU.add),
                 reads=["t4", "t1"], writes=["t1"])
            S.op("dve", lambda v: v.tensor_scalar_mul(out=sc[:], in0=t1[:], scalar1=-8.0), reads=["t1"], writes=["sc"])
            S.op("dve", lambda v: v.tensor_scalar_mul(out=sch[:], in0=t1[:], scalar1=-4.0), reads=["t1"], writes=["sch"])
            S.op("dve", lambda v: v.tensor_scalar_mul(out=hba[:], in0=vec[:, 5, :], scalar1=0.5), reads=["vec"], writes=["hba"])
            S.op("dve", lambda v: v.tensor_scalar_mul(out=hbi[:], in0=vec[:, 6, :], scalar1=0.5), reads=["vec"], writes=["hbi"])
            state = sb(st, "state", [128, KC])
            S.op("dve", lambda v: v.memset(state[:], 0.0), writes=[("state", c) for c in range(KC)])
            cbhl = sb(st, "cbhl", [2, D], BF16); ones2 = sb(st, "ones2", [2, 512], BF16)
            st_tmp = contextlib.ExitStack()
            cb2 = sb(st_tmp, "cb2", [2, D]); cbh = sb(st_tmp, "cbh", [2, D], BF16); cbhf = sb(st_tmp, "cbhf", [2, D])
            S.dma("sp", lambda g: g.dma_start(out=cb2[:], in_=a_cbrow[:, :]), writes=["cb2"])
            S.op("dve", lambda v: v.tensor_copy(out=cbh[:], in_=cb2[:]), reads=["cb2"], writes=["cbh"])
            S.op("dve", lambda v: v.tensor_copy(out=cbhf[:], in_=cbh[:]), reads=["cbh"], writes=["cbhf"])
            S.op("dve", lambda v: v.tensor_sub(out=cb2[:], in0=cb2[:], in1=cbhf[:]), reads=["cb2", "cbhf"], writes=["cb2"])
            S.op("dve", lambda v: v.tensor_scalar(out=cbhf[:], in0=cbhf[:], scalar1=ident_f[0:2, 0:1], scalar2=None, op0=ALU.mult),
                 reads=["cbhf", "ident_f"], writes=["cbhf"])
            S.op("dve", lambda v: v.scalar_tensor_tensor(out=cbhl[:], in0=cb2[:], scalar=ident_f[0:2, 1:2], in1=cbhf[:],
                                                         op0=ALU.mult, op1=ALU.add), reads=["cb2", "cbhf", "ident_f"], writes=["cbhl"])
            S.op("dve", lambda v: v.memset(ones2[:], 1.0), writes=["ones2"])
            S.barrier()
            st_tmp.close()
        else:
            HIST, NK = 2, 3
            vec = sb(st, "vec", [128, 3, KC])
            S.dma("sp", lambda g: g.dma_start(out=vec[:], in_=b_vec.rearrange("p (v c) -> p v c", v=3)), writes=["vec"])
        ub = sb(st, "ub", [128, KC, HIST + 512], BF16)
        S.op("dve", lambda v: v.memset(ub[:], 0.0), writes=[("ub", c) for c in range(KC)])
        dg = sb(st, "dg", [128, KC, NK, 128], BF16)
        for c in range(KC):
            for k in range(NK):
                S.op("dve", lambda v, c=c, k=k: v.tensor_scalar(out=dg[:, c, k, :], in0=ident_f[:], scalar1=vec[:, k, c:c + 1], scalar2=None,
                                                                op0=ALU.mult), reads=["vec", "ident_f"], writes=[("dg", c)])

        xtok = sb(st, "xtok", [128, 4, D], BF16)
        xT = [sb(st, "xT%d" % i, [128, KC, 512], BF16) for i in range(2)]
        zT = [sb(st, "zT%d" % i, [128, KC, 512], BF16) for i in range(2)]
        NS = 4
        names = ("thr", "a2", "thi", "g", "uys") if isA else ("g", "thr")
        T = [{nm: sb(st, "t_%s%d" % (nm, i), [128, 512]) for nm in names} for i in range(NS)]
        xcb = [sb(st, "xcb%d" % i, [128, 512], BF16) for i in range(NS)] if isA else None
        if isA:
            B_TP, B_UX, B_UY, B_XC, B_R, B_I, B_MIX = 0, 1, 2, (3, 4), 5, 6, 7
            LGB = 2
        else:
            B_TP, B_CG, B_V, B_BG, B_XC, B_MIX = 0, 1, 2, 3, (4, 5), 7
            LGB = 6
        xres2 = [sb(st, "xres%d" % i, [128, D]) for i in range(3)]
        h1 = sb(st, "h1", [128, D]); h1T = sb(st, "h1T", [128, D])
        stats = sb(st, "stats", [128, 2, 6]); mv = sb(st, "mv", [128, 2]); rstd = sb(st, "rstd", [128, 1])
        nmr = sb(st, "nmr", [128, 1])
        lg2 = [sb(st, "lg%d" % i, [128, 4, 36]) for i in range(2)]; gmx = sb(st, "gmx", [128, 4]); gsh = sb(st, "gsh", [128, 4, 4])
        gex = sb(st, "gex", [128, 4, 4]); gsum = sb(st, "gsum", [128, 4]); pg = sb(st, "pg", [128, 4])
        goh = sb(st, "goh", [128, 4, 4]); elm = sb(st, "elm", [128, 4, NE]); el2 = sb(st, "el2", [128, 4, NE])
        m1 = sb(st, "m1", [128, 4]); m2 = sb(st, "m2", [128, 4]); oh1 = sb(st, "oh1", [128, 4, NE]); oh2 = sb(st, "oh2", [128, 4, NE])
        ohb = sb(st, "ohb", [128, 4, NE], BF16); w1 = sb(st, "w1", [128, 4])
        tot = sb(st, "tot", [128, NE]); rk = sb(st, "rk", [128, 4, NE]); ov = sb(st, "ov", [128, 4, NE])
        sel = sb(st, "sel", [128, 4, NE]); rkk = sb(st, "rkk", [128, 4]); ysf = sb(st, "ysf", [128, 4])
        pf = sb(st, "pf", [128, 4]); bf = sb(st, "bf", [128, 4]); bf2 = sb(st, "bf2", [128, 4])
        eif = sb(st, "eif", [128, 4]); ovk = sb(st, "ovk", [128, 4])
        S.op("dve", lambda v: v.memset(tot[:], 0.0), writes=["tot"])
        S.op("dve", lambda v: v.memset(ohb[:], 0.0), writes=["ohb"])
        for k in range(2):
            S.op("dve", lambda v, k=k: v.memset(dest[k][:], 1 << 24), writes=[("dest", k)])
            S.op("dve", lambda v, k=k: v.memset(ysrow[k][:], NSLOT), writes=[("ysrow", k)])
            S.op("dve", lambda v, k=k: v.memset(gate[k][:], 0.0), writes=[("gate", k)])
        stinit = sb(st, "stinit", [128, NE * NB * 2], I32)
        S.op("dve", lambda v: v.memset(stinit[:], NT), writes=["stinit"])
        S.dma("sp", lambda g: g.dma_start(out=stok.rearrange("(p f) o -> p (f o)", p=128), in_=stinit[:]),
              reads=["stinit"], writes=["stok"])

        def load_dma(dsrc, tok0, n):
            nt = (n + 127) // 128
            if n >= 128:
                S.dma("pool", lambda g: g.dma_start(out=xtok[:, 0:nt, :],
                                                    in_=dsrc[tok0:tok0 + n, :].rearrange("(t p) d -> p t d", p=128)),
                      writes=["xtok"])
            else:
                S.dma("pool", lambda g: g.dma_start(out=xtok[0:n, 0, :], in_=dsrc[tok0:tok0 + n, :]), writes=["xtok"])

        def load_tr(n, slot):
            nt = (n + 127) // 128
            rows = min(n, 128)
            for t in range(nt):
                bk_ = 0 if t % 2 == 0 else 7
                tpx = tpb_all if bk_ == 0 else tpb_7
                S.group("pe", [lambda pe, t=t, j=j: pe.transpose(out=tpx[:, j * 128:j * 128 + rows],
                                                                 in_=xtok[0:rows, t, j * 128:(j + 1) * 128],
                                                                 identity=ident_b[0:rows, 0:rows]) for j in range(KC)],
                        reads=["xtok", "ident_b"], writes=[("bank", bk_)])
                if t % 2 == 0:
                    S.op("act", lambda a, t=t: a.activation(out=xT[slot][:, :, t * 128:t * 128 + rows],
                                                            in_=tpx.rearrange("p (k t) -> p k t", k=KC)[:, :, 0:rows], func=AF.Copy),
                         reads=[("bank", bk_)], writes=[("xT", slot, t)])
                else:
                    S.op("dve", lambda v, t=t: v.tensor_copy(out=xT[slot][:, :, t * 128:t * 128 + rows],
                                                             in_=tpx.rearrange("p (k t) -> p k t", k=KC)[:, :, 0:rows]),
                         reads=[("bank", bk_)], writes=[("xT", slot, t)])

        def mm_acc(bk, col0, n, slot):
            return [lambda pe, k=k: pe.matmul(out=bank[bk][:, 0:n], lhsT=win[:, k, col0:col0 + 128], rhs=xT[slot][:, k, 0:n],
                                              start=(k == 0), stop=(k == KC - 1)) for k in range(KC)]

        def conv_mm(c, n, bk):
            fns = [lambda pe, k=k: pe.matmul(out=bank[bk][:, 0:n], lhsT=dg[:, c, k, :], rhs=ub[:, c, k:k + n],
                                             start=(k == 0), stop=(k == NK - 1 and not isA)) for k in range(NK)]
            if isA:
                fns.append(lambda pe: pe.matmul(out=bank[bk][:, 0:n], lhsT=cbhl[0:2, c * 128:(c + 1) * 128], rhs=ones2[0:2, 0:n],
                                                start=False, stop=True))
            return fns

        def hist_update(c, n):
            S.op("pool", lambda g: g.tensor_copy(out=ub[:, c, 0:HIST], in_=ub[:, c, n:n + HIST]),
                 reads=[("ub", c)], writes=[("ub", c)])

        F = 1

        def A_F1(P, n, slot, full):
            XTR = [("xT", slot, t_) for t_ in range(4)]
            bux = {}
            for idx, (c, si, xb) in enumerate(P):
                bux[c] = B_UX if idx == 0 else B_UY
                S.group("pe", mm_acc(bux[c], D + c * 128, n, slot), reads=WIN_R + XTR, writes=[("bank", bux[c])])
                if full:
                    S.op("act", lambda a: a.activation(out=ub[:, c, HIST:HIST + n], in_=bank[bux[c]][:, 0:n], func=AF.Copy),
                         reads=[("bank", bux[c])], writes=[("ub", c)])
                else:
                    S.op("dve", lambda v: v.tensor_copy(out=ub[:, c, HIST:HIST + n], in_=bank[bux[c]][:, 0:n]),
                         reads=[("bank", bux[c])], writes=[("ub", c)])
            for (c, si, xb) in P:
                S.group("pe", conv_mm(c, n, xb), reads=[("ub", c), ("dg", c), "cbhl", "ones2"], writes=[("bank", xb)])
                S.op("act", lambda a: a.activation(out=xcb[si][:, 0:n], in_=bank[xb][:, 0:n], func=AF.Copy),
                     reads=[("bank", xb)], writes=[("xcb", si)])
                hist_update(c, n)
            for (c, si, xb) in P:
                t = T[si]
                S.op("pe", lambda pe: pe.matmul(out=bank[B_R][:, 0:n], lhsT=wa[:, c, :], rhs=xcb[si][:, 0:n], start=True, stop=True),
                     reads=["wa", ("xcb", si)], writes=[("bank", B_R)])
                S.op("pe", lambda pe: pe.matmul(out=bank[B_I][:, 0:n], lhsT=wi[:, c, :], rhs=xcb[si][:, 0:n], start=True, stop=True),
                     reads=["wi", ("xcb", si)], writes=[("bank", B_I)])
                S.op("act", lambda a: a.activation(out=t["thr"][:, 0:n], in_=bank[B_R][:, 0:n], func=AF.Tanh, scale=0.5,
                                                   bias=hba[:, c:c + 1]), reads=[("bank", B_R), "hba"], writes=[("thr", si)])
                S.op("act", lambda a: a.activation(out=t["thi"][:, 0:n], in_=bank[B_I][:, 0:n], func=AF.Tanh, scale=0.5,
                                                   bias=hbi[:, c:c + 1]), reads=[("bank", B_I), "hbi"], writes=[("thi", si)])
            if full:
                for (c, si, xb) in P:
                    t = T[si]
                    S.op("act", lambda a: a.activation(out=t["a2"][:, 0:n], in_=t["thr"][:, 0:n], func=AF.Exp,
                                                       scale=sc[:, c:c + 1], bias=sc[:, c:c + 1]), reads=[("thr", si), "sc"], writes=[("a2", si)])
            for (c, si, xb) in P:
                t = T[si]
                S.op("act", lambda a: a.activation(out=t["thr"][:, 0:n], in_=t["thr"][:, 0:n], func=AF.Exp,
                                                   scale=sch[:, c:c + 1], bias=sch[:, c:c + 1]), reads=[("thr", si), "sch"], writes=[("thr", si)])
            if full:
                for (c, si, xb) in P:
                    S.group("pe", mm_acc(bux[c], c * 128, n, slot), reads=WIN_R + XTR, writes=[("bank", bux[c])])
                    S.op("act", lambda a: a.activation(out=T[si]["uys"][:, 0:n], in_=bank[bux[c]][:, 0:n], func=AF.Copy),
                         reads=[("bank", bux[c])], writes=[("uys", si)])
                for (c, si, xb) in P:
                    t = T[si]
                    S.op("act", lambda a: a.activation(out=t["g"][:, 0:n], in_=t["uys"][:, 0:n], func=AF.Square),
                         reads=[("uys", si)], writes=[("g", si)])

        def A_F2(P, n, slot, full):
            if not full:
                for (c, si, xb) in P:
                    t = T[si]
                    S.op("dve", lambda v: v.tensor_tensor(out=t["a2"][:, 0:n], in0=t["thr"][:, 0:n], in1=t["thr"][:, 0:n], op=ALU.mult),
                         reads=[("thr", si)], writes=[("a2", si)])
            for (c, si, xb) in P:
                t = T[si]
                S.op("dve", lambda v: v.scalar_tensor_tensor(out=t["thi"][:, 0:n], in0=t["thi"][:, 0:n], scalar=1.0,
                                                             in1=bank[xb][:, 0:n], op0=ALU.add, op1=ALU.mult),
                     reads=[("thi", si), ("bank", xb)], writes=[("thi", si)])
            if full:
                for (c, si, xb) in P:
                    t = T[si]
                    S.op("dve", lambda v: v.tensor_scalar(out=t["g"][:, 0:n], in0=t["g"][:, 0:n], scalar1=GC0 * GC1, scalar2=GC0,
                                                          op0=ALU.mult, op1=ALU.add), reads=[("g", si)], writes=[("g", si)])
                for (c, si, xb) in P:
                    t = T[si]
                    S.op("dve", lambda v: v.tensor_tensor(out=t["g"][:, 0:n], in0=t["g"][:, 0:n], in1=t["uys"][:, 0:n], op=ALU.mult),
                         reads=[("g", si), ("uys", si)], writes=[("g", si)])

        def A_BQ(P, n):
            for (c, si, xb) in P:
                t = T[si]
                S.op("act", lambda a: a.activation(out=t["a2"][:, 0:n], in_=t["a2"][:, 0:n], func=AF.Sqrt,
                                                   scale=-1.0, bias=1.0), reads=[("a2", si)], writes=[("a2", si)], dist=F)

        def A_B1(P, n):
            for (c, si, xb) in P:
                t = T[si]
                S.op("dve", lambda v: v.tensor_tensor(out=t["thi"][:, 0:n], in0=t["a2"][:, 0:n], in1=t["thi"][:, 0:n], op=ALU.mult),
                     reads=[("a2", si), ("thi", si)], writes=[("thi", si)], dist=F)
            for (c, si, xb) in P:
                t = T[si]
                S.op("dve", lambda v: v.tensor_tensor_scan(out=t["a2"][:, 0:n], data0=t["thr"][:, 0:n], data1=t["thi"][:, 0:n],
                                                           initial=state[:, c:c + 1], op0=ALU.mult, op1=ALU.add),
                     reads=[("thr", si), ("thi", si), ("state", c)], writes=[("a2", si)], dist=F)
            for (c, si, xb) in P:
                t = T[si]
                S.op("dve", lambda v: v.tensor_copy(out=state[:, c:c + 1], in_=t["a2"][:, n - 1:n]), reads=[("a2", si)], writes=[("state", c)], dist=F)

        def A_B2(P, n, slot):
            for (c, si, xb) in P:
                t = T[si]
                S.op("act", lambda a: a.activation(out=t["g"][:, 0:n], in_=t["g"][:, 0:n], func=AF.Tanh),
                     reads=[("g", si)], writes=[("g", si)], dist=F)
            for (c, si, xb) in P:
                t = T[si]
                S.op("dve", lambda v: v.scalar_tensor_tensor(out=t["g"][:, 0:n], in0=t["g"][:, 0:n], scalar=1.0,
                                                             in1=t["uys"][:, 0:n], op0=ALU.add, op1=ALU.mult),
                     reads=[("g", si), ("uys", si)], writes=[("g", si)], dist=F)
            for (c, si, xb) in P:
                t = T[si]
                S.op("dve", lambda v: v.scalar_tensor_tensor(out=zT[slot][:, c, 0:n], in0=t["a2"][:, 0:n], scalar=0.25,
                                                             in1=t["g"][:, 0:n], op0=ALU.mult, op1=ALU.mult),
                     reads=[("a2", si), ("g", si)], writes=[("zT", slot, c)], dist=F)

        def B_F1(P, n, slot):
            for (c, si, xb) in P:
                t = T[si]
                S.group("pe", mm_acc(B_CG, D + c * 128, n, slot), reads=WIN_R + [("xT", slot, t_) for t_ in range(4)], writes=[("bank", B_CG)])
                S.op("act", lambda a: a.activation(out=t["g"][:, 0:n], in_=bank[B_CG][:, 0:n], func=AF.Copy),
                     reads=[("bank", B_CG)], writes=[("g", si)], dist=F)
                S.group("pe", mm_acc(B_V, 2 * D + c * 128, n, slot), reads=WIN_R + [("xT", slot, t_) for t_ in range(4)], writes=[("bank", B_V)])
                S.op("dve", lambda v: v.tensor_tensor(out=ub[:, c, HIST:HIST + n], in0=t["g"][:, 0:n], in1=bank[B_V][:, 0:n], op=ALU.mult),
                     reads=[("g", si), ("bank", B_V)], writes=[("ub", c)], dist=F)
            for (c, si, xb) in P:
                t = T[si]
                S.group("pe", mm_acc(B_BG, c * 128, n, slot), reads=WIN_R + [("xT", slot, t_) for t_ in range(4)], writes=[("bank", B_BG)])
                S.op("act", lambda a: a.activation(out=t["thr"][:, 0:n], in_=bank[B_BG][:, 0:n], func=AF.Copy),
                     reads=[("bank", B_BG)], writes=[("thr", si)], dist=F)

        def B_F2(P, n, slot):
            for (c, si, xb) in P:
                S.group("pe", conv_mm(c, n, xb), reads=[("ub", c), ("dg", c)], writes=[("bank", xb)])
                hist_update(c, n)
            for (c, si, xb) in P:
                t = T[si]
                S.op("dve", lambda v: v.tensor_tensor(out=zT[slot][:, c, 0:n], in0=t["thr"][:, 0:n], in1=bank[xb][:, 0:n], op=ALU.mult),
                     reads=[("thr", si), ("bank", xb)], writes=[("zT", slot, c)], dist=F)

        ZT_R = lambda slot: [("zT", slot, c) for c in range(KC)]

        def mix_mm(ti, slot, off, half):
            tok0, n = TT[ti]
            if half == 0 and ti == 0:
                S.dma("sp", lambda g: g.dma_start(out=xres2[0][0:n, :], in_=src[tok0:tok0 + n, :]), writes=[("xres", 0)])
            S.group("pe", [lambda pe, k=k: pe.matmul(out=bank[B_MIX][0:n, :], lhsT=zT[slot][:, k, off:off + n],
                                                     rhs=wout[:, k, half * 512:(half + 1) * 512],
                                                     start=(k == 0), stop=(k == KC - 1)) for k in range(KC)],
                    reads=ZT_R(slot) + ["wout"], writes=[("bank", B_MIX)])

        def mix_res(ti, half):
            tok0, n = TT[ti]
            xres = xres2[ti % 3]
            S.op("dve", lambda v: v.scalar_tensor_tensor(
                out=xres[0:n, half * 512:(half + 1) * 512], in0=xres[0:n, half * 512:(half + 1) * 512], scalar=ALPHA,
                in1=bank[B_MIX][0:n, :], op0=ALU.mult, op1=ALU.add),
                reads=[("xres", ti % 3), ("bank", B_MIX)], writes=[("xres", ti % 3)])
            S.op("dve", lambda v: v.bn_stats(out=stats[0:n, half, :], in_=xres[0:n, half * 512:(half + 1) * 512]),
                 reads=[("xres", ti % 3)], writes=[("stats", half)])
            if half == 1:
                S.op("dve", lambda v: v.bn_aggr(out=mv[0:n, :], in_=stats[0:n].rearrange("p a b -> p (a b)")),
                     reads=[("stats", 0), ("stats", 1)], writes=["mv"])
                S.op("dve", lambda v: v.tensor_scalar_add(out=rstd[0:n, :], in0=mv[0:n, 1:2], scalar1=LN_EPS), reads=["mv"], writes=["rstd"])
                S.op("dve", lambda v: v.tensor_scalar_mul(out=nmr[0:n, :], in0=mv[0:n, 0:1], scalar1=-1.0), reads=["mv"], writes=["nmr"])
                S.op("pool", lambda v: v.tensor_tensor(out=rstd[0:n, :], in0=rstd[0:n, :], in1=c_mhalf[0:n, :], op=ALU.pow),
                     reads=["rstd", "c_mhalf"], writes=["rstd"])
                S.op("pool", lambda v: v.tensor_tensor(out=nmr[0:n, :], in0=nmr[0:n, :], in1=rstd[0:n, :], op=ALU.mult),
                     reads=["rstd", "nmr"], writes=["nmr"])

        def ln_out(ti):
            tok0, n = TT[ti]
            xres = xres2[ti % 3]
            S.op("act", lambda a: a.activation(out=h1[0:n, :], in_=xres[0:n, :], func=AF.Identity, scale=rstd[0:n, :], bias=nmr[0:n, :]),
                 reads=[("xres", ti % 3), "rstd", "nmr"], writes=["h1"])
            S.op("dve", lambda g: g.tensor_tensor(out=h1[0:n, :], in0=h1[0:n, :], in1=lng[0:n, :], op=ALU.mult),
                 reads=["h1", "lng"], writes=["h1"], dist=1)
            S.op("dve", lambda g: g.tensor_tensor(out=h1[0:n, :], in0=h1[0:n, :], in1=lnb[0:n, :], op=ALU.add),
                 reads=["h1", "lnb"], writes=["h1"])
            S.dma("sp", lambda g: g.dma_start(out=hA[tok0:tok0 + n, :], in_=h1[0:n, :]), reads=["h1"], writes=["hA"])
            S.dma("pool", lambda g: g.dma_start(out=hAb[tok0:tok0 + n, :], in_=h1[0:n, :]), reads=["h1"], writes=["hAb"])

        def rt_tr(ti, half):
            tok0, n = TT[ti]
            S.group("pe", [lambda pe, jj=jj: pe.transpose(out=tpb_f[:, jj * 128:jj * 128 + n],
                                                          in_=h1[0:n, (half * 4 + jj) * 128:(half * 4 + jj + 1) * 128],
                                                          identity=ident_f[0:n, 0:n]) for jj in range(4)],
                    reads=["h1", "ident_f"], writes=[("bank", 0)])

        def rt_cp(ti, half):
            tok0, n = TT[ti]
            S.op("act", lambda a: a.activation(
                out=h1T[:, half * 512:(half + 1) * 512].rearrange("p (j t) -> p j t", j=4)[:, :, 0:n],
                in_=tpb_f.rearrange("p (j t) -> p j t", j=4)[:, :, 0:n], func=AF.Copy),
                reads=[("bank", 0)], writes=[("h1T", half)])

        def rt_logits(ti, j, gp):
            tok0, n = TT[ti]
            lg = lg2[gp]
            S.group("pe", [lambda pe, k=k: pe.matmul(out=bank[LGB][0:n, 0:36], lhsT=h1T[:, k * 128:k * 128 + n], rhs=wrt[:, k, :],
                                                     start=(k == 0), stop=(k == KC - 1)) for k in range(KC)],
                    reads=[("h1T", 0), ("h1T", 1), "wrt"], writes=[("bank", LGB)])
            S.op("dve", lambda v: v.tensor_tensor(out=lg[0:n, j, :], in0=bank[LGB][0:n, 0:36], in1=brt[0:n, :], op=ALU.add),
                 reads=[("bank", LGB), "brt"], writes=[("lg", gp, j)])

        BG = []

        class _Deferred:
            def op(self, *a, **k):
                BG.append(("op", a, k))

            def group(self, *a, **k):
                BG.append(("group", a, k))

            def dma(self, *a, **k):
                BG.append(("dma", a, k))

        SD = _Deferred()

        def drain_bg(nmax):
            for _ in range(nmax):
                if not BG:
                    return
                kind_, a, k = BG.pop(0)
                getattr(S, kind_)(*a, **k)

        def router_part(tis, part, gp):
            T_ = len(tis); ti0 = tis[0]; n = TT[ti0][1]
            lg = lg2[gp]
            LG = [("lg", gp, j) for j in range(T_)]
            V = lambda fn, r, w: SD.op("dve", fn, reads=r, writes=w)
            bc = lambda ap, shape: ap.unsqueeze(2).to_broadcast(shape)
            lgv = lg[0:n, 0:T_, :]
            if part == 1:
                V(lambda v: v.reduce_max(out=gmx[0:n, 0:T_], in_=lgv[:, :, 0:4], axis=AX.X), LG, ["gmx"])
                V(lambda v: v.tensor_tensor(out=gsh[0:n, 0:T_, :], in0=lgv[:, :, 0:4], in1=bc(gmx[0:n, 0:T_], [n, T_, 4]), op=ALU.subtract), LG + ["gmx"], ["gsh"])
                SD.op("act", lambda a: a.activation(out=gex[0:n, 0:T_, :], in_=gsh[0:n, 0:T_, :], func=AF.Exp), reads=["gsh"], writes=["gex"])
                V(lambda v: v.tensor_scalar(out=goh[0:n, 0:T_, :], in0=gsh[0:n, 0:T_, :], scalar1=0.0, scalar2=None, op0=ALU.is_ge), ["gsh"], ["goh"])
                V(lambda v: v.tensor_scalar(out=goh[0:n, 0:T_, :], in0=goh[0:n, 0:T_, :], scalar1=BIG, scalar2=-BIG, op0=ALU.mult, op1=ALU.add), ["goh"], ["goh"])
                V(lambda v: v.tensor_tensor(out=elm[0:n, 0:T_, :].rearrange("p t (g e) -> p t g e", g=4),
                                            in0=lgv[:, :, 4:36].rearrange("p t (g e) -> p t g e", g=4),
                                            in1=goh[0:n, 0:T_, :].unsqueeze(3).to_broadcast([n, T_, 4, 8]), op=ALU.add), LG + ["goh"], ["elm"])
                V(lambda v: v.reduce_sum(out=gsum[0:n, 0:T_], in_=gex[0:n, 0:T_, :], axis=AX.X), ["gex"], ["gsum"])
                V(lambda v: v.reduce_max(out=m1[0:n, 0:T_], in_=elm[0:n, 0:T_, :], axis=AX.X), ["elm"], ["m1"])
                V(lambda v: v.reciprocal(out=pg[0:n, 0:T_], in_=gsum[0:n, 0:T_]), ["gsum"], ["pg"])
                V(lambda v: v.tensor_tensor(out=oh1[0:n, 0:T_, :], in0=elm[0:n, 0:T_, :], in1=bc(m1[0:n, 0:T_], [n, T_, NE]), op=ALU.is_ge), ["elm", "m1"], ["oh1"])
                V(lambda v: v.scalar_tensor_tensor(out=el2[0:n, 0:T_, :], in0=oh1[0:n, 0:T_, :], scalar=-BIG, in1=elm[0:n, 0:T_, :],
                                                   op0=ALU.mult, op1=ALU.add), ["oh1", "elm"], ["el2"])
                V(lambda v: v.reduce_max(out=m2[0:n, 0:T_], in_=el2[0:n, 0:T_, :], axis=AX.X), ["el2"], ["m2"])
                V(lambda v: v.tensor_tensor(out=oh2[0:n, 0:T_, :], in0=el2[0:n, 0:T_, :], in1=bc(m2[0:n, 0:T_], [n, T_, NE]), op=ALU.is_ge), ["el2", "m2"], ["oh2"])
                V(lambda v: v.tensor_tensor(out=ohb[0:n, 0:T_, :], in0=oh1[0:n, 0:T_, :], in1=oh2[0:n, 0:T_, :], op=ALU.add), ["oh1", "oh2"], ["ohb"])
                V(lambda v: v.tensor_sub(out=w1[0:n, 0:T_], in0=m1[0:n, 0:T_], in1=m2[0:n, 0:T_]), ["m1", "m2"], ["w1"])
                SD.op("act", lambda a: a.activation(out=w1[0:n, 0:T_], in_=w1[0:n, 0:T_], func=AF.Tanh, scale=0.5), reads=["w1"], writes=["w1"])
            if part == 2:
                fns = []
                for j in range(T_):
                    seq = [(tri_b, j)] + [(ones_b, i) for i in range(j)]
                    for q_, (lh, i) in enumerate(seq):
                        fns.append(lambda pe, lh=lh, i=i, j=j, q_=q_, L=len(seq): pe.matmul(
                            out=bank[LGB][:, 64 + j * NE:64 + (j + 1) * NE], lhsT=lh[:], rhs=ohb[:, i, :], start=(q_ == 0), stop=(q_ == L - 1)))
                for j in range(T_):
                    fns.append(lambda pe, j=j: pe.matmul(out=bank[LGB][:, 64 + T_ * NE:64 + (T_ + 1) * NE], lhsT=ones_b[:], rhs=ohb[:, j, :],
                                                         start=(j == 0), stop=(j == T_ - 1)))
                SD.group("pe", fns, reads=["tri_b", "ones_b", "ohb"] + LG, writes=[("bank", LGB)])
                V(lambda v: v.tensor_scalar(out=w1[0:n, 0:T_], in0=w1[0:n, 0:T_], scalar1=0.5, scalar2=0.5, op0=ALU.mult, op1=ALU.add), ["w1"], ["w1"])
                V(lambda v: v.tensor_mul(out=gate[0][0:n, ti0:ti0 + T_], in0=w1[0:n, 0:T_], in1=pg[0:n, 0:T_]), ["w1", "pg", ("gate", 0)], [("gate", 0)])
                V(lambda v: v.tensor_sub(out=gate[1][0:n, ti0:ti0 + T_], in0=pg[0:n, 0:T_], in1=gate[0][0:n, ti0:ti0 + T_]), ["pg", ("gate", 0), ("gate", 1)], [("gate", 1)])
                V(lambda v: v.tensor_tensor(out=rk[:, 0:T_, :], in0=bank[LGB][:, 64:64 + T_ * NE].rearrange("p (t e) -> p t e", e=NE),
                                            in1=tot[:].unsqueeze(1).to_broadcast([128, T_, NE]), op=ALU.add), [("bank", LGB), "tot"], ["rk"])
                V(lambda v: v.tensor_tensor(out=tot[:], in0=bank[LGB][:, 64 + T_ * NE:64 + (T_ + 1) * NE], in1=tot[:], op=ALU.add), [("bank", LGB), "tot", "rk"], ["tot"])
                V(lambda v: v.tensor_scalar(out=ov[0:n, 0:T_, :], in0=rk[0:n, 0:T_, :], scalar1=float(CAP), scalar2=BIG, op0=ALU.is_ge, op1=ALU.mult), ["rk"], ["ov"])
            for k, ohk in ((0, oh1), (1, oh2)):
                if (part == 2 and k == 1) or (part == 3 and k == 0) or part == 1:
                    continue
                kk = ["sel", "rkk", "ysf", "ovk", "eif", "pf", "bf", "bf2"]
                V(lambda v, ohk=ohk: v.tensor_mul(out=sel[0:n, 0:T_, :], in0=ohk[0:n, 0:T_, :], in1=rk[0:n, 0:T_, :]), ["oh1", "oh2", "rk"] + kk, ["sel"])
                V(lambda v: v.reduce_sum(out=rkk[0:n, 0:T_], in_=sel[0:n, 0:T_, :], axis=AX.X), ["sel"], ["rkk"])
                V(lambda v, ohk=ohk: v.tensor_mul(out=sel[0:n, 0:T_, :], in0=ohk[0:n, 0:T_, :], in1=ov[0:n, 0:T_, :]), ["oh1", "oh2", "ov", "sel"], ["sel"])
                V(lambda v: v.reduce_sum(out=ovk[0:n, 0:T_], in_=sel[0:n, 0:T_, :], axis=AX.X), ["sel"], ["ovk"])
                V(lambda v, ohk=ohk: v.tensor_mul(out=sel[0:n, 0:T_, :], in0=ohk[0:n, 0:T_, :], in1=ecrow4[0:n, 0:T_, :]), ["oh1", "oh2", "sel"] + EC4, ["sel"])
                V(lambda v: v.reduce_sum(out=eif[0:n, 0:T_], in_=sel[0:n, 0:T_, :], axis=AX.X), ["sel"], ["eif"])
                V(lambda v: v.tensor_add(out=ysf[0:n, 0:T_], in0=eif[0:n, 0:T_], in1=rkk[0:n, 0:T_]), ["eif", "rkk"], ["ysf"])
                V(lambda v: v.tensor_scalar(out=bf[0:n, 0:T_], in0=rkk[0:n, 0:T_], scalar1=128.0, scalar2=None, op0=ALU.is_ge), ["rkk"], ["bf"])
                V(lambda v: v.tensor_add(out=ysf[0:n, 0:T_], in0=ysf[0:n, 0:T_], in1=ovk[0:n, 0:T_]), ["ysf", "ovk"], ["ysf"])
                for m in range(2, NB):
                    V(lambda v, m=m: v.tensor_scalar(out=bf2[0:n, 0:T_], in0=rkk[0:n, 0:T_], scalar1=128.0 * m, scalar2=None, op0=ALU.is_ge), ["rkk"], ["bf2"])
                    V(lambda v: v.tensor_add(out=bf[0:n, 0:T_], in0=bf[0:n, 0:T_], in1=bf2[0:n, 0:T_]), ["bf", "bf2"], ["bf"])
                V(lambda v: v.tensor_scalar_min(out=ysf[0:n, 0:T_], in0=ysf[0:n, 0:T_], scalar1=float(NSLOT)), ["ysf"], ["ysf"])
                V(lambda v: v.scalar_tensor_tensor(out=pf[0:n, 0:T_], in0=bf[0:n, 0:T_], scalar=-128.0, in1=rkk[0:n, 0:T_],
                                                   op0=ALU.mult, op1=ALU.add), ["bf", "rkk"], ["pf"])
                V(lambda v, k=k: v.tensor_copy(out=ysrow[k][0:n, ti0:ti0 + T_], in_=ysf[0:n, 0:T_]), ["ysf", ("ysrow", k)], [("ysrow", k)])
                V(lambda v: v.scalar_tensor_tensor(out=pf[0:n, 0:T_], in0=pf[0:n, 0:T_], scalar=float(NE * NB), in1=bf[0:n, 0:T_],
                                                   op0=ALU.mult, op1=ALU.add), ["pf", "bf"], ["pf"])
                V(lambda v: v.scalar_tensor_tensor(out=pf[0:n, 0:T_], in0=eif[0:n, 0:T_], scalar=float(NB) / float(CAP), in1=pf[0:n, 0:T_],
                                                   op0=ALU.mult, op1=ALU.add), ["eif", "pf"], ["pf"])
                V(lambda v: v.tensor_add(out=pf[0:n, 0:T_], in0=pf[0:n, 0:T_], in1=ovk[0:n, 0:T_]), ["pf", "ovk"], ["pf"])
                V(lambda v, k=k: v.tensor_copy(out=dest[k][0:n, ti0:ti0 + T_], in_=pf[0:n, 0:T_]), ["pf", ("dest", k)], [("dest", k)])
                for j, ti in enumerate(tis):
                    SD.dma("pool", lambda g, k=k, ti=ti: g.indirect_dma_start(
                        out=stok[:, :], out_offset=bass.IndirectOffsetOnAxis(ap=dest[k][:, ti:ti + 1], axis=0),
                        in_=tokidx[:, ti, :], in_offset=None, bounds_check=bc_reg, oob_is_err=False),
                        reads=[("dest", k), "tokidx", "stok"], writes=[("stok_sc", ti, k)])

        gctr = [0]
        pending = []
        active = []
        groups = []
        stg = STG_Q

        def jobs_in(ph):
            return [jb for jb in active if jb["ph"] == ph]

        NBG = 8

        def hook_pe_early():
            drain_bg(NBG)
            for jb in jobs_in(1):
                mix_mm(jb["ti"], jb["slot"], jb["off"], 1)
            for jb in jobs_in(2):
                rt_tr(jb["ti"], 0)

        def hook_dve_mid():
            drain_bg(NBG)
            for jb in jobs_in(1):
                mix_res(jb["ti"], 1)
            for jb in jobs_in(2):
                rt_cp(jb["ti"], 0)

        def hook_pe_mid():
            drain_bg(NBG)
            for jb in jobs_in(0):
                mix_mm(jb["ti"], jb["slot"], jb["off"], 0)
            for jb in jobs_in(2):
                rt_tr(jb["ti"], 1)

        def hook_start():
            for jb in jobs_in(2):
                ln_out(jb["ti"])

        def hook_out():
            drain_bg(NBG)
            for jb in jobs_in(2):
                rt_cp(jb["ti"], 1)

        def hook_end():
            for g_ in list(groups):
                if all(jb["ph"] >= 3 for jb in g_["jobs"]):
                    for part in (1, 2, 3):
                        router_part(g_["tis"], part, g_["gp"])
                    groups.remove(g_)
                    for jb in g_["jobs"]:
                        active.remove(jb)
                    break
            for jb in jobs_in(0):
                mix_res(jb["ti"], 0)
                ti = jb["ti"]
                if ti + 1 < NTT:
                    t1_, n1 = TT[ti + 1]
                    S.dma("sp", lambda g: g.dma_start(out=xres2[(ti + 1) % 3][0:n1, :], in_=src[t1_:t1_ + n1, :]), writes=[("xres", (ti + 1) % 3)])
            for jb in jobs_in(2):
                rt_logits(jb["ti"], jb["j"], jb["gp"])
            drain_bg(NBG)

        def end_slot():
            for jb in active:
                if jb["ph"] < 3:
                    jb["ph"] += 1
            if pending:
                jb = pending.pop(0)
                jb["ph"] = 0
                active.append(jb)

        slot_ctr = [0]

        def stg_rate(k, full):
            slot_ctr[0] += 1
            if isA and not full:
                return 1
            if isA:
                return 3
            return 2

        tiles = []
        if isA:
            tiles += [(src_pre, tok0, n, None, False) for (tok0, n, _) in FMPRE]
        tiles += [(src, tok0, n, tts, True) for (tok0, n, tts) in FM]
        ntl = len(tiles)
        pairs = []
        for k in range(ntl):
            for pr in range(KC // 2):
                pairs.append((k, pr))

        def mkP(q):
            k, pr = pairs[q]
            base = 2 * (q % 2)
            return [(2 * pr, base, B_XC[0]), (2 * pr + 1, base + 1, B_XC[1])]

        def front1(q):
            k, pr = pairs[q]; n = tiles[k][2]; slot = k % 2
            if isA:
                A_F1(mkP(q), n, slot, tiles[k][4])
            else:
                B_F1(mkP(q), n, slot)

        load_dma(tiles[0][0], tiles[0][1], tiles[0][2]); load_tr(tiles[0][2], 0)
        if ntl > 1:
            load_dma(tiles[1][0], tiles[1][1], tiles[1][2])
        npairs = len(pairs)
        front1(0)
        if isA:
            A_F2(mkP(0), tiles[0][2], 0, tiles[0][4])
        for q in range(npairs):
            k, pr = pairs[q]; n = tiles[k][2]; slot = k % 2; full = tiles[k][4]
            P = mkP(q)
            if isA:
                A_BQ(P, n)
            hook_start()
            if q + 1 < npairs:
                front1(q + 1)
            hook_pe_early()
            if isA:
                A_B1(P, n)
            hook_dve_mid()
            if isA:
                if q + 1 < npairs:
                    k1, _ = pairs[q + 1]
                    A_F2(mkP(q + 1), tiles[k1][2], k1 % 2, tiles[k1][4])
            hook_pe_mid()
            if not isA:
                B_F2(P, n, slot)
            hook_out()
            if isA and full:
                A_B2(P, n, slot)
            if isA and (not full) and q + 1 < npairs and tiles[pairs[q + 1][0]][4]:
                S.op("dve", lambda v: v.tensor_scalar(out=state[:], in0=state[:], scalar1=flg[:, 0:1], scalar2=None, op0=ALU.mult),
                     reads=[("state", c) for c in range(KC)] + ["flg"], writes=[("state", c) for c in range(KC)])
            hook_end()
            if pr == 1 and k + 1 < ntl:
                load_tr(tiles[k + 1][2], (k + 1) % 2)
                if k + 2 < ntl:
                    load_dma(tiles[k + 2][0], tiles[k + 2][1], tiles[k + 2][2])
            for _ in range(stg_rate(k, full)):
                if stg:
                    stg.pop(0)()
            if pr == KC // 2 - 1 and tiles[k][3] is not None:
                tis = list(tiles[k][3])
                gctr[0] += 1
                g_ = dict(tis=tis, jobs=[], part=0, gp=gctr[0] % 2)
                for j, ti in enumerate(tis):
                    jb = dict(ti=ti, slot=slot, off=j * 128, j=j, ph=-1, gp=gctr[0] % 2)
                    g_["jobs"].append(jb); pending.append(jb)
                groups.append(g_)
            end_slot()
        while pending or active or BG:
            hook_start(); hook_pe_early(); hook_dve_mid(); hook_pe_mid(); hook_out(); hook_end()
            end_slot()
        while stg and STG_DONE[0] < STG_NEED[layer]:
            stg.pop(0)()
        S.barrier()
        st.close()

    def moe_phase(layer, final):
        st = contextlib.ExitStack()
        lng = sb(st, "m_lng", [128, D]); lnb = sb(st, "m_lnb", [128, D])
        S.dma("sp", lambda g: g.dma_start(out=lng[:], in_=lnp[layer * 2 + 1, 0]), writes=["lng"])
        S.dma("sp", lambda g: g.dma_start(out=lnb[:], in_=lnp[layer * 2 + 1, 1]), writes=["lnb"])
        idx = sb(st, "idx", [128, NE * NB, 2], I32)
        S.dma("sp", lambda g: g.dma_start(out=idx[:], in_=stok.rearrange("(p f) o -> p f o", p=128)), writes=["idx"])
        NWB = 3
        wg = [sb(st, "wg%d" % i, [128, KC, DEXP], BF16) for i in range(NWB)]
        wu = [sb(st, "wu%d" % i, [128, KC, DEXP], BF16) for i in range(NWB)]
        wd = [sb(st, "wd%d" % i, [128, 4, D], BF16) for i in range(NWB)]
        xs = [sb(st, "xs%d" % i, [128, NB, D], BF16) for i in range(NWB)]
        xsT = [sb(st, "xsT%d" % i, [128, KC, CAP], BF16) for i in range(2)]
        sg = [sb(st, "sg%d" % i, [128, CAP]) for i in range(2)]
        hbT = [sb(st, "hbT%d" % i, [128, 4, CAP], BF16) for i in range(2)]
        ysb = [sb(st, "ysb%d" % i, [128, D]) for i in range(2)]
        tpb = tpb_all

        def load_w(e):
            s = e % NWB
            if "g" in STAGED[layer]:
                S.dma("sp", lambda g: g.dma_start(out=wg[s][:], in_=wgbL[layer][e].rearrange("(k p) f -> p k f", p=128)), writes=[("wg", s)])
            else:
                S.dma("pool", lambda g: g.dma_start(out=wg[s][:], in_=wgate[layer, e].rearrange("(k p) f -> p k f", p=128)), writes=[("wg", s)])
            if "u" in STAGED[layer]:
                S.dma("sp", lambda g: g.dma_start(out=wu[s][:], in_=wubL[layer][e].rearrange("(k p) f -> p k f", p=128)), writes=[("wu", s)])
            else:
                S.dma("pool", lambda g: g.dma_start(out=wu[s][:], in_=wup[layer, e].rearrange("(k p) f -> p k f", p=128)), writes=[("wu", s)])
            if "d" in STAGED[layer]:
                S.dma("sp", lambda g: g.dma_start(out=wd[s][:], in_=wdbL[layer][e].rearrange("(j p) m -> p j m", p=128)), writes=[("wd", s)])
            else:
                S.dma("pool", lambda g: g.dma_start(out=wd[s][:], in_=wdown[layer, e].rearrange("(j p) m -> p j m", p=128)), writes=[("wd", s)])
            for b in range(NB):
                S.dma("pool", lambda g, b=b: g.indirect_dma_start(
                    out=xs[s][:, b, :], out_offset=None, in_=hAb[:, :],
                    in_offset=bass.IndirectOffsetOnAxis(ap=idx[:, e * NB + b, 0:1], axis=0)),
                    reads=["idx"], writes=[("xs", s, b)])

        def xs_transpose(e, b):
            s = e % 2
            sw = e % NWB
            tb_i = 0 if (e * NB + b) % 2 == 0 else 7
            tpx = tpb_all if tb_i == 0 else tpb_7
            S.group("pe", [lambda pe, j=j: pe.transpose(out=tpx[:, j * 128:(j + 1) * 128], in_=xs[sw][:, b, j * 128:(j + 1) * 128],
                                                        identity=ident_b[:]) for j in range(KC)],
                    reads=[("xs", sw, b), "ident_b"], writes=[("bank", tb_i)])
            if b % 2 == 0:
                S.op("act", lambda a: a.activation(out=xsT[s][:, :, b * 128:(b + 1) * 128],
                                                   in_=tpx.rearrange("p (k t) -> p k t", k=KC), func=AF.Copy),
                     reads=[("bank", tb_i)], writes=[("xsT", s, b)])
            else:
                S.op("dve", lambda v: v.tensor_copy(out=xsT[s][:, :, b * 128:(b + 1) * 128],
                                                    in_=tpx.rearrange("p (k t) -> p k t", k=KC)),
                     reads=[("bank", tb_i)], writes=[("xsT", s, b)])

        load_w(0)
        load_w(1)
        for b in range(NB):
            xs_transpose(0, b)
        ysi = 0
        for e in range(NE):
            s = e % 2
            sw = e % NWB
            if e + 2 < NE:
                load_w(e + 2)
            XR = [("xsT", s, b) for b in range(NB)]
            for j in range(4):
                gb, ubk = (1, 2) if j % 2 == 0 else (3, 4)
                S.group("pe", [lambda pe, k=k, j=j: pe.matmul(out=bank[gb][:, 0:CAP], lhsT=wg[sw][:, k, j * 128:(j + 1) * 128],
                                                              rhs=xsT[s][:, k, :], start=(k == 0), stop=(k == KC - 1)) for k in range(KC)],
                        reads=XR + [("wg", sw)], writes=[("bank", gb)])
                S.group("pe", [lambda pe, k=k, j=j: pe.matmul(out=bank[ubk][:, 0:CAP], lhsT=wu[sw][:, k, j * 128:(j + 1) * 128],
                                                              rhs=xsT[s][:, k, :], start=(k == 0), stop=(k == KC - 1)) for k in range(KC)],
                        reads=XR + [("wu", sw)], writes=[("bank", ubk)])
                S.op("act", lambda a, j=j: a.activation(out=sg[j % 2][:], in_=bank[gb][:, 0:CAP], func=AF.Silu),
                     reads=[("bank", gb)], writes=[("sg", j % 2)])
                S.op("dve", lambda v, j=j: v.tensor_tensor(out=hbT[s][:, j, :], in0=sg[j % 2][:], in1=bank[ubk][:, 0:CAP], op=ALU.mult),
                     reads=[("sg", j % 2), ("bank", ubk)], writes=[("hbT", s, j)])
                if j < NB and e + 1 < NE:
                    xs_transpose(e + 1, j)
            HR = [("hbT", s, j) for j in range(4)]
            for b in range(NB):
                yb = ysb[ysi % 2]
                for half in range(2):
                    S.group("pe", [lambda pe, j=j, b=b, half=half: pe.matmul(out=bank[5 + half][:, :], lhsT=hbT[s][:, j, b * 128:(b + 1) * 128],
                                                                             rhs=wd[sw][:, j, half * 512:(half + 1) * 512],
                                                                             start=(j == 0), stop=(j == 3)) for j in range(4)],
                            reads=HR + [("wd", sw)], writes=[("bank", 5 + half)])
                S.op("act", lambda a: a.activation(out=yb[:, 0:512], in_=bank[5][:, :], func=AF.Copy),
                     reads=[("bank", 5)], writes=[("ysb", ysi % 2, 0)])
                S.op("dve", lambda v: v.tensor_copy(out=yb[:, 512:1024], in_=bank[6][:, :]),
                     reads=[("bank", 6)], writes=[("ysb", ysi % 2, 1)])
                r0 = (e * NB + b) * 128
                S.dma("sp", lambda g, r0=r0, yb=yb: g.dma_start(out=ys[r0:r0 + 128, :], in_=yb[:]),
                      reads=[("ysb", ysi % 2, 0), ("ysb", ysi % 2, 1)], writes=[("ys", e, b)])
                ysi += 1
        S.barrier()
        ya = [sb(st, "ya%d" % i, [128, D]) for i in range(2)]
        ybb = [sb(st, "yb%d" % i, [128, D]) for i in range(2)]
        hr = [sb(st, "hr%d" % i, [128, D]) for i in range(2)]
        vvs = [sb(st, "m_vv%d" % i, [128, D]) for i in range(3)]
        ho = [sb(st, "m_ho%d" % i, [128, D]) for i in range(2)]
        stats2 = [sb(st, "m_stats%d" % i, [128, 2, 6]) for i in range(3)]
        mv2 = [sb(st, "m_mv%d" % i, [128, 2]) for i in range(3)]
        rstd2 = [sb(st, "m_rstd%d" % i, [128, 1]) for i in range(3)]
        nmr2 = [sb(st, "m_nmr%d" % i, [128, 1]) for i in range(3)]

        eps_t = sb(st, "eps_t", [128, 1])
        S.op("dve", lambda v: v.memset(eps_t[:], LN_EPS), writes=["eps_t"])

        def comb_load(ti):
            tok0, n = TT[ti]
            s = ti % 2
            S.dma("pool", lambda g: g.indirect_dma_start(out=ya[s][:], out_offset=None, in_=ys[:, :],
                                                         in_offset=bass.IndirectOffsetOnAxis(ap=ysrow[0][:, ti:ti + 1], axis=0)),
                  reads=[("ysrow", 0)], writes=[("ya", s)])
            S.dma("pool", lambda g: g.indirect_dma_start(out=ybb[s][:], out_offset=None, in_=ys[:, :],
                                                         in_offset=bass.IndirectOffsetOnAxis(ap=ysrow[1][:, ti:ti + 1], axis=0)),
                  reads=[("ysrow", 1)], writes=[("yb", s)])
            S.dma("sp", lambda g: g.dma_start(out=hr[s][0:n, :], in_=hA[tok0:tok0 + n, :]), writes=[("hr", s)])

        def comb_front(ti):
            tok0, n = TT[ti]
            s = ti % 2
            r = ti % 3
            v_ = vvs[r]; stats = stats2[r]; mv = mv2[r]; rstd = rstd2[r]; nmr = nmr2[r]
            S.op("act", lambda a: a.activation(out=v_[0:n, :], in_=ya[s][0:n, :], func=AF.Copy, scale=gate[0][0:n, ti:ti + 1]),
                 reads=[("ya", s), ("gate", 0)], writes=[("vv", r)])
            S.op("dve", lambda v: v.scalar_tensor_tensor(out=v_[0:n, :], in0=ybb[s][0:n, :], scalar=gate[1][0:n, ti:ti + 1], in1=v_[0:n, :],
                                                         op0=ALU.mult, op1=ALU.add), reads=[("yb", s), ("gate", 1), ("vv", r)], writes=[("vv", r)])
            S.op("dve", lambda v: v.scalar_tensor_tensor(out=v_[0:n, :], in0=hr[s][0:n, :], scalar=ALPHA, in1=v_[0:n, :],
                                                         op0=ALU.mult, op1=ALU.add), reads=[("hr", s), ("vv", r)], writes=[("vv", r)])
            for half in range(2):
                S.op("dve", lambda v, half=half: v.bn_stats(out=stats[0:n, half, :], in_=v_[0:n, half * 512:(half + 1) * 512]),
                     reads=[("vv", r)], writes=[("stats", r, half)])
            S.op("dve", lambda v: v.bn_aggr(out=mv[0:n, :], in_=stats[0:n].rearrange("p a b -> p (a b)")),
                 reads=[("stats", r, 0), ("stats", r, 1)], writes=[("mv", r)])
            S.op("act", lambda a: a.activation(out=rstd[0:n, :], in_=mv[0:n, 1:2], func=AF.Sqrt, bias=eps_t[0:n, :], scale=1.0),
                 reads=[("mv", r), "eps_t"], writes=[("rstd", r)])
            S.op("dve", lambda v: v.reciprocal(out=rstd[0:n, :], in_=rstd[0:n, :]), reads=[("rstd", r)], writes=[("rstd", r)])
            S.op("dve", lambda v: v.scalar_tensor_tensor(out=nmr[0:n, :], in0=mv[0:n, 0:1], scalar=-1.0, in1=rstd[0:n, :],
                                                         op0=ALU.mult, op1=ALU.mult), reads=[("mv", r), ("rstd", r)], writes=[("nmr", r)])

        def comb_tail(ti):
            tok0, n = TT[ti]
            s = ti % 2
            r = ti % 3
            v_ = vvs[r]; rstd = rstd2[r]; nmr = nmr2[r]
            S.op("act", lambda a: a.activation(out=v_[0:n, :], in_=v_[0:n, :], func=AF.Identity, scale=rstd[0:n, :], bias=nmr[0:n, :]),
                 reads=[("vv", r), ("rstd", r), ("nmr", r)], writes=[("vv", r)])
            S.op("dve", lambda g: g.tensor_tensor(out=v_[0:n, :], in0=v_[0:n, :], in1=lng[0:n, :], op=ALU.mult),
                 reads=[("vv", r), "lng"], writes=[("vv", r)])
            S.op("dve", lambda g: g.tensor_tensor(out=ho[s][0:n, :], in0=v_[0:n, :], in1=lnb[0:n, :], op=ALU.add),
                 reads=[("vv", r), "lnb"], writes=[("ho", s)])
            if final:
                if ti >= 1:
                    S.dma("sp", lambda g: g.dma_start(out=out[tok0 - NHEAD:tok0 - NHEAD + n, :], in_=ho[s][0:n, :]),
                          reads=[("ho", s)], writes=[("out", ti)])
            else:
                S.dma("sp", lambda g: g.dma_start(out=hB[tok0:tok0 + n, :], in_=ho[s][0:n, :]), reads=[("ho", s)], writes=[("hB", ti)])

        comb_load(0)
        comb_load(1)
        comb_front(0)
        for ti in range(NTT):
            if ti + 2 < NTT:
                comb_load(ti + 2)
            if ti + 1 < NTT:
                comb_front(ti + 1)
            comb_tail(ti)
        S.barrier()
        st.close()

    S.barrier()
    for ph in phases:
        if ph == "A":
            mixer_phase("A", 0, xin, xpre)
        elif ph == "B":
            mixer_phase("B", 1, hB, None)
        elif ph == "M0":
            moe_phase(0, False)
        elif ph == "M1":
            moe_phase(1, True)
    S.barrier()
    top.close()
    return nc


def _host_inputs(inp):
    f = lambda a: np.ascontiguousarray(np.asarray(a, dtype=np.float32))
    x = f(inp["x"]); meta = f(inp["meta_tokens"])

    def pvec(v):
        return np.ascontiguousarray(f(v).reshape(KC, 128).T)

    cw = f(inp["lru_conv_w"])[0]
    a_vec = np.stack([pvec(cw[0]), pvec(cw[1]), pvec(cw[2]), pvec(cw[3]), pvec(inp["lru_conv_b"][0]),
                      pvec(inp["lru_b_a"][0]), pvec(inp["lru_b_i"][0]), pvec(inp["lru_lambda"][0])], axis=1).reshape(128, 8 * KC)
    cwb = f(inp["sc_conv_w"])[0]
    b_vec = np.stack([pvec(cwb[0]), pvec(cwb[1]), pvec(cwb[2])], axis=1).reshape(128, 3 * KC)

    def blockdiag(w):
        w = f(w)
        o = np.zeros((128, KC, 128), np.float32)
        for c in range(KC):
            o[0:64, c, 0:64] = w[2 * c]
            o[64:128, c, 64:128] = w[2 * c + 1]
        return o

    lnp = np.zeros((4, 2, 128, D), np.float32)
    g = f(inp["ln_g"]); b = f(inp["ln_b"])
    for l in range(2):
        for j in range(2):
            lnp[l * 2 + j, 0] = g[l, j][None, :]
            lnp[l * 2 + j, 1] = b[l, j][None, :]
    wr = np.concatenate([f(inp["moe_w_group"]), f(inp["moe_w_expert"])], axis=2)
    brv = np.concatenate([f(inp["moe_b_group"]), f(inp["moe_b_expert"]).reshape(2, 32)], axis=1)
    br = np.ascontiguousarray(np.broadcast_to(brv[:, None, :], (2, 128, 36)))
    cst = np.zeros((128, 128 * 4 + 64), np.float32)
    cst[:, 0:128] = np.eye(128, dtype=np.float32)
    cst[:, 128:256] = np.triu(np.ones((128, 128), np.float32), k=1)
    cst[:, 256:384] = 1.0
    cst[:, 512:512 + NE] = (np.arange(NE, dtype=np.float32) * CAP)[None, :]
    csti = np.zeros((128, NTT, 2), np.int32)
    for ti, (t0, n) in enumerate(TT):
        csti[:, ti, 0] = t0 + np.arange(128)
        csti[:, ti, 1] = t0 + np.arange(128)
    csti = csti.reshape(128, NTT * 2)
    cbrow = np.ascontiguousarray(np.broadcast_to(f(inp["lru_conv_b"])[0][None, :], (2, D)))
    shared = dict(a_cbrow=cbrow, a_win=f(inp["lru_w_in"])[0], a_wout=f(inp["lru_w_out"])[0], a_vec=a_vec,
                  a_wa=blockdiag(inp["lru_w_a"][0]), a_wi=blockdiag(inp["lru_w_i"][0]),
                  b_win=f(inp["sc_w_in"])[0], b_wout=f(inp["sc_w_out"])[0], b_vec=b_vec, lnp=lnp, wr=wr, br=br,
                  wgate=f(inp["moe_w_gate"]), wup=f(inp["moe_w_up"]), wdown=f(inp["moe_w_down"]), cst=cst, csti=csti)
    maps = []
    for c in range(8):
        bb, half = c // 2, c % 2
        if half == 0:
            xin = np.concatenate([meta, x[bb, 0:NMAIN]], axis=0)
            xpre = np.zeros((NPRE, D), np.float32)
            flag = np.zeros((128, 1), np.float32)
        else:
            xin = x[bb, NMAIN - NHEAD:2 * NMAIN]
            xpre = np.concatenate([meta, x[bb, 0:NMAIN - NHEAD]], axis=0)
            flag = np.ones((128, 1), np.float32)
        m = dict(shared)
        m.update(xin=np.ascontiguousarray(xin), xpre=np.ascontiguousarray(xpre), flag=flag)
        maps.append(m)
    return maps


_NC_CACHE = {}


def kernel(**inputs):
    maps = _host_inputs(inputs)
    if "nc" not in _NC_CACHE:
        _NC_CACHE["nc"] = build_program()
    nc = _NC_CACHE["nc"]
    res = run_bass_kernel_spmd(nc, maps, core_ids=list(range(8)))
    outs = [np.asarray(r["out"], dtype=np.float32) for r in res.results]
    full = np.zeros((4, 2 * NMAIN, D), np.float32)
    for c in range(8):
        bb, half = c // 2, c % 2
        full[bb, half * NMAIN:(half + 1) * NMAIN] = outs[c]
    return full
```
